# Optimizing a Trainium2 kernel written in Bass

```python
import jax
import jax.numpy as jnp
from jax import lax
import numpy as np

D_MODEL = 1024
BATCH = 16
SEQ = 2048
DEPTH = 1

EPS = 1e-6
HEAD_DIM = 64
ROPE_THETA = 10000.0

NSA_HEADS = 8
NSA_KV_GROUPS = 2
NSA_GROUP = NSA_HEADS // NSA_KV_GROUPS
NSA_WIDTH = NSA_HEADS * HEAD_DIM
KV_WIDTH = NSA_KV_GROUPS * HEAD_DIM
CMP_BLOCK = 32
CMP_STRIDE = 16
CMP_HIDDEN = 128
SEL_BLOCK = 64
SEL_TOPK = 16
WINDOW = 512
Q_CHUNK = 32
FORCE_BONUS = 1000.0

HGRN_HEADS = 4
HGRN_EXPAND = 128
HGRN_HEAD_V = 128
HGRN_WIDTH = HGRN_HEADS * HGRN_EXPAND
HGRN_V_WIDTH = HGRN_HEADS * HGRN_HEAD_V
HGRN_CHUNK = 32

PEER_HEADS = 8
PEER_NKEYS = 128
PEER_NEXPERTS = PEER_NKEYS * PEER_NKEYS
PEER_QDIM = 256
PEER_TOPK = 16
PEER_TOK_CHUNK = 128

IN_SPLITS = (NSA_WIDTH, KV_WIDTH, KV_WIDTH, KV_WIDTH, KV_WIDTH, KV_WIDTH, KV_WIDTH, 3 * NSA_HEADS, HGRN_WIDTH, HGRN_WIDTH, HGRN_V_WIDTH, HGRN_V_WIDTH, 2 * D_MODEL)
IN_COLS = NSA_WIDTH + 6 * KV_WIDTH + 3 * NSA_HEADS + 2 * HGRN_WIDTH + 2 * HGRN_V_WIDTH + 2 * D_MODEL

kernel_name = 'nsa_hgrn2_peer_hybrid_block'


def _rmsnorm(x, g):
    xf = x.astype(jnp.float32)
    y = xf * lax.rsqrt(jnp.mean(xf * xf, axis=-1, keepdims=True) + EPS)
    return (y * g.astype(jnp.float32)).astype(x.dtype)


def _masked_softmax(s, mask):
    s = jnp.where(mask, s.astype(jnp.float32), -jnp.inf)
    m = jnp.max(s, axis=-1, keepdims=True)
    m = jnp.where(jnp.isfinite(m), m, 0.0)
    p = jnp.exp(s - m)
    return p / jnp.maximum(jnp.sum(p, axis=-1, keepdims=True), 1e-30)


def _rope(x, pos):
    half = HEAD_DIM // 2
    inv = ROPE_THETA ** (-jnp.arange(half, dtype=jnp.float32) / half)
    ang = pos.astype(jnp.float32)[:, None] * inv[None, :]
    cos = jnp.cos(ang)[None, :, None, :]
    sin = jnp.sin(ang)[None, :, None, :]
    xf = x.astype(jnp.float32)
    x1, x2 = xf[..., :half], xf[..., half:]
    return jnp.concatenate([x1 * cos - x2 * sin, x2 * cos + x1 * sin], axis=-1).astype(x.dtype)


def _selection_map(n_cmp, n_sel):
    r_sel = SEL_BLOCK // CMP_STRIDE
    r_cmp = CMP_BLOCK // CMP_STRIDE
    i = np.arange(n_cmp)[:, None]
    j = np.arange(n_sel)[None, :]
    d = i - r_sel * j
    cnt = np.minimum(d, r_sel - 1) - np.maximum(d - r_cmp + 1, 0) + 1
    return np.clip(cnt, 0, None).astype(np.float32)


def _compress(k, pos_emb, w1, w2):
    B, T = k.shape[0], k.shape[1]
    r = CMP_BLOCK // CMP_STRIDE
    pieces = k.reshape(B, T // CMP_STRIDE, CMP_STRIDE, NSA_KV_GROUPS, HEAD_DIM)
    n_cmp = T // CMP_STRIDE - r + 1
    blocks = jnp.concatenate([pieces[:, j:j + n_cmp] for j in range(r)], axis=2)
    blocks = blocks + pos_emb[None, None, :, None, :]
    flat = blocks.transpose(0, 1, 3, 2, 4).reshape(B, n_cmp, NSA_KV_GROUPS, CMP_BLOCK * HEAD_DIM)
    return jax.nn.gelu(flat @ w1) @ w2


def _nsa(q, k_c, v_c, k_s, v_s, k_w, v_w, gate_logits, cmp_pos_k, cmp_pos_v, w_ck1, w_ck2, w_cv1, w_cv2):
    B, T = q.shape[0], q.shape[1]
    G, R, dh = NSA_KV_GROUPS, NSA_GROUP, HEAD_DIM
    scale = HEAD_DIM ** -0.5
    kc = _compress(k_c, cmp_pos_k, w_ck1, w_ck2)
    vc = _compress(v_c, cmp_pos_v, w_cv1, w_cv2)
    n_cmp = kc.shape[1]
    cmp_end = jnp.arange(n_cmp) * CMP_STRIDE + CMP_BLOCK - 1
    n_sel = T // SEL_BLOCK
    top_n = min(SEL_TOPK, n_sel)
    sel_map = jnp.asarray(_selection_map(n_cmp, n_sel))
    ks_blocks = k_s.reshape(B, n_sel, SEL_BLOCK, G, dh).transpose(0, 3, 1, 2, 4)
    vs_blocks = v_s.reshape(B, n_sel, SEL_BLOCK, G, dh).transpose(0, 3, 1, 2, 4)
    kw_pad = jnp.pad(k_w, ((0, 0), (WINDOW, 0), (0, 0), (0, 0)))
    vw_pad = jnp.pad(v_w, ((0, 0), (WINDOW, 0), (0, 0), (0, 0)))
    n_chunks = T // Q_CHUNK
    q_chunks = q.reshape(B, n_chunks, Q_CHUNK, G, R, dh).transpose(1, 0, 2, 3, 4, 5)
    g_chunks = gate_logits.reshape(B, n_chunks, Q_CHUNK, G, R, 3).transpose(1, 0, 2, 3, 4, 5)
    b_ix = jnp.arange(B)[:, None, None, None]
    g_ix = jnp.arange(G)[None, None, :, None]
    blk_ids = jnp.arange(n_sel)
    in_blk = jnp.arange(SEL_BLOCK)
    win_off = jnp.arange(WINDOW + Q_CHUNK)

    def chunk_fn(args):
        ci, qg, gl = args
        start = ci * Q_CHUNK
        t = start + jnp.arange(Q_CHUNK)
        s_c = jnp.einsum('bqgrd,bngd->bqgrn', qg, kc) * scale
        m_c = (cmp_end[None, :] <= t[:, None])[None, :, None, None, :]
        p_c = _masked_softmax(s_c, m_c)
        o_c = jnp.einsum('bqgrn,bngd->bqgrd', p_c.astype(vc.dtype), vc)
        imp = jnp.einsum('bqgrn,nj->bqgj', p_c, sel_map)
        cur = t // SEL_BLOCK
        forced = (blk_ids[None, :] == 0) | (blk_ids[None, :] == cur[:, None]) | (blk_ids[None, :] == cur[:, None] - 1)
        causal_blk = blk_ids[None, :] * SEL_BLOCK <= t[:, None]
        imp = jnp.where(forced[None, :, None, :], imp + FORCE_BONUS, imp)
        imp = jnp.where(causal_blk[None, :, None, :], imp, -1.0)
        _, idx = lax.top_k(imp, top_n)
        kg = ks_blocks[b_ix, g_ix, idx]
        vg = vs_blocks[b_ix, g_ix, idx]
        s_s = jnp.einsum('bqgrd,bqgnld->bqgrnl', qg, kg) * scale
        kpos = idx[..., None] * SEL_BLOCK + in_blk
        m_s = (kpos <= t[None, :, None, None, None]).reshape(B, Q_CHUNK, G, 1, top_n * SEL_BLOCK)
        p_s = _masked_softmax(s_s.reshape(B, Q_CHUNK, G, R, top_n * SEL_BLOCK), m_s)
        p_s = p_s.reshape(B, Q_CHUNK, G, R, top_n, SEL_BLOCK).astype(vg.dtype)
        o_s = jnp.einsum('bqgrnl,bqgnld->bqgrd', p_s, vg)
        kw_c = lax.dynamic_slice_in_dim(kw_pad, start, WINDOW + Q_CHUNK, axis=1)
        vw_c = lax.dynamic_slice_in_dim(vw_pad, start, WINDOW + Q_CHUNK, axis=1)
        kpos_w = start - WINDOW + win_off
        m_w = (kpos_w[None, :] <= t[:, None]) & (t[:, None] - kpos_w[None, :] < WINDOW) & (kpos_w[None, :] >= 0)
        s_w = jnp.einsum('bqgrd,bkgd->bqgrk', qg, kw_c) * scale
        p_w = _masked_softmax(s_w, m_w[None, :, None, None, :])
        o_w = jnp.einsum('bqgrk,bkgd->bqgrd', p_w.astype(vw_c.dtype), vw_c)
        g = jax.nn.sigmoid(gl.astype(jnp.float32)).astype(qg.dtype)
        o = g[..., 0:1] * o_c + g[..., 1:2] * o_s + g[..., 2:3] * o_w
        return o.reshape(B, Q_CHUNK, NSA_WIDTH)

    out = lax.map(chunk_fn, (jnp.arange(n_chunks), q_chunks, g_chunks))
    return out.transpose(1, 0, 2, 3).reshape(B, T, NSA_WIDTH)


def _hgrn2(hq, hf, hi, hg, lb, norm_g):
    B, T = hq.shape[0], hq.shape[1]
    H, dk, dv, C = HGRN_HEADS, HGRN_EXPAND, HGRN_HEAD_V, HGRN_CHUNK
    f = lb + (1.0 - lb) * jax.nn.sigmoid(hf.astype(jnp.float32))
    log_f = jnp.log(f)
    k = 1.0 - f
    q = jax.nn.silu(hq.astype(jnp.float32))
    v = hi.astype(jnp.float32)
    nc = T // C

    def to_chunks(a, d):
        return a.reshape(B, nc, C, H, d).transpose(1, 0, 3, 2, 4)

    causal = jnp.tril(jnp.ones((C, C), dtype=bool))

    def step(S, xs):
        qc, kc, vc, lfc = xs
        b = jnp.cumsum(lfc, axis=2)
        b_end = b[:, :, -1:, :]
        q_dec = qc * jnp.exp(b)
        k_inv = kc * jnp.exp(-b)
        k_end = kc * jnp.exp(b_end - b)
        A = jnp.where(causal, jnp.einsum('bhtd,bhsd->bhts', q_dec, k_inv), 0.0)
        o = jnp.einsum('bhts,bhse->bhte', A, vc) + jnp.einsum('bhtd,bhde->bhte', q_dec, S)
        S = jnp.exp(b_end)[:, :, 0, :, None] * S + jnp.einsum('bhsd,bhse->bhde', k_end, vc)
        return S, o

    S0 = jnp.zeros((B, H, dk, dv), jnp.float32)
    _, o = lax.scan(step, S0, (to_chunks(q, dk), to_chunks(k, dk), to_chunks(v, dv), to_chunks(log_f, dk)))
    o = o.transpose(1, 0, 3, 2, 4).reshape(B, T, H, dv).astype(hq.dtype)
    o = _rmsnorm(o, norm_g) * jax.nn.silu(hg.reshape(B, T, H, dv))
    return o.reshape(B, T, HGRN_V_WIDTH)


def _hybrid_mixer(h, layer_lb, w_in, cmp_pos_k, cmp_pos_v, w_ck1, w_ck2, w_cv1, w_cv2, norm_g, w_branch, w_out):
    B, T = h.shape[0], h.shape[1]
    proj = h @ w_in
    cuts = np.cumsum(IN_SPLITS)[:-1].tolist()
    q, k_c, v_c, k_s, v_s, k_w, v_w, nsa_gl, hq, hf, hi, hg, merge = jnp.split(proj, cuts, axis=-1)
    pos = jnp.arange(T)

    def heads(a, n):
        return a.reshape(B, T, n, HEAD_DIM)

    q = _rope(heads(q, NSA_HEADS), pos)
    k_c = _rope(heads(k_c, NSA_KV_GROUPS), pos)
    k_s = _rope(heads(k_s, NSA_KV_GROUPS), pos)
    k_w = _rope(heads(k_w, NSA_KV_GROUPS), pos)
    o_nsa = _nsa(q, k_c, heads(v_c, NSA_KV_GROUPS), k_s, heads(v_s, NSA_KV_GROUPS), k_w, heads(v_w, NSA_KV_GROUPS), nsa_gl.reshape(B, T, NSA_HEADS, 3), cmp_pos_k, cmp_pos_v, w_ck1, w_ck2, w_cv1, w_cv2)
    o_hgrn = _hgrn2(hq, hf, hi, hg, layer_lb, norm_g)
    g_a, g_b = jnp.split(jax.nn.sigmoid(merge.astype(jnp.float32)).astype(h.dtype), 2, axis=-1)
    y = g_a * (o_nsa @ w_branch[0]) + g_b * (o_hgrn @ w_branch[1])
    return y @ w_out


def _peer(h, w_q, sub_keys, u_tab, v_tab):
    B, T, D = h.shape
    n = B * T
    K = PEER_TOPK
    xt = h.reshape(n, D)
    q = (xt @ w_q).reshape(n, PEER_HEADS, 2, PEER_QDIM // 2)
    s = jnp.einsum('nhpd,phkd->nhpk', q, sub_keys).astype(jnp.float32)
    s1, i1 = lax.top_k(s[:, :, 0], K)
    s2, i2 = lax.top_k(s[:, :, 1], K)
    comb = (s1[..., :, None] + s2[..., None, :]).reshape(n, PEER_HEADS, K * K)
    cand = (i1[..., :, None] * PEER_NKEYS + i2[..., None, :]).reshape(n, PEER_HEADS, K * K)
    top_s, pos = lax.top_k(comb, K)
    experts = jnp.take_along_axis(cand, pos, axis=-1)
    gate = jax.nn.softmax(top_s, axis=-1).astype(h.dtype)
    n_chunks = n // PEER_TOK_CHUNK

    def chunk_fn(args):
        xc, ec, gc = args
        a = jax.nn.gelu(jnp.einsum('nd,nhkd->nhk', xc, u_tab[ec]))
        return jnp.einsum('nhk,nhkd->nd', gc * a, v_tab[ec])

    y = lax.map(chunk_fn, (xt.reshape(n_chunks, PEER_TOK_CHUNK, D), experts.reshape(n_chunks, PEER_TOK_CHUNK, PEER_HEADS, K), gate.reshape(n_chunks, PEER_TOK_CHUNK, PEER_HEADS, K)))
    return y.reshape(B, T, D)


def setup_inputs(seed: int = 0) -> dict:
    key = jax.random.key(seed)
    ks = jax.random.split(key, 22)

    def nrm(k, shape, s):
        return jax.random.normal(k, shape, jnp.float32) * s

    D = D_MODEL
    return {
        'x': nrm(ks[0], (BATCH, SEQ, D), 1.0),
        'c': nrm(ks[1], (BATCH, D), 1.0),
        'w_ada': nrm(ks[2], (DEPTH, D, 6 * D), 0.02),
        'b_ada': nrm(ks[3], (DEPTH, 6 * D), 0.02),
        'g_mix': 1.0 + nrm(ks[4], (DEPTH, D), 0.02),
        'g_ffn': 1.0 + nrm(ks[5], (DEPTH, D), 0.02),
        'w_in': nrm(ks[6], (DEPTH, D, IN_COLS), D ** -0.5),
        'cmp_pos_k': nrm(ks[7], (DEPTH, CMP_BLOCK, HEAD_DIM), 0.02),
        'cmp_pos_v': nrm(ks[8], (DEPTH, CMP_BLOCK, HEAD_DIM), 0.02),
        'w_ck1': nrm(ks[9], (DEPTH, CMP_BLOCK * HEAD_DIM, CMP_HIDDEN), (CMP_BLOCK * HEAD_DIM) ** -0.5),
        'w_ck2': nrm(ks[10], (DEPTH, CMP_HIDDEN, HEAD_DIM), CMP_HIDDEN ** -0.5),
        'w_cv1': nrm(ks[11], (DEPTH, CMP_BLOCK * HEAD_DIM, CMP_HIDDEN), (CMP_BLOCK * HEAD_DIM) ** -0.5),
        'w_cv2': nrm(ks[12], (DEPTH, CMP_HIDDEN, HEAD_DIM), CMP_HIDDEN ** -0.5),
        'hgrn_lb_logits': nrm(ks[13], (DEPTH + 1, HGRN_WIDTH), 0.5),
        'hgrn_out_norm': 1.0 + nrm(ks[14], (DEPTH, HGRN_HEAD_V), 0.02),
        'w_branch': nrm(ks[15], (DEPTH, 2, NSA_WIDTH, D), NSA_WIDTH ** -0.5),
        'w_out': nrm(ks[16], (DEPTH, D, D), D ** -0.5),
        'w_peer_q': nrm(ks[17], (DEPTH, D, PEER_HEADS * PEER_QDIM), D ** -0.5),
        'peer_sub_keys': nrm(ks[18], (DEPTH, 2, PEER_HEADS, PEER_NKEYS, PEER_QDIM // 2), (PEER_QDIM // 2) ** -0.5),
        'peer_u': nrm(ks[19], (DEPTH, PEER_NEXPERTS, D), D ** -0.5),
        'peer_v': nrm(ks[20], (DEPTH, PEER_NEXPERTS, D), 0.5),
        'g_final': 1.0 + nrm(ks[21], (D,), 0.02),
    }


def reference(x, c, w_ada, b_ada, g_mix, g_ffn, w_in, cmp_pos_k, cmp_pos_v, w_ck1, w_ck2, w_cv1, w_cv2, hgrn_lb_logits, hgrn_out_norm, w_branch, w_out, w_peer_q, peer_sub_keys, peer_u, peer_v, g_final):
    lb_all = jnp.cumsum(jax.nn.softmax(hgrn_lb_logits.astype(jnp.float32), axis=0), axis=0)
    cs = jax.nn.silu(c)
    for l in range(DEPTH):
        mod = cs @ w_ada[l] + b_ada[l]
        sh_m, sc_m, gt_m, sh_f, sc_f, gt_f = [m[:, None, :] for m in jnp.split(mod, 6, axis=-1)]
        h = _rmsnorm(x, g_mix[l]) * (1.0 + sc_m) + sh_m
        y = _hybrid_mixer(h, lb_all[l], w_in[l], cmp_pos_k[l], cmp_pos_v[l], w_ck1[l], w_ck2[l], w_cv1[l], w_cv2[l], hgrn_out_norm[l], w_branch[l], w_out[l])
        x = x + gt_m * y
        h = _rmsnorm(x, g_ffn[l]) * (1.0 + sc_f) + sh_f
        x = x + gt_f * _peer(h, w_peer_q[l], peer_sub_keys[l], peer_u[l], peer_v[l])
    return _rmsnorm(x, g_final)
```

```python
from contextlib import ExitStack

import numpy as np
import concourse.bass as bass
import concourse.mybir as mybir
from concourse.bass_utils import run_bass_kernel_spmd

F32 = mybir.dt.float32
BF16 = mybir.dt.bfloat16
AF = mybir.ActivationFunctionType
ALU = mybir.AluOpType
AX = mybir.AxisListType

T = 2048
D = 1024
NT = T // 128
EPS = 1e-6
NCORES = 8
DBG = {}


class Buf:
    __slots__ = ("name", "w", "r")

    def __init__(self, name):
        self.name = name
        self.w = {}
        self.r = {}


class Sched:
    def __init__(self, nc, es):
        self.nc = nc
        self.es = es
        self.eng = {"pe": nc.tensor, "act": nc.scalar, "dve": nc.vector, "pool": nc.gpsimd, "sp": nc.sync}
        self.sem = {}
        self.cnt = {}
        for e in ("pe", "act", "dve", "pool"):
            self.sem["E" + e] = es.enter_context(nc.semaphore("s_" + e))
            self.cnt["E" + e] = 0
        self.wm = {e: {} for e in self.eng}
        self.nwait = 0
        self.nins = 0

    def _waits(self, eng, reads, writes):
        deps = {}
        for b in reads:
            for k, v in b.w.items():
                if deps.get(k, 0) < v:
                    deps[k] = v
        for b in writes:
            for k, v in b.w.items():
                if deps.get(k, 0) < v:
                    deps[k] = v
            for k, v in b.r.items():
                if deps.get(k, 0) < v:
                    deps[k] = v
        wm = self.wm[eng]
        e = self.eng[eng]
        for k, v in deps.items():
            if eng == "pe" and k == "Epe":
                continue
            if wm.get(k, 0) < v:
                e.wait_ge(self.sem[k], v)
                wm[k] = v
                self.nwait += 1

    def op(self, eng, fn, r=(), w=()):
        self._waits(eng, r, w)
        k = "E" + eng
        self.cnt[k] += 1
        v = self.cnt[k]
        fn(self.eng[eng]).then_inc(self.sem[k], 1)
        self.nins += 1
        for b in r:
            b.r[k] = v
        for b in w:
            b.w[k] = v

    def dma(self, q, out, in_, r=(), w=(), key="d"):
        k = "D" + key
        if k not in self.sem:
            self.sem[k] = self.es.enter_context(self.nc.semaphore("d_" + key))
            self.cnt[k] = 0
        self._waits(q, r, w)
        if self.cnt[k] > 0 and self.wm[q].get(k, 0) < self.cnt[k]:
            self.eng[q].wait_ge(self.sem[k], self.cnt[k])
            self.wm[q][k] = self.cnt[k]
        self.cnt[k] += 16
        v = self.cnt[k]
        self.eng[q].dma_start(out=out, in_=in_).then_inc(self.sem[k], 16)
        self.nins += 1
        for b in r:
            b.r[k] = v
        for b in w:
            b.w[k] = v

    def barrier(self, engines=("pe", "act", "dve", "pool", "sp")):
        for eng in engines:
            wm = self.wm[eng]
            e = self.eng[eng]
            for k, v in self.cnt.items():
                if v > 0 and wm.get(k, 0) < v and not (eng == "pe" and k == "Epe"):
                    e.wait_ge(self.sem[k], v)
                    wm[k] = v


class _Stop(Exception):
    pass


def build_nc(nb=2, upto="all", dbg=()):
    nc = bass.Bass("TRN2", target_bir_lowering=False)
    NTOK = nb * T

    def din(name, shape, dt=F32):
        return nc.dram_tensor(name, list(shape), dt, kind="ExternalInput").ap()

    x_d = din("x", [nb, T, D])
    cT_d = din("cT", [128, nb, 8])
    wada_d = din("wada", [12, 128, 8, 512])
    bada_d = din("bada", [1, 6 * D])
    gmix_d = din("gmix", [1, D])
    gffn_d = din("gffn", [1, D])
    gfin_d = din("gfin", [1, D])
    wfm_d = din("wfm", [22, 128, 8, 256])
    wtk_d = din("wtk", [4, 128, 8, 256])
    cst_d = din("cst", [128, 640])
    rope_d = din("rope", [128, 2, T])
    scanm_d = din("scanm", [128, 512])
    mcmp_d = din("mcmp", [128, T])
    f1c_d = din("f1c", [128, 2, NT, 32])
    eall_d = din("eall", [128, T])
    selmap_d = din("selmap", [128, 32])
    w1_d = din("w1", [2, 128, 32, 128])
    pos_d = din("pos", [2, 128, 32])
    w2k_d = din("w2k", [128, 256])
    w2v_d = din("w2v", [128, 64])
    lbl_d = din("lbl", [128, 4, 2])
    ng_d = din("ng", [128, 1])
    wbr_d = din("wbr", [2, 128, 4, D])
    wout_d = din("wout", [128, 8, D])
    wpq_d = din("wpq", [4, 128, 8, 512])
    skT_d = din("skT", [128, 16, 128])
    uT_d = din("uT", [32, 128, 8, 512])
    pv_d = din("pv", [32, 128, 4, D])
    out_d = nc.dram_tensor("out", [nb, T, D], F32, kind="ExternalOutput").ap()
    x1s_d = nc.dram_tensor("x1s", [nb, T, D], F32, kind="Internal").ap()
    uTb_d = nc.dram_tensor("uTb", [32, 128, 8, 512], BF16, kind="Internal").ap()
    pvb_d = nc.dram_tensor("pvb", [32, 128, 4, D], BF16, kind="Internal").ap()
    wpqb_d = nc.dram_tensor("wpqb", [4, 128, 8, 512], BF16, kind="Internal").ap()
    dbg_d = {}

    PH = ["conv", "ada1", "hT", "c1", "cmp", "attn", "c2", "hgrn", "merge", "peer", "all"]
    lim = PH.index(upto)

    def active(p):
        return PH.index(p) <= lim

    with ExitStack() as es:
        S = Sched(nc, es)

        uniq = [0]

        def sb(name, shape, dt=F32, st=es):
            uniq[0] += 1
            return st.enter_context(nc.sbuf_tensor("sb%d_%s" % (uniq[0], name), list(shape), dt))

        banks = [es.enter_context(nc.psum_tensor("bank%d" % i, [128, 512], F32)) for i in range(8)]
        bbuf = [Buf("bank%d" % i) for i in range(8)]

        class Ring:
            def __init__(self, idx):
                self.idx = list(idx)
                self.p = 0

            def next(self):
                i = self.idx[self.p % len(self.idx)]
                self.p += 1
                return banks[i], bbuf[i]

        def dump(name, ap, bufs, shape, dt=F32):
            if name not in dbg:
                return
            d = nc.dram_tensor("dbg_" + name, list(shape), dt, kind="ExternalOutput").ap()
            dbg_d[name] = d
            S.dma("sp", d, ap, r=bufs, key="dbg")

        es.enter_context(nc.Block())

        cst = sb("cst", [128, 640])
        cstb = sb("cstb", [128, 640], BF16)
        B_cst = Buf("cst")
        S.dma("sp", cst[:], cst_d[:, :], w=[B_cst], key="cst")
        S.op("act", lambda e: e.activation(out=cstb[:], in_=cst[:], func=AF.Copy), r=[B_cst], w=[B_cst])
        identb = cstb[:, 0:128]
        Mcb = cstb[:, 128:256]
        Mwb = cstb[:, 256:384]
        blkf = cst[:, 384:512]
        onesf = cst[:, 512:640]

        if active("conv") and lim >= PH.index("peer"):
            with ExitStack() as st:
                NCV = 4
                stg = [sb("cv_s%d" % i, [128, 4096], F32, st) for i in range(NCV)]
                stb = [sb("cv_b%d" % i, [128, 4096], BF16, st) for i in range(NCV)]
                Bs = [Buf("cvs%d" % i) for i in range(NCV)]
                Bb = [Buf("cvb%d" % i) for i in range(NCV)]
                jobs = []
                for g in range(32):
                    jobs.append((uT_d[g].rearrange("p k e -> p (k e)"), uTb_d[g].rearrange("p k e -> p (k e)")))
                    jobs.append((pv_d[g].rearrange("p j d -> p (j d)"), pvb_d[g].rearrange("p j d -> p (j d)")))
                for g in range(4):
                    jobs.append((wpq_d[g].rearrange("p k e -> p (k e)"), wpqb_d[g].rearrange("p k e -> p (k e)")))
                for n, (src, dst) in enumerate(jobs):
                    i = n % NCV
                    S.dma("sp", stg[i][:], src, w=[Bs[i]], key="cvl%d" % i)
                    ce = ("dve", "act", "pool")[n % 3]
                    if ce == "act":
                        S.op("act", lambda e, i=i: e.activation(out=stb[i][:], in_=stg[i][:], func=AF.Copy), r=[Bs[i]], w=[Bb[i]])
                    else:
                        S.op(ce, lambda e, i=i: e.tensor_copy(out=stb[i][:], in_=stg[i][:]), r=[Bs[i]], w=[Bb[i]])
                    S.dma("pool", dst, stb[i][:], r=[Bb[i]], key="cvs%d" % i)
                S.barrier()
        B_scr = Buf("scratch_tables")

        for b in range(nb):
            try:
                with ExitStack() as sq:
                    _sequence(nc, S, sq, b, locals())
            except _Stop:
                pass
        S.barrier()
    return nc, dbg_d


def _sequence(nc, S, sq, b, G):
    sb = G["sb"]
    banks, bbuf, Ring, dump, active = G["banks"], G["bbuf"], G["Ring"], G["dump"], G["active"]
    cst, cstb, identb, Mcb, Mwb, blkf, onesf = (G[k] for k in ("cst", "cstb", "identb", "Mcb", "Mwb", "blkf", "onesf"))
    B_cst = G["B_cst"]
    x_d, out_d, x1s_d = G["x_d"], G["out_d"], G["x1s_d"]
    ring = Ring(range(8))

    def T_(name, shape, dt=F32, st=sq):
        return sb("b%d_%s" % (b, name), shape, dt, st)

    def bcast_row(dst, src_row, bufs, key):
        S.dma("sp", dst, src_row.partition_broadcast(128), w=bufs, key=key)

    def ada_round(pieces, outs, tag):
        with ExitStack() as st:
            res = _ada_round(pieces, st, tag, outs)
            S.barrier()
        return res

    def _ada_round(pieces, st, tag, outs):
        csr = T_("csr" + tag, [128, 8, 128], BF16, st)
        cs = T_("cs" + tag, [128, 8], F32, st)
        B_cs = Buf("cs")
        S.dma("sp", cs[:], G["cT_d"][:, b, :], w=[B_cs], key="small")
        S.op("act", lambda e: e.activation(out=cs[:], in_=cs[:], func=AF.Silu), r=[B_cs], w=[B_cs])
        S.op("dve", lambda e: e.tensor_copy(out=csr[:], in_=cs[:].unsqueeze(2).to_broadcast([128, 8, 128])), r=[B_cs], w=[B_cs])
        res = {}
        wst = [T_("adaw%s%d" % (tag, i), [128, 8, 512], F32, st) for i in range(2)]
        wbf = [T_("adab%s%d" % (tag, i), [128, 8, 512], BF16, st) for i in range(2)]
        Bw = [Buf("adaw%d" % i) for i in range(2)]
        Bwb = [Buf("adawb%d" % i) for i in range(2)]
        for n, j in enumerate(pieces):
            i = n % 2
            S.dma("sp", wst[i][:], G["wada_d"][j], w=[Bw[i]], key="adaw%d" % i)
            S.op("pool", lambda e, i=i: e.tensor_copy(out=wbf[i][:], in_=wst[i][:]), r=[Bw[i]], w=[Bwb[i]])
            bk, Bk = ring.next()
            for k in range(8):
                S.op("pe", lambda e, k=k, i=i, bk=bk: e.matmul(bk[:], lhsT=csr[:, k, :], rhs=wbf[i][:, k, :], start=(k == 0), stop=(k == 7)),
                     r=[B_cs, Bwb[i]], w=[Bk])
            o = outs[j]
            Bo = Buf("mod%d" % j)
            bcast_row(o[:], G["bada_d"][0:1, j * 512:(j + 1) * 512], [Bo], "small")
            S.op("dve", lambda e, bk=bk, o=o: e.tensor_tensor(out=o[:], in0=bk[:], in1=o[:], op=ALU.add), r=[Bk, Bo], w=[Bo])
            res[j] = (o, Bo)
        return res

    def make_scale(res, j0, g_d, st, tag):
        with ExitStack() as tmp:
            gb = T_("g" + tag, [128, D], F32, tmp)
            Bg = Buf("g" + tag)
            bcast_row(gb[:], g_d[0:1, :], [Bg], "small")
            for h in range(2):
                o, Bo = res[j0 + h]
                S.op("dve", lambda e, o=o, h=h: e.scalar_tensor_tensor(out=o[:], in0=o[:], scalar=1.0, op0=ALU.add,
                                                                       in1=gb[:, h * 512:(h + 1) * 512], op1=ALU.mult),
                     r=[Bo, Bg], w=[Bo])
            S.barrier()

    def norm_to_T(xt, Bx, Asc, Ash, hT, B_hT, i, st_bufs):
        ss, rstd, h1, hb, Bs_, Bh1, Bhb = st_bufs
        S.op("act", lambda e: e.activation(out=h1[:], in_=xt[:], func=AF.Square, accum_out=ss[:]), r=[Bx], w=[Bh1, Bs_])
        S.op("dve", lambda e: e.tensor_scalar(out=rstd[:], in0=ss[:], scalar1=1.0 / D, scalar2=EPS, op0=ALU.mult, op1=ALU.add), r=[Bs_], w=[Bs_])
        S.op("act", lambda e: e.activation(out=rstd[:], in_=rstd[:], func=AF.Sqrt), r=[Bs_], w=[Bs_])
        S.op("dve", lambda e: e.reciprocal(out=rstd[:], in_=rstd[:]), r=[Bs_], w=[Bs_])
        for h in range(2):
            sl = slice(h * 512, (h + 1) * 512)
            S.op("dve", lambda e, sl=sl, h=h: e.scalar_tensor_tensor(out=h1[:, sl], in0=xt[:, sl], scalar=rstd[:, 0:1], op0=ALU.mult,
                                                                      in1=Asc[h][0][:], op1=ALU.mult),
                 r=[Bx, Bs_, Asc[h][1]], w=[Bh1])
            S.op("pool", lambda e, sl=sl, h=h: e.tensor_tensor(out=hb[:, sl], in0=h1[:, sl], in1=Ash[h][0][:], op=ALU.add),
                 r=[Bh1, Ash[h][1]], w=[Bhb])
        bk, Bk = ring.next()
        bkb = bk[:].bitcast(BF16).rearrange("p (k t) -> p k t", k=8)
        for k in range(8):
            S.op("pe", lambda e, k=k: e.transpose(bkb[:, k, :], hb[:, k * 128:(k + 1) * 128], identb), r=[Bhb, B_cst], w=[Bk])
        S.op("act", lambda e: e.activation(out=hT[:, :, i * 128:(i + 1) * 128], in_=bkb, func=AF.Copy), r=[Bk], w=[B_hT])

    mouts = {j: T_("mod%d" % j, [128, 512], F32) for j in (10, 11)}
    hT = T_("hT", [128, 8, T], BF16)
    B_hT = Buf("hT")
    smx = ExitStack()
    sq.enter_context(smx)
    o_nsaT = T_("o_nsaT", [128, 4, T], BF16, smx)
    B_onT = Buf("o_nsaT")
    o_hgT = T_("o_hgT", [128, 4, T], BF16, smx)
    B_ohT = Buf("o_hgT")
    for j in range(4, 10):
        mouts[j] = T_("mod%d" % j, [128, 512], F32, smx)

    with ExitStack() as st:
        for j in range(4):
            mouts[j] = T_("mod%d" % j, [128, 512], F32, st)
        res = ada_round(list(range(12)), mouts, "m")
        make_scale(res, 2, G["gmix_d"], st, "m")
        make_scale(res, 8, G["gffn_d"], st, "f")
        Ash = [res[0], res[1]]
        Asc = [res[2], res[3]]
        if not active("hT"):
            return None
        xr = [T_("xr%d" % i, [128, D], F32, st) for i in range(2)]
        Bxr = [Buf("xr%d" % i) for i in range(2)]
        nb_ = (T_("n_ss", [128, 1], F32, st), T_("n_rstd", [128, 1], F32, st), T_("n_h1", [128, D], F32, st), T_("n_hb", [128, D], BF16, st),
               Buf("n_ss"), Buf("n_h1"), Buf("n_hb"))
        for i in range(NT):
            j = i % 2
            S.dma("sp", xr[j][:], x_d[b, i * 128:(i + 1) * 128, :], w=[Bxr[j]], key="xr%d" % j)
            norm_to_T(xr[j], Bxr[j], Asc, Ash, hT, B_hT, i, nb_)
        S.barrier()
    dump("hT", hT[:], [B_hT], [128, 8, T], BF16)
    if not active("c1"):
        return None

    def fm_units(units, st, epilogue, nbuf=2):
        wst = [T_("wst%d" % i, [128, 8, 256], F32, st) for i in range(nbuf)]
        wbf = [T_("wbf%d" % i, [128, 8, 256], BF16, st) for i in range(nbuf)]
        Bw = [Buf("wst%d" % i) for i in range(nbuf)]
        Bwb = [Buf("wbf%d" % i) for i in range(nbuf)]
        for n, (u, ng_) in enumerate(units):
            i = n % nbuf
            S.dma("sp", wst[i][:], G["wfm_d"][u], w=[Bw[i]], key="wst%d" % i)
            S.op("pool", lambda e, i=i: e.tensor_copy(out=wbf[i][:], in_=wst[i][:]), r=[Bw[i]], w=[Bwb[i]])
            for tc in range(4):
                bl = []
                for g in range(ng_):
                    bk, Bk = ring.next()
                    for k in range(8):
                        S.op("pe", lambda e, k=k, g=g, bk=bk, i=i, tc=tc: e.matmul(bk[:], lhsT=wbf[i][:, k, g * 128:(g + 1) * 128],
                                                                               rhs=hT[:, k, tc * 512:(tc + 1) * 512], start=(k == 0), stop=(k == 7)),
                             r=[Bwb[i], B_hT], w=[Bk])
                    bl.append((bk, Bk))
                epilogue(u, tc, bl)

    def tk_units(units, st, epilogue):
        wst = [T_("twst%d" % u, [128, 8, 256], F32, st) for u in units]
        wbf = [T_("twbf%d" % u, [128, 8, 256], BF16, st) for u in units]
        Bwb = [Buf("twbf%d" % u) for u in units]
        for n, u in enumerate(units):
            S.dma("sp", wst[n][:], G["wtk_d"][u], w=[Bwb[n]], key="twst")
            S.op("pool", lambda e, n=n: e.tensor_copy(out=wbf[n][:], in_=wst[n][:]), r=[Bwb[n]], w=[Bwb[n]])
        for i in range(NT):
            for n, u in enumerate(units):
                bk, Bk = ring.next()
                ncol = 24 if u == 1 else 256
                for k in range(8):
                    S.op("pe", lambda e, k=k, bk=bk, n=n, i=i, ncol=ncol: e.matmul(bk[:, 0:ncol], lhsT=hT[:, k, i * 128:(i + 1) * 128],
                                                                                 rhs=wbf[n][:, k, 0:ncol], start=(k == 0), stop=(k == 7)),
                         r=[Bwb[n], B_hT], w=[Bk])
                epilogue(u, i, bk, Bk)

    with ExitStack() as sn:
        qT = T_("qT", [128, 4, T], BF16, sn)
        ksT = T_("ksT", [128, T], BF16, sn)
        kwT = T_("kwT", [128, T], BF16, sn)
        kcin = T_("kcin", [128, T], BF16, sn)
        vcin = T_("vcin", [128, T], BF16, sn)
        vsw = T_("vsw", [128, NT, 2, 2, 65], BF16, sn)
        gates = T_("gates", [128, NT, 24], F32, sn)
        B_q, B_ks, B_kw, B_kc, B_vc, B_vsw, B_gt = (Buf(n) for n in ("qT", "ksT", "kwT", "kcin", "vcin", "vsw", "gates"))
        with ExitStack() as st:
            rope = T_("rope", [128, 2, T], F32, st)
            B_rope = Buf("rope")
            S.dma("sp", rope[:], G["rope_d"][:, :, :], w=[B_rope], key="rope")
            t1 = [T_("rt1_%d" % i, [128, 512], F32, st) for i in range(2)]
            t2 = [T_("rt2_%d" % i, [128, 512], F32, st) for i in range(2)]
            Bt1 = [Buf("rt1") for i in range(2)]
            Bt2 = [Buf("rt2") for i in range(2)]
            cnt = [0]

            def ep_c1(u, tc, bl):
                sl = slice(tc * 512, (tc + 1) * 512)
                if u == 7:
                    S.op("act", lambda e: e.activation(out=vcin[:, sl], in_=bl[0][0][:], func=AF.Copy), r=[bl[0][1]], w=[B_vc])
                    return
                dst, Bd = {0: (qT[:, 0, sl], B_q), 1: (qT[:, 1, sl], B_q), 2: (qT[:, 2, sl], B_q), 3: (qT[:, 3, sl], B_q),
                           4: (ksT[:, sl], B_ks), 5: (kwT[:, sl], B_kw), 6: (kcin[:, sl], B_kc)}[u]
                i = cnt[0] % 2
                cnt[0] += 1
                S.op("dve", lambda e: e.tensor_tensor(out=t1[i][:], in0=bl[0][0][:], in1=rope[:, 0, sl], op=ALU.mult), r=[bl[0][1], B_rope], w=[Bt1[i]])
                S.op("dve", lambda e: e.tensor_tensor(out=t2[i][:], in0=bl[1][0][:], in1=rope[:, 1, sl], op=ALU.mult), r=[bl[1][1], B_rope], w=[Bt2[i]])
                S.op("pool", lambda e: e.tensor_tensor(out=dst, in0=t1[i][:], in1=t2[i][:], op=ALU.add), r=[Bt1[i], Bt2[i]], w=[Bd])

            fm_units([(u, 2) for u in range(7)] + [(7, 1)], st, ep_c1)

            S.op("pool", lambda e: e.memset(vsw[:, :, :, :, 64:65], 1.0), w=[B_vsw])

            def ep_t1(u, i, bk, Bk):
                if u == 0:
                    S.op("act", lambda e: e.activation(out=vsw[:, i, :, :, 0:64], in_=bk[:, 0:256].rearrange("p (a g d) -> p a g d", a=2, g=2),
                                                       func=AF.Copy), r=[Bk], w=[B_vsw])
                else:
                    S.op("act", lambda e: e.activation(out=gates[:, i, :], in_=bk[:, 0:24], func=AF.Sigmoid), r=[Bk], w=[B_gt])

            S.barrier()
        with ExitStack() as st:
            tk_units([0, 1], st, ep_t1)
            S.barrier()
        dump("qT", qT[:], [B_q], [128, 4, T], BF16)
        dump("ksT", ksT[:], [B_ks], [128, T], BF16)
        dump("vsw", vsw[:], [B_vsw], [128, NT, 2, 2, 65], BF16)
        dump("gates", gates[:], [B_gt], [128, NT, 24])
        if not active("cmp"):
            return None

        kcc = T_("kcc", [128, 128], BF16, sn)
        rhsC = T_("rhsC", [128, 2, 97], BF16, sn)
        B_kcc, B_rc = Buf("kcc"), Buf("rhsC")
        with ExitStack() as st:
            w1s = T_("w1s", [128, 32, 128], F32, st)
            w1b = [T_("w1b%d" % i, [128, 32, 128], BF16, st) for i in range(2)]
            posf = T_("posf", [128, 2, 32], F32, st)
            posb = T_("posb", [128, 2, 32], BF16, st)
            w2f = T_("w2f", [128, 320], F32, st)
            w2b = T_("w2b", [128, 320], BF16, st)
            smf = T_("smf", [128, 32], F32, st)
            hid = [T_("hid%d" % i, [128, 128], BF16, st) for i in range(4)]
            bias = T_("cbias", [128, 2], F32, st)
            B_w1s, B_w1b, B_pos, B_w2, B_sm, B_hid, B_bias = (Buf(n) for n in ("w1s", "w1b", "pos", "w2", "sm", "hid", "bias"))
            S.dma("sp", w2f[:, 0:256], G["w2k_d"][:, :], w=[B_w2], key="cmpw")
            S.dma("sp", w2f[:, 256:320], G["w2v_d"][:, :], w=[B_w2], key="cmpw")
            S.dma("sp", smf[:], G["selmap_d"][:, :], w=[B_sm], key="cmpw")
            for kv in range(2):
                S.dma("sp", posf[:, kv, :], G["pos_d"][kv], w=[B_pos], key="cmpw")
            S.op("act", lambda e: e.activation(out=w2b[:], in_=w2f[:], func=AF.Copy), r=[B_w2], w=[B_w2])
            S.op("act", lambda e: e.activation(out=posb[:], in_=posf[:], func=AF.Copy), r=[B_pos], w=[B_pos])
            S.op("pool", lambda e: e.memset(kcc[:], 0.0), w=[B_kcc])
            S.op("pool", lambda e: e.memset(rhsC[:], 0.0), w=[B_rc])
            S.op("pool", lambda e: e.memset(rhsC[:, :, 64:65], 1.0), w=[B_rc])
            for g in range(2):
                S.op("act", lambda e, g=g: e.activation(out=rhsC[:, g, 65:97], in_=smf[:], func=AF.Copy), r=[B_sm], w=[B_rc])
            for kv in range(2):
                S.dma("sp", w1s[:], G["w1_d"][kv], w=[B_w1s], key="w1s")
                S.op("pool", lambda e, kv=kv: e.tensor_copy(out=w1b[kv][:], in_=w1s[:]), r=[B_w1s], w=[B_w1b])
            for kv in range(2):
                src, Bsrc = (kcin, B_kc) if kv == 0 else (vcin, B_vc)
                bkb_, Bkb_ = ring.next()
                for j in range(32):
                    S.op("pe", lambda e, j=j, kv=kv, bkb_=bkb_: e.matmul(bkb_[:, 0:1], lhsT=w1b[kv][0:64, j, :], rhs=posb[0:64, kv, j:j + 1],
                                                                       start=(j == 0), stop=(j == 31)), r=[B_w1b, B_pos], w=[Bkb_])
                S.op("act", lambda e, kv=kv, bkb_=bkb_: e.activation(out=bias[:, kv:kv + 1], in_=bkb_[:, 0:1], func=AF.Copy), r=[Bkb_], w=[B_bias])
                for g in range(2):
                    bk, Bk = ring.next()
                    ps = slice(g * 64, (g + 1) * 64)
                    for j in range(32):
                        S.op("pe", lambda e, j=j, kv=kv, bk=bk, ps=ps, src=src: e.matmul(bk[:, 0:127], lhsT=w1b[kv][ps, j, :],
                                                                                       rhs=src[ps, j:j + 16 * 126 + 1:16], start=(j == 0), stop=(j == 31)),
                             r=[B_w1b, Bsrc], w=[Bk])
                    hh = hid[kv * 2 + g]
                    S.op("act", lambda e, hh=hh, bk=bk, kv=kv: e.activation(out=hh[:, 0:127], in_=bk[:, 0:127], func=AF.Gelu_apprx_tanh,
                                                                          bias=bias[:, kv:kv + 1]), r=[Bk, B_bias], w=[B_hid])
            bk, Bk = ring.next()
            S.op("pe", lambda e: e.matmul(bk[:, 0:127], lhsT=w2b[:, 0:128], rhs=hid[0][:, 0:127], start=True, stop=False), r=[B_w2, B_hid], w=[Bk])
            S.op("pe", lambda e: e.matmul(bk[:, 0:127], lhsT=w2b[:, 128:256], rhs=hid[1][:, 0:127], start=False, stop=True), r=[B_w2, B_hid], w=[Bk])
            S.op("act", lambda e: e.activation(out=kcc[:, 0:127], in_=bk[:, 0:127], func=AF.Copy), r=[Bk], w=[B_kcc])
            for g in range(2):
                bk2, Bk2 = ring.next()
                S.op("pe", lambda e, g=g, bk2=bk2: e.matmul(bk2[0:127, 0:64], lhsT=hid[2 + g][:, 0:127], rhs=w2b[:, 256:320], start=True, stop=True),
                     r=[B_w2, B_hid], w=[Bk2])
                S.op("act", lambda e, g=g, bk2=bk2: e.activation(out=rhsC[0:127, g, 0:64], in_=bk2[0:127, 0:64], func=AF.Copy), r=[Bk2], w=[B_rc])
            S.barrier()
        dump("kcc", kcc[:], [B_kcc], [128, 128], BF16)
        dump("rhsC", rhsC[:], [B_rc], [128, 2, 97], BF16)
        if not active("attn"):
            return None

        with ExitStack() as st:
            mcmp_f = T_("mcmp_f", [128, T], F32, st)
            mcmp = T_("mcmp", [128, T], BF16, st)
            eall_f = T_("eall_f", [128, T], F32, st)
            eall = T_("eall", [128, T], BF16, st)
            f1c = T_("f1c", [128, 2, NT, 32], F32, st)
            B_tab = Buf("attn_tables")
            S.dma("sp", mcmp_f[:], G["mcmp_d"][:, :], w=[B_tab], key="atab")
            S.dma("sp", eall_f[:], G["eall_d"][:, :], w=[B_tab], key="atab")
            S.dma("sp", f1c[:], G["f1c_d"][:, :, :, :], w=[B_tab], key="atab")
            S.op("act", lambda e: e.activation(out=mcmp[:], in_=mcmp_f[:], func=AF.Copy), r=[B_tab], w=[B_tab])
            S.op("act", lambda e: e.activation(out=eall[:], in_=eall_f[:], func=AF.Copy), r=[B_tab], w=[B_tab])
            NP = 3
            ringO = Ring([0, 1, 2])
            ringS = Ring([3, 4, 5, 6, 7])
            Pt = [T_("Pt%d" % i, [128, 4, 128], BF16, st) for i in range(NP)]
            BP = [Buf("Pt%d" % i) for i in range(NP)]
            pc = [0]
            oacc = [T_("oacc%d" % i, [128, 512], F32, st) for i in range(2)]
            Boa = [Buf("oacc%d" % i) for i in range(2)]
            oab = T_("oab", [128, 512], BF16, st)
            Boab = Buf("oab")
            sm = T_("att_small", [128, 64], F32, st)
            Bsm = Buf("att_small")
            imp = T_("imp", [128, 32], F32, st)
            scr = T_("imp_scr", [128, 32], F32, st)
            m8 = T_("imp_m8", [128, 16], F32, st)
            selb = T_("selb", [128, 32], BF16, st)
            selT = T_("selT", [128, 128], BF16, st)
            tmpo = T_("tmpo", [128, 4, 64], F32, st)
            Bimp, Bsel, BselT, Btmpo = Buf("imp"), Buf("selb"), Buf("selT"), Buf("tmpo")

            def combine(bo, Bo, i, g, br, first, oa, Boa_):
                o4 = bo[:].rearrange("p (r c) -> p r c", r=4)
                S.op("dve", lambda e: e.tensor_scalar(out=sm[:, 0:4], in0=o4[:, :, 64], scalar1=1e-30, scalar2=None, op0=ALU.max), r=[Bo], w=[Bsm])
                S.op("dve", lambda e: e.reciprocal(out=sm[:, 4:8], in_=sm[:, 0:4]), r=[Bsm], w=[Bsm])
                S.op("dve", lambda e: e.tensor_tensor(out=sm[:, 8:12], in0=sm[:, 4:8], in1=gates[:, i, g * 12 + br:g * 12 + 12:3], op=ALU.mult),
                     r=[Bsm, B_gt], w=[Bsm])
                cb = sm[:, 8:12].unsqueeze(2).to_broadcast([128, 4, 64])
                dst = oa[:, g * 256:(g + 1) * 256].rearrange("p (r c) -> p r c", r=4)
                if first:
                    S.op("dve", lambda e: e.tensor_tensor(out=dst, in0=o4[:, :, 0:64], in1=cb, op=ALU.mult), r=[Bo, Bsm], w=[Boa_])
                else:
                    S.op("dve", lambda e: e.tensor_tensor(out=tmpo[:], in0=o4[:, :, 0:64], in1=cb, op=ALU.mult), r=[Bo, Bsm], w=[Btmpo])
                    S.op("pool", lambda e: e.tensor_tensor(out=dst, in0=dst, in1=tmpo[:], op=ALU.add), r=[Btmpo, Boa_], w=[Boa_])

            def scores(kT, Bk_, ps, c, i, ncols=128):
                bk, Bk = ringS.next()
                S.op("pe", lambda e: e.matmul(bk[0:ncols, :], lhsT=kT[ps, c * 128:c * 128 + ncols], rhs=qT[ps, :, i * 128:(i + 1) * 128],
                                              start=True, stop=True), r=[Bk_, B_q], w=[Bk])
                j = pc[0] % NP
                pc[0] += 1
                S.op("act", lambda e: e.activation(out=Pt[j][0:ncols].rearrange("p r t -> p (r t)"), in_=bk[0:ncols, :], func=AF.Exp, scale=0.125),
                     r=[Bk], w=[BP[j]])
                return Pt[j], BP[j]

            for i in range(NT):
                oa, Boa_ = oacc[i % 2], Boa[i % 2]
                tsl = slice(i * 128, (i + 1) * 128)
                for g in range(2):
                    ps = slice(g * 64, (g + 1) * 64)
                    P, BPj = scores(kcc, B_kcc, ps, 0, i, ncols=127)
                    S.op("dve", lambda e, P=P: e.tensor_tensor(out=P[0:127], in0=P[0:127], in1=mcmp[0:127, tsl].unsqueeze(1).to_broadcast([127, 4, 128]),
                                                          op=ALU.mult), r=[BPj, B_tab], w=[BPj])
                    bo, Bo = ringO.next()
                    o4 = bo[:].rearrange("p (r c) -> p r c", r=4)
                    for r_ in range(4):
                        S.op("pe", lambda e, r_=r_, P=P, o4=o4: e.matmul(o4[:, r_, 0:97], lhsT=P[0:127, r_, :], rhs=rhsC[0:127, g, :], start=True, stop=True),
                             r=[BPj, B_rc], w=[Bo])
                    combine(bo, Bo, i, g, 0, True, oa, Boa_)
                    if i >= 8:
                        S.op("dve", lambda e, o4=o4: e.tensor_scalar(out=imp[:], in0=o4[:, 0, 65:97], scalar1=sm[:, 4:5], scalar2=None, op0=ALU.mult),
                             r=[Bo, Bsm], w=[Bimp])
                        for r_ in range(1, 4):
                            S.op("dve", lambda e, r_=r_, o4=o4: e.scalar_tensor_tensor(out=imp[:], in0=o4[:, r_, 65:97], scalar=sm[:, 4 + r_:5 + r_], op0=ALU.mult,
                                                                                 in1=imp[:], op1=ALU.add), r=[Bo, Bsm, Bimp], w=[Bimp])
                        S.op("dve", lambda e: e.tensor_tensor(out=imp[:], in0=imp[:], in1=f1c[:, 0, i, :], op=ALU.add), r=[Bimp, B_tab], w=[Bimp])
                        S.op("dve", lambda e: e.tensor_tensor(out=imp[:], in0=imp[:], in1=f1c[:, 1, i, :], op=ALU.mult), r=[Bimp, B_tab], w=[Bimp])
                        S.op("dve", lambda e: e.tensor_scalar(out=imp[:], in0=imp[:], scalar1=-1.0, scalar2=None, op0=ALU.add), r=[Bimp], w=[Bimp])
                        S.op("dve", lambda e: e.max(out=m8[:, 0:8], in_=imp[:]), r=[Bimp], w=[Bimp])
                        S.op("dve", lambda e: e.match_replace(out=scr[:], in_to_replace=m8[:, 0:8], in_values=imp[:], imm_value=-2.0), r=[Bimp], w=[Bimp])
                        S.op("dve", lambda e: e.max(out=m8[:, 8:16], in_=scr[:]), r=[Bimp], w=[Bimp])
                        S.op("dve", lambda e: e.tensor_scalar(out=selb[:], in0=imp[:], scalar1=m8[:, 15:16], scalar2=None, op0=ALU.is_ge), r=[Bimp], w=[Bsel])
                        bt, Bt = ringS.next()
                        btb = bt[:].bitcast(BF16)
                        S.op("pe", lambda e, btb=btb: e.transpose(btb[0:32, 0:128], selb[:], identb), r=[Bsel, B_cst], w=[Bt])
                        S.op("act", lambda e, btb=btb: e.activation(out=selT[0:32, :], in_=btb[0:32, 0:128], func=AF.Copy), r=[Bt], w=[BselT])
                    for br, kT, Bk_, sw, clo in ((1, ksT, B_ks, 0, 0), (2, kwT, B_kw, 1, max(0, i - 4))):
                        bo, Bo = ringO.next()
                        o4 = bo[:].rearrange("p (r c) -> p r c", r=4)
                        for c in range(clo, i + 1):
                            P, BPj = scores(kT, Bk_, ps, c, i)
                            if br == 1 and i >= 8:
                                bm, Bm = ringS.next()
                                S.op("pe", lambda e, bm=bm, c=c: e.matmul(bm[:, 0:128], lhsT=eall[0:32, c * 128:(c + 1) * 128], rhs=selT[0:32, :], start=True, stop=True),
                                     r=[B_tab, BselT], w=[Bm])
                                S.op("dve", lambda e, P=P, bm=bm: e.tensor_tensor(out=P[:], in0=P[:], in1=bm[:, 0:128].unsqueeze(1).to_broadcast([128, 4, 128]), op=ALU.mult),
                                     r=[BPj, Bm], w=[BPj])
                            msk = None
                            if c == i:
                                msk = Mcb
                            elif br == 2 and c == i - 4:
                                msk = Mwb
                            if msk is not None:
                                S.op("pool", lambda e, P=P, msk=msk: e.tensor_tensor(out=P[:], in0=P[:], in1=msk.unsqueeze(1).to_broadcast([128, 4, 128]), op=ALU.mult),
                                     r=[BPj, B_cst], w=[BPj])
                            for r_ in range(4):
                                S.op("pe", lambda e, r_=r_, P=P, o4=o4, c=c: e.matmul(o4[:, r_, 0:65], lhsT=P[:, r_, :], rhs=vsw[:, c, sw, g, :], start=(c == clo and r_ == 0), stop=(c == i and r_ == 3)),
                                     r=[BPj, B_vsw], w=[Bo])
                        combine(bo, Bo, i, g, br, False, oa, Boa_)
                S.op("act", lambda e, oa=oa: e.activation(out=oab[:], in_=oa[:], func=AF.Copy), r=[Boa_], w=[Boab])
                bt, Bt = ringS.next()
                btb = bt[:].bitcast(BF16).rearrange("p (k t) -> p k t", k=8)
                for k in range(4):
                    S.op("pe", lambda e, k=k, btb=btb: e.transpose(btb[:, k, :], oab[:, k * 128:(k + 1) * 128], identb), r=[Boab, B_cst], w=[Bt])
                S.op("act", lambda e, btb=btb: e.activation(out=o_nsaT[:, :, tsl], in_=btb[:, 0:4, :], func=AF.Copy), r=[Bt], w=[B_onT])
            S.barrier()
    dump("o_nsaT", o_nsaT[:], [B_onT], [128, 4, T], BF16)
    if not active("c2"):
        return None
    gtf = _sequence2(nc, S, sq, b, G, locals())
    if gtf is None:
        return None
    smx.close()
    _peer(nc, S, sq, b, G, locals(), gtf)


def _sequence2(nc, S, sq, b, G, L):
    sb = G["sb"]
    Ring, dump, active = G["Ring"], G["dump"], G["active"]
    cst, cstb, identb, Mcb, Mwb, blkf, onesf = (G[k] for k in ("cst", "cstb", "identb", "Mcb", "Mwb", "blkf", "onesf"))
    B_cst = G["B_cst"]
    x_d, out_d, x1s_d = G["x_d"], G["out_d"], G["x1s_d"]
    T_, ring, fm_units, tk_units = L["T_"], L["ring"], L["fm_units"], L["tk_units"]
    hT, B_hT, o_nsaT, B_onT = L["hT"], L["B_hT"], L["o_nsaT"], L["B_onT"]
    norm_to_T, ada_round, make_scale, bcast_row = L["norm_to_T"], L["ada_round"], L["make_scale"], L["bcast_row"]
    banks, bbuf = G["banks"], G["bbuf"]

    smx = L["smx"]
    o_hgT, B_ohT, res2 = L["o_hgT"], L["B_ohT"], L["res"]
    with ExitStack() as sh:
        qdec = T_("qdec", [128, 4, T], BF16, sh)
        kinv = T_("kinv", [128, 4, T], BF16, sh)
        sgt = T_("sgt", [128, 4, T], BF16, sh)
        vtok = T_("vtok", [128, NT, 512], BF16, sh)
        ebend = T_("ebend", [128, 4, 64], F32, sh)
        lb = T_("lb", [128, 12], F32, sh)
        lbl = T_("lbl", [128, 4, 2], F32, sh)
        ngt = T_("ngt", [128, 1], F32, sh)
        B_qd, B_ki, B_sg, B_vt, B_eb, B_lb = (Buf(n) for n in ("qdec", "kinv", "sgt", "vtok", "ebend", "lb"))
        S.dma("sp", lbl[:], G["lbl_d"][:, :, :], w=[B_lb], key="small")
        S.dma("sp", ngt[:], G["ng_d"][:, :], w=[B_lb], key="small")
        S.op("dve", lambda e: e.tensor_tensor(out=lb[:, 8:12], in0=lbl[:, :, 0], in1=lbl[:, :, 1], op=ALU.subtract), r=[B_lb], w=[B_lb])
        S.op("act", lambda e: e.activation(out=lb[:, 0:4], in_=lb[:, 8:12], func=AF.Sigmoid), r=[B_lb], w=[B_lb])
        S.op("dve", lambda e: e.tensor_scalar(out=lb[:, 4:8], in0=lb[:, 0:4], scalar1=-1.0, scalar2=1.0, op0=ALU.mult, op1=ALU.add), r=[B_lb], w=[B_lb])
        with ExitStack() as st:
            scanm = T_("scanm", [128, 512], F32, st)
            B_sc = Buf("scanm")
            S.dma("sp", scanm[:], G["scanm_d"][:, :], w=[B_sc], key="small")
            tf = [T_("hg_t%d" % i, [128, 512], F32, st) for i in range(6)]
            Btf = [Buf("hg_t%d" % i) for i in range(6)]

            def ep_c2(u, tc, bl):
                sl = slice(tc * 512, (tc + 1) * 512)
                if u >= 12:
                    for g in range(2):
                        h = (u - 12) * 2 + g
                        S.op("act", lambda e, h=h, g=g: e.activation(out=sgt[:, h, sl], in_=bl[g][0][:], func=AF.Silu), r=[bl[g][1]], w=[B_sg])
                    return
                h = u - 8
                (bq, Bq), (bf, Bf) = bl
                f, lf, bb, eb, en, om = tf
                S.op("act", lambda e: e.activation(out=f[:], in_=bf[:], func=AF.Sigmoid), r=[Bf], w=[Btf[0]])
                S.op("dve", lambda e: e.tensor_scalar(out=f[:], in0=f[:], scalar1=lb[:, 4 + h:5 + h], scalar2=lb[:, h:h + 1], op0=ALU.mult, op1=ALU.add),
                     r=[Btf[0], B_lb], w=[Btf[0]])
                S.op("act", lambda e: e.activation(out=lf[:], in_=f[:], func=AF.Ln), r=[Btf[0]], w=[Btf[1]])
                S.op("dve", lambda e: e.tensor_tensor_scan(out=bb[:], data0=scanm[:], data1=lf[:], initial=0.0, op0=ALU.mult, op1=ALU.add),
                     r=[Btf[1], B_sc], w=[Btf[2]])
                S.op("act", lambda e: e.activation(out=eb[:], in_=bb[:], func=AF.Exp), r=[Btf[2]], w=[Btf[3]])
                S.op("act", lambda e: e.activation(out=en[:], in_=bb[:], func=AF.Exp, scale=-1.0), r=[Btf[2]], w=[Btf[4]])
                S.op("pool", lambda e: e.tensor_copy(out=ebend[:, h, tc * 16:(tc + 1) * 16], in_=eb[:, 31:512:32]), r=[Btf[3]], w=[B_eb])
                S.op("pool", lambda e: e.tensor_scalar(out=om[:], in0=f[:], scalar1=-1.0, scalar2=1.0, op0=ALU.mult, op1=ALU.add), r=[Btf[0]], w=[Btf[5]])
                S.op("dve", lambda e: e.tensor_tensor(out=kinv[:, h, sl], in0=om[:], in1=en[:], op=ALU.mult), r=[Btf[5], Btf[4]], w=[B_ki])
                S.op("act", lambda e: e.activation(out=lf[:], in_=bq[:], func=AF.Silu), r=[Bq, Btf[1]], w=[Btf[1]])
                S.op("dve", lambda e: e.tensor_tensor(out=qdec[:, h, sl], in0=lf[:], in1=eb[:], op=ALU.mult), r=[Btf[1], Btf[3]], w=[B_qd])

            fm_units([(u, 2) for u in range(8, 14)], st, ep_c2, nbuf=1)
            S.barrier()
        with ExitStack() as st:

            def ep_t2(u, i, bk, Bk):
                S.op("act", lambda e: e.activation(out=vtok[:, i, (u - 2) * 256:(u - 1) * 256], in_=bk[:, 0:256], func=AF.Copy), r=[Bk], w=[B_vt])

            tk_units([2, 3], st, ep_t2)
            S.barrier()
        dump("qdec", qdec[:], [B_qd], [128, 4, T], BF16)
        dump("kinv", kinv[:], [B_ki], [128, 4, T], BF16)
        dump("ebend", ebend[:], [B_eb], [128, 4, 64])
        dump("vtok", vtok[:], [B_vt], [128, NT, 512], BF16)
        if not active("hgrn"):
            return None

        with ExitStack() as st:
            Sf = T_("Sf", [128, 4, 128], F32, st)
            Sb = T_("Sb", [128, 4, 128], BF16, st)
            Tt = T_("Tt", [128, 4, 128], F32, st)
            kit = [T_("kit%d" % i, [128, 4, 128], BF16, st) for i in range(2)]
            kitz = [T_("kitz%d" % i, [128, 4, 128], BF16, st) for i in range(2)]
            Bkitz = [Buf("kitz%d" % i) for i in range(2)]
            At = [T_("At%d" % i, [128, 4, 128], BF16, st) for i in range(2)]
            sqf = T_("hsq", [128, 512], F32, st)
            rsd = T_("hrsd", [128, 512], F32, st)
            o1 = T_("ho1", [128, 512], F32, st)
            B_S, B_Sb, B_Tt, B_sq, B_rs, B_o1 = (Buf(n) for n in ("Sf", "Sb", "Tt", "hsq", "hrsd", "ho1"))
            Bkit = [Buf("kit%d" % i) for i in range(2)]
            BAt = [Buf("At%d" % i) for i in range(2)]
            S.op("pool", lambda e: e.memset(Sf[:], 0.0), w=[B_S])
            S.op("pool", lambda e: e.memset(Sb[:], 0.0), w=[B_Sb])
            obank = [0, 1, 2, 3]
            r2 = Ring([4, 5, 6, 7])
            for i in range(DBG.get("hg_tiles", NT)):
                tsl = slice(i * 128, (i + 1) * 128)
                j = i % 2
                bt, Bt = r2.next()
                btb = bt[:].bitcast(BF16).rearrange("p (k t) -> p k t", k=8)
                for h in range(4):
                    S.op("pe", lambda e, h=h, btb=btb: e.transpose(btb[:, h, :], kinv[:, h, tsl], identb), r=[B_ki, B_cst], w=[Bt])
                S.op("act", lambda e, btb=btb, j=j: e.activation(out=kit[j][:], in_=btb[:, 0:4, :], func=AF.Copy), r=[Bt], w=[Bkit[j]])
                S.op("dve", lambda e, j=j: e.tensor_scalar(out=kitz[j][:], in0=kit[j][:], scalar1=cst[:, 351:352], scalar2=None, op0=ALU.mult),
                     r=[Bkit[j], B_cst], w=[Bkitz[j]])
                ba, Ba = r2.next()
                ba4 = ba[:].rearrange("p (h t) -> p h t", h=4)
                for h in range(4):
                    S.op("pe", lambda e, h=h, ba4=ba4: e.matmul(ba4[:, h, :], lhsT=kinv[:, h, tsl], rhs=qdec[:, h, tsl], start=True, stop=True),
                         r=[B_ki, B_qd], w=[Ba])
                S.op("dve", lambda e, ba4=ba4, j=j: e.tensor_tensor(out=At[j][:], in0=ba4, in1=blkf.unsqueeze(1).to_broadcast([128, 4, 128]), op=ALU.mult),
                     r=[Ba, B_cst], w=[BAt[j]])
                for sub in range(DBG.get("hg_subs", 4)):
                    c = 4 * i + sub
                    psl = slice(sub * 32, (sub + 1) * 32) if sub < 3 else slice(64, 128)
                    asl = slice(sub * 32, (sub + 1) * 32)
                    kt_, Bkt_ = (kit[j], Bkit[j]) if sub < 3 else (kitz[j], Bkitz[j])
                    csl = slice(c * 32, (c + 1) * 32)
                    osl = slice((c % 16) * 32, (c % 16) * 32 + 32)
                    for h in range(4):
                        ob, Bob = banks[obank[h]], bbuf[obank[h]]
                        S.op("pe", lambda e, h=h, ob=ob: e.matmul(ob[:, osl], lhsT=vtok[psl, i, h * 128:(h + 1) * 128], rhs=At[j][psl, h, asl], start=True, stop=False),
                             r=[B_vt, BAt[j]], w=[Bob])
                        S.op("pe", lambda e, h=h, ob=ob: e.matmul(ob[:, osl], lhsT=Sb[:, h, :], rhs=qdec[:, h, csl], start=False, stop=True),
                             r=[B_Sb, B_qd], w=[Bob])
                    bu, Bu = r2.next()
                    bu4 = bu[:].rearrange("p (h t) -> p h t", h=4)
                    for h in range(4):
                        S.op("pe", lambda e, h=h, bu4=bu4: e.matmul(bu4[:, h, :], lhsT=kt_[psl, h, :], rhs=vtok[psl, i, h * 128:(h + 1) * 128], start=True, stop=True),
                             r=[Bkt_, B_vt], w=[Bu])
                    S.op("dve", lambda e, bu4=bu4: e.tensor_tensor(out=Tt[:], in0=bu4, in1=Sf[:], op=ALU.add), r=[Bu, B_S], w=[B_Tt])
                    S.op("dve", lambda e, c=c: e.tensor_tensor(out=Sf[:], in0=Tt[:], in1=ebend[:, :, c:c + 1].to_broadcast([128, 4, 128]), op=ALU.mult),
                         r=[B_Tt, B_eb], w=[B_S])
                    S.op("act", lambda e: e.activation(out=Sb[:], in_=Sf[:], func=AF.Copy), r=[B_S], w=[B_Sb])
                if i % 4 == 3 and DBG.get("hg_final", True):
                    span = slice((i // 4) * 512, (i // 4 + 1) * 512)
                    for h in range(4):
                        ob, Bob = banks[obank[h]], bbuf[obank[h]]
                        S.op("act", lambda e, ob=ob: e.activation(out=sqf[:], in_=ob[:], func=AF.Square), r=[Bob], w=[B_sq])
                        bm, Bm = r2.next()
                        S.op("pe", lambda e, bm=bm: e.matmul(bm[:], lhsT=onesf, rhs=sqf[:], start=True, stop=True), r=[B_sq, B_cst], w=[Bm])
                        S.op("dve", lambda e, bm=bm: e.tensor_scalar(out=rsd[:], in0=bm[:], scalar1=EPS, scalar2=None, op0=ALU.add), r=[Bm], w=[B_rs])
                        S.op("act", lambda e: e.activation(out=rsd[:], in_=rsd[:], func=AF.Sqrt), r=[B_rs], w=[B_rs])
                        S.op("dve", lambda e: e.reciprocal(out=rsd[:], in_=rsd[:]), r=[B_rs], w=[B_rs])
                        S.op("dve", lambda e, ob=ob: e.tensor_tensor(out=o1[:], in0=ob[:], in1=rsd[:], op=ALU.mult), r=[Bob, B_rs], w=[B_o1])
                        S.op("dve", lambda e, h=h: e.scalar_tensor_tensor(out=o_hgT[:, h, span], in0=o1[:], scalar=ngt[:, 0:1], op0=ALU.mult, in1=sgt[:, h, span], op1=ALU.mult),
                             r=[B_o1, B_lb, B_sg], w=[B_ohT])
            S.barrier()
    dump("o_hgT", o_hgT[:], [B_ohT], [128, 4, T], BF16)
    if not active("merge"):
        return None

    gtm = [res2[4], res2[5]]
    Ashf = [res2[6], res2[7]]
    Ascf = [res2[8], res2[9]]
    gtf = [res2[10], res2[11]]
    yT = T_("yT", [128, 8, T], BF16, smx)
    B_yT = Buf("yT")
    with ExitStack() as st:
        wbs = T_("wbs", [128, 4, D], F32, st)
        wbb = [T_("wbb%d" % i, [128, 4, D], BF16, st) for i in range(2)]
        B_wbs, B_wbb = Buf("wbs"), Buf("wbb")
        for j in range(2):
            S.dma("sp", wbs[:], G["wbr_d"][j], w=[B_wbs], key="wbs")
            S.op("pool", lambda e, j=j: e.tensor_copy(out=wbb[j][:], in_=wbs[:]), r=[B_wbs], w=[B_wbb])
        gaf = [T_("gaf%d" % i, [128, 512], F32, st) for i in range(2)]
        Bga = [Buf("gaf%d" % i) for i in range(2)]

        def ep_m(u, tc, bl):
            c = u - 14
            sl = slice(tc * 512, (tc + 1) * 512)
            srcs = ((o_nsaT, B_onT), (o_hgT, B_ohT))
            for j in range(2):
                S.op("act", lambda e, j=j: e.activation(out=gaf[j][:], in_=bl[j][0][:], func=AF.Sigmoid), r=[bl[j][1]], w=[Bga[j]])
                bk, Bk = ring.next()
                for k in range(4):
                    S.op("pe", lambda e, k=k, j=j, bk=bk: e.matmul(bk[:], lhsT=wbb[j][:, k, c * 128:(c + 1) * 128], rhs=srcs[j][0][:, k, sl], start=(k == 0), stop=(k == 3)),
                         r=[B_wbb, srcs[j][1]], w=[Bk])
                S.op("dve", lambda e, j=j, bk=bk: e.tensor_tensor(out=gaf[j][:], in0=gaf[j][:], in1=bk[:], op=ALU.mult), r=[Bga[j], Bk], w=[Bga[j]])
            S.op("pool", lambda e: e.tensor_tensor(out=yT[:, c, sl], in0=gaf[0][:], in1=gaf[1][:], op=ALU.add), r=[Bga[0], Bga[1]], w=[B_yT])

        fm_units([(u, 2) for u in range(14, 22)], st, ep_m)
        S.barrier()
    dump("yT", yT[:], [B_yT], [128, 8, T], BF16)

    with ExitStack() as st:
        wos = T_("wos", [128, 8, 512], F32, st)
        wob = T_("wob", [128, 8, D], BF16, st)
        B_wos, B_wob = Buf("wos"), Buf("wob")
        for hf in range(2):
            S.dma("sp", wos[:], G["wout_d"][:, :, hf * 512:(hf + 1) * 512], w=[B_wos], key="wos")
            S.op("pool", lambda e, hf=hf: e.tensor_copy(out=wob[:, :, hf * 512:(hf + 1) * 512], in_=wos[:]), r=[B_wos], w=[B_wob])
        xr = [T_("xr2_%d" % i, [128, D], F32, st) for i in range(2)]
        Bxr = [Buf("xr2_%d" % i) for i in range(2)]
        x1t = [T_("x1t%d" % i, [128, D], F32, st) for i in range(2)]
        Bx1 = [Buf("x1t%d" % i) for i in range(2)]
        tmy = T_("tmy", [128, D], F32, st)
        Btmy = Buf("tmy")
        nb_ = (T_("n2_ss", [128, 1], F32, st), T_("n2_rstd", [128, 1], F32, st), T_("n2_h1", [128, D], F32, st), T_("n2_hb", [128, D], BF16, st),
               Buf("n2_ss"), Buf("n2_h1"), Buf("n2_hb"))
        for i in range(NT):
            j = i % 2
            tsl = slice(i * 128, (i + 1) * 128)
            S.dma("sp", xr[j][:], x_d[b, tsl, :], w=[Bxr[j]], key="xr2_%d" % j)
            for hf in range(2):
                bk, Bk = ring.next()
                sl = slice(hf * 512, (hf + 1) * 512)
                for k in range(8):
                    S.op("pe", lambda e, k=k, bk=bk, sl=sl: e.matmul(bk[:], lhsT=yT[:, k, tsl], rhs=wob[:, k, sl], start=(k == 0), stop=(k == 7)),
                         r=[B_yT, B_wob], w=[Bk])
                S.op("dve", lambda e, bk=bk, sl=sl, hf=hf: e.tensor_tensor(out=tmy[:, sl], in0=bk[:], in1=gtm[hf][0][:], op=ALU.mult), r=[Bk, gtm[hf][1]], w=[Btmy])
            S.op("pool", lambda e, j=j: e.tensor_tensor(out=x1t[j][:], in0=xr[j][:], in1=tmy[:], op=ALU.add), r=[Bxr[j], Btmy], w=[Bx1[j]])
            S.dma("sp", x1s_d[b, tsl, :], x1t[j][:], r=[Bx1[j]], key="x1w%d" % j)
            norm_to_T(x1t[j], Bx1[j], Ascf, Ashf, hT, B_hT, i, nb_)
        S.barrier()
    dump("h2T", hT[:], [B_hT], [128, 8, T], BF16)
    if "x1" in G["dbg"]:
        with ExitStack() as st:
            xx = T_("dbgx1", [128, NT, D], F32, st)
            Bxx = Buf("dbgx1")
            S.dma("sp", xx[:], x1s_d[b].rearrange("(i p) d -> p i d", p=128), w=[Bxx], key="dbgx")
            dump("x1", xx[:], [Bxx], [128, NT, D])
            S.barrier()
    if not active("peer"):
        return None
    return gtf


def _peer(nc, S, sq, b, G, L, gtf):
    sb = G["sb"]
    Ring, dump = G["Ring"], G["dump"]
    identb = G["identb"]
    B_cst = G["B_cst"]
    x1s_d, out_d = G["x1s_d"], G["out_d"]
    T_, hT, B_hT, bcast_row = L["T_"], L["hT"], L["B_hT"], L["bcast_row"]
    banks, bbuf = G["banks"], G["bbuf"]
    uTb_d, pvb_d, wpqb_d = G["uTb_d"], G["pvb_d"], G["wpqb_d"]
    B_scr = G["B_scr"]
    with ExitStack() as st:
        skb = T_("skb", [128, 16, 128], BF16, st)
        gfin = T_("gfin", [128, D], F32, st)
        B_sk, B_gf = Buf("skb"), Buf("gfin")
        with ExitStack() as tmp:
            skf = T_("skf", [128, 16, 128], F32, tmp)
            S.dma("sp", skf[:], G["skT_d"][:, :, :], w=[B_sk], key="small")
            S.op("act", lambda e: e.activation(out=skb[:], in_=skf[:], func=AF.Copy), r=[B_sk], w=[B_sk])
            S.barrier()
        bcast_row(gfin[:], G["gfin_d"][0:1, :], [B_gf], "small")
        qpT = T_("qpT", [128, 16, 256], BF16, st)
        B_qp = Buf("qpT")
        ssb = [T_("ssb%d" % i, [128, 16, 128], F32, st) for i in range(2)]
        Bss = [Buf("ssb%d" % i) for i in range(2)]
        top = T_("ptop", [128, 16, 16], F32, st)
        scr = T_("pscr", [128, 128], F32, st)
        comb = T_("pcomb", [128, 8, 256], F32, st)
        scrc = T_("pscrc", [128, 256], F32, st)
        ctop = [T_("pctop%d" % i, [128, 8, 16], F32, st) for i in range(2)]
        pez = T_("pez", [128, 8, 16], F32, st)
        negc = [T_("pnegc%d" % i, [128, 8], F32, st) for i in range(2)]
        Btk = Buf("topk_scratch")
        Bct = [Buf("ctop%d" % i) for i in range(2)]
        tb = [T_("ptb%d" % i, [128, 16, 128], F32, st) for i in range(3)]
        eb = [T_("peb%d" % i, [128, 16, 128], BF16, st) for i in range(2)]
        wh = [T_("pwh0", [128, 16, 128], BF16, st)] * 2
        Btb = [Buf("ptb%d" % i) for i in range(3)]
        Beb = [Buf("peb%d" % i) for i in range(2)]
        Bwh = [Buf("pwh0")] * 2
        acc = [T_("pacc%d" % i, [128, 2, 4096], BF16, st) for i in range(2)]
        Bacc = [[Buf("pacc%d_%d" % (i, t)) for t in range(2)] for i in range(2)]
        ub = [T_("pub%d" % i, [128, 8, 512], BF16, st) for i in range(2)]
        vb = [T_("pvb%d" % i, [128, 4, D], BF16, st) for i in range(2)]
        Bub = [Buf("pub%d" % i) for i in range(2)]
        Bvb = [Buf("pvb%d" % i) for i in range(2)]
        ga = [T_("pga%d" % i, [128, 512], BF16, st) for i in range(3)]
        wa = [T_("pwa%d" % i, [128, 512], BF16, st) for i in range(3)]
        waT = [T_("pwaT%d" % i, [128, 4, 128], BF16, st) for i in range(3)]
        Bga = [Buf("pga%d" % i) for i in range(3)]
        Bwa = [Buf("pwa%d" % i) for i in range(3)]
        BwaT = [Buf("pwaT%d" % i) for i in range(3)]
        x1t = T_("px1", [128, D], F32, st)
        xo = x1t
        ptmp = T_("ptmp", [128, D], F32, st)
        pss = T_("pss", [128, 2], F32, st)
        Bx1 = Buf("px1")
        Bxo, Bpt, Bpss = Bx1, Buf("ptmp"), Buf("pss")
        r2 = Ring([4, 5, 6, 7])
        rA = Ring([4, 5])
        ucnt = [0]
        mcnt = [0]
        wcnt = [0]

        for st_ in range(T // 256):
            n0 = st_ * 256
            for pi in range(4):
                j = ucnt[0] % 2
                ucnt[0] += 1
                S.dma("sp", ub[j][:], wpqb_d[pi], r=[B_scr], w=[Bub[j]], key="pub%d" % j)
                for gg in range(4):
                    bk, Bk = r2.next()
                    for k in range(8):
                        S.op("pe", lambda e, k=k, gg=gg, bk=bk, j=j: e.matmul(bk[:, 0:256], lhsT=ub[j][:, k, gg * 128:(gg + 1) * 128], rhs=hT[:, k, n0:n0 + 256],
                                                                             start=(k == 0), stop=(k == 7)), r=[Bub[j], B_hT], w=[Bk])
                    S.op("act", lambda e, gg=gg, bk=bk, pi=pi: e.activation(out=qpT[:, pi * 4 + gg, :], in_=bk[:, 0:256], func=AF.Copy), r=[Bk], w=[B_qp])
            for tt in range(2):
                for q4 in range(4):
                    bk, Bk = r2.next()
                    for hh in range(4):
                        hp = q4 * 4 + hh
                        S.op("pe", lambda e, hp=hp, hh=hh, bk=bk: e.matmul(bk[:, hh * 128:(hh + 1) * 128], lhsT=qpT[:, hp, tt * 128:(tt + 1) * 128], rhs=skb[:, hp, :],
                                                                          start=True, stop=True), r=[B_qp, B_sk], w=[Bk])
                    S.op("act", lambda e, q4=q4, bk=bk: e.activation(out=ssb[tt][:, q4 * 4:(q4 + 1) * 4, :], in_=bk[:].rearrange("p (a k) -> p a k", a=4), func=AF.Copy),
                         r=[Bk], w=[Bss[tt]])
                for hp in range(16):
                    S.op("dve", lambda e, hp=hp: e.max(out=top[:, hp, 0:8], in_=ssb[tt][:, hp, :]), r=[Bss[tt]], w=[Btk])
                    S.op("dve", lambda e, hp=hp: e.match_replace(out=scr[:], in_to_replace=top[:, hp, 0:8], in_values=ssb[tt][:, hp, :], imm_value=-1e30), r=[Bss[tt], Btk], w=[Btk])
                    S.op("dve", lambda e, hp=hp: e.max(out=top[:, hp, 8:16], in_=scr[:]), r=[Btk], w=[Btk])
                t4 = top[:].rearrange("p (h q) r -> p h q r", q=2)
                S.op("dve", lambda e, t4=t4: e.tensor_tensor(out=comb[:].rearrange("p h (a c) -> p h a c", a=16), in0=t4[:, :, 0, :].unsqueeze(3).to_broadcast([128, 8, 16, 16]),
                                                          in1=t4[:, :, 1, :].unsqueeze(2).to_broadcast([128, 8, 16, 16]), op=ALU.add), r=[Btk], w=[Btk])
                for h in range(8):
                    S.op("dve", lambda e, h=h: e.max(out=ctop[tt][:, h, 0:8], in_=comb[:, h, :]), r=[Btk], w=[Bct[tt]])
                    S.op("dve", lambda e, h=h: e.match_replace(out=scrc[:], in_to_replace=ctop[tt][:, h, 0:8], in_values=comb[:, h, :], imm_value=-1e30), r=[Btk, Bct[tt]], w=[Btk])
                    S.op("dve", lambda e, h=h: e.max(out=ctop[tt][:, h, 8:16], in_=scrc[:]), r=[Btk], w=[Bct[tt]])
                S.op("dve", lambda e: e.tensor_tensor(out=pez[:], in0=ctop[tt][:], in1=ctop[tt][:, :, 0:1].to_broadcast([128, 8, 16]), op=ALU.subtract), r=[Bct[tt]], w=[Btk])
                S.op("act", lambda e: e.activation(out=pez[:], in_=pez[:], func=AF.Exp), r=[Btk], w=[Btk])
                S.op("dve", lambda e: e.reduce_sum(out=negc[tt][:], in_=pez[:], axis=AX.X), r=[Btk], w=[Bct[tt]])
                S.op("act", lambda e: e.activation(out=negc[tt][:], in_=negc[tt][:], func=AF.Ln), r=[Bct[tt]], w=[Bct[tt]])
                S.op("dve", lambda e: e.tensor_tensor(out=negc[tt][:], in0=negc[tt][:], in1=ctop[tt][:, :, 0], op=ALU.add), r=[Bct[tt]], w=[Bct[tt]])
                S.op("dve", lambda e: e.tensor_scalar(out=negc[tt][:], in0=negc[tt][:], scalar1=-1.0, scalar2=None, op0=ALU.mult), r=[Bct[tt]], w=[Bct[tt]])

            def w_tadd(q):
                k, n = divmod(q, 32)
                tt, rem = divmod(n, 16)
                hh, h = divmod(rem, 8)
                r0 = k * 32 + hh * 16
                i = q % 3
                S.op("pool", lambda e: e.tensor_tensor(out=tb[i][:], in0=ssb[tt][:, 2 * h, r0:r0 + 16].unsqueeze(2).to_broadcast([128, 16, 128]),
                                                      in1=ssb[tt][:, 2 * h + 1, :].unsqueeze(1).to_broadcast([128, 16, 128]), op=ALU.add), r=[Bss[tt]], w=[Btb[i]])

            def w_rest(q):
                k, n = divmod(q, 32)
                tt, rem = divmod(n, 16)
                hh, h = divmod(rem, 8)
                kb = k % 2
                i = q % 2
                t3 = q % 3
                S.op("act", lambda e: e.activation(out=eb[i][:], in_=tb[t3][:], func=AF.Exp, bias=negc[tt][:, h:h + 1]), r=[Btb[t3], Bct[tt]], w=[Beb[i]])
                dst = acc[kb][:, tt, hh * 2048:(hh + 1) * 2048].rearrange("p (a c) -> p a c", a=16)
                if h == 0:
                    S.op("dve", lambda e: e.scalar_tensor_tensor(out=dst, in0=tb[t3][:], scalar=ctop[tt][:, h, 15:16], op0=ALU.is_ge, in1=eb[i][:], op1=ALU.mult),
                         r=[Btb[t3], Beb[i], Bct[tt]], w=[Bacc[kb][tt]])
                else:
                    S.op("dve", lambda e: e.scalar_tensor_tensor(out=wh[i][:], in0=tb[t3][:], scalar=ctop[tt][:, h, 15:16], op0=ALU.is_ge, in1=eb[i][:], op1=ALU.mult),
                         r=[Btb[t3], Beb[i], Bct[tt]], w=[Bwh[i]])
                    S.op("dve", lambda e: e.tensor_tensor(out=dst, in0=dst, in1=wh[i][:], op=ALU.add), r=[Bwh[i], Bacc[kb][tt]], w=[Bacc[kb][tt]])

            wq = [0]
            wt = [0]

            def w_emit(upto_q):
                while wq[0] < upto_q:
                    q = wq[0]
                    while wt[0] < min(q + 3, 128):
                        w_tadd(wt[0])
                        wt[0] += 1
                    w_rest(q)
                    wq[0] += 1

            mst = {}

            def mA(s_):
                k, mi = divmod(s_, 16)
                jg, tt = divmod(mi, 2)
                eg = k * 8 + jg
                if tt == 0:
                    j = ucnt[0] % 2
                    ucnt[0] += 1
                    S.dma("sp", ub[j][:], uTb_d[eg], r=[B_scr], w=[Bub[j]], key="pub%d" % j)
                    S.dma("sp", vb[j][:], pvb_d[eg], r=[B_scr], w=[Bvb[j]], key="pvb%d" % j)
                j = (ucnt[0] - 1) % 2
                bk, Bk = rA.next()
                tok = slice(n0 + tt * 128, n0 + (tt + 1) * 128)
                for kk in range(8):
                    S.op("pe", lambda e, kk=kk: e.matmul(bk[:], lhsT=hT[:, kk, tok], rhs=ub[j][:, kk, :], start=(kk == 0), stop=(kk == 7)), r=[B_hT, Bub[j]], w=[Bk])
                mst[s_] = dict(j=j, bk=bk, Bk=Bk, k=k, jg=jg, tt=tt, eg=eg, m=s_ % 3)

            def mB(s_):
                d_ = mst[s_]
                m, bk, Bk, kb, tt, jg = d_["m"], d_["bk"], d_["Bk"], d_["k"] % 2, d_["tt"], d_["jg"]
                S.op("act", lambda e: e.activation(out=ga[m][:], in_=bk[:], func=AF.Gelu_apprx_tanh), r=[Bk], w=[Bga[m]])
                S.op("dve", lambda e: e.tensor_tensor(out=wa[m][:], in0=ga[m][:], in1=acc[kb][:, tt, jg * 512:(jg + 1) * 512], op=ALU.mult), r=[Bga[m], Bacc[kb][tt]], w=[Bwa[m]])

            def mC(s_):
                d_ = mst[s_]
                m = d_["m"]
                hb_ = 6 + s_ % 2
                btb = banks[hb_][:].bitcast(BF16).rearrange("p (k t) -> p k t", k=8)[:, 0:4, :]
                for jj in range(4):
                    S.op("pe", lambda e, jj=jj: e.transpose(btb[:, jj, :], wa[m][:, jj * 128:(jj + 1) * 128], identb), r=[Bwa[m], B_cst], w=[bbuf[hb_]])
                S.op("act", lambda e: e.activation(out=waT[m][:], in_=btb, func=AF.Copy), r=[bbuf[hb_]], w=[BwaT[m]])

            def mD(s_):
                d_ = mst.pop(s_)
                m, j, tt, eg = d_["m"], d_["j"], d_["tt"], d_["eg"]
                for jj in range(4):
                    for hf in range(2):
                        yb, By = banks[tt * 2 + hf], bbuf[tt * 2 + hf]
                        S.op("pe", lambda e, jj=jj, hf=hf, yb=yb: e.matmul(yb[:], lhsT=waT[m][:, jj, :], rhs=vb[j][:, jj, hf * 512:(hf + 1) * 512],
                                                                          start=(eg == 0 and jj == 0), stop=(eg == 31 and jj == 3)), r=[BwaT[m], Bvb[j]], w=[By])

            ms = [0]

            def m_emit(nsteps):
                for _ in range(nsteps):
                    s_ = ms[0]
                    if s_ < 64:
                        mA(s_)
                        mB(s_)
                    if 0 <= s_ - 1 < 64:
                        mC(s_ - 1)
                    if 0 <= s_ - 2 < 64:
                        mD(s_ - 2)
                    ms[0] += 1

            w_emit(32)
            for k in range(4):
                for ch in range(4):
                    m_emit(4)
                    if k < 3:
                        w_emit(32 * (k + 1) + 8 * (ch + 1))
            m_emit(2)
            for tt in range(2):
                tok = slice(n0 + tt * 128, n0 + (tt + 1) * 128)
                S.dma("sp", x1t[:], x1s_d[b, tok, :], w=[Bx1], key="px1")
                for hf in range(2):
                    sl = slice(hf * 512, (hf + 1) * 512)
                    yb, By = banks[tt * 2 + hf], bbuf[tt * 2 + hf]
                    S.op("dve", lambda e, yb=yb, sl=sl, hf=hf: e.tensor_tensor(out=ptmp[:, sl], in0=yb[:], in1=gtf[hf][0][:], op=ALU.mult), r=[By, gtf[hf][1]], w=[Bpt])
                S.op("pool", lambda e: e.tensor_tensor(out=xo[:], in0=x1t[:], in1=ptmp[:], op=ALU.add), r=[Bx1, Bpt], w=[Bxo])
                S.op("act", lambda e: e.activation(out=ptmp[:], in_=xo[:], func=AF.Square, accum_out=pss[:, 0:1]), r=[Bxo, Bpt], w=[Bpt, Bpss])
                S.op("dve", lambda e: e.tensor_scalar(out=pss[:, 1:2], in0=pss[:, 0:1], scalar1=1.0 / D, scalar2=EPS, op0=ALU.mult, op1=ALU.add), r=[Bpss], w=[Bpss])
                S.op("act", lambda e: e.activation(out=pss[:, 1:2], in_=pss[:, 1:2], func=AF.Sqrt), r=[Bpss], w=[Bpss])
                S.op("dve", lambda e: e.reciprocal(out=pss[:, 1:2], in_=pss[:, 1:2]), r=[Bpss], w=[Bpss])
                S.op("dve", lambda e: e.scalar_tensor_tensor(out=xo[:], in0=xo[:], scalar=pss[:, 1:2], op0=ALU.mult, in1=gfin[:], op1=ALU.mult), r=[Bxo, Bpss, B_gf], w=[Bxo])
                S.dma("sp", out_d[b, tok, :], xo[:], r=[Bxo], key="outw")
        S.barrier()


def _sel_map():
    i = np.arange(127)[:, None]
    j = np.arange(32)[None, :]
    d = i - 4 * j
    cnt = np.minimum(d, 3) - np.maximum(d - 1, 0) + 1
    return np.clip(cnt, 0, None).astype(np.float32)


def _const_inputs():
    f32 = np.float32
    s = np.arange(128)[:, None]
    t = np.arange(128)[None, :]
    cst = np.concatenate([np.eye(128), (s <= t), (s > t), ((s // 32 == t // 32) & (s <= t)), np.full((128, 128), 1.0 / 128)], axis=1).astype(f32)
    half = 32
    inv = (np.float32(10000.0) ** (-np.arange(half, dtype=f32) / np.float32(half))).astype(f32)
    ang = (np.arange(T, dtype=f32)[:, None] * inv[None, :]).astype(f32)
    cos = np.cos(ang).astype(f32).T
    sin = np.sin(ang).astype(f32).T
    cos64 = np.concatenate([cos, cos], axis=0)
    sin64 = np.concatenate([-sin, sin], axis=0)
    rope = np.stack([np.concatenate([cos64, cos64], 0), np.concatenate([sin64, sin64], 0)], axis=1).astype(f32)
    scanm = np.ones((128, 512), f32)
    scanm[:, 0::32] = 0.0
    n = np.arange(128)[:, None]
    tt = np.arange(T)[None, :]
    mcmp = ((16 * n + 31 <= tt) & (n < 127)).astype(f32)
    tok = (np.arange(NT)[None, :] * 128 + np.arange(128)[:, None])
    jb = np.arange(32)[None, None, :]
    cur = (tok // 64)[:, :, None]
    forced = (jb == 0) | (jb == cur) | (jb == cur - 1)
    causal = (jb * 64 <= tok[:, :, None])
    f1c = np.stack([1000.0 * forced + 1.0, causal.astype(np.float64)], axis=1).astype(f32)
    eall = np.zeros((128, T), f32)
    col = np.arange(T)
    eall[(2 * (col // 128) + (col % 128) // 64), col] = 1.0
    selmap = np.zeros((128, 32), f32)
    selmap[:127] = _sel_map()
    return dict(cst=cst, rope=rope, scanm=scanm, mcmp=mcmp, f1c=f1c, eall=eall, selmap=selmap)


def _kchunk(w):
    return np.ascontiguousarray(w.reshape(8, 128, w.shape[1]).transpose(1, 0, 2))


def _weight_inputs(w_ada, b_ada, g_mix, g_ffn, w_in, cmp_pos_k, cmp_pos_v, w_ck1, w_ck2, w_cv1, w_cv2, hgrn_lb_logits, hgrn_out_norm,
                   w_branch, w_out, w_peer_q, peer_sub_keys, peer_u, peer_v, g_final):
    f32 = np.float32
    W = np.asarray(w_in[0], f32)
    sw = (np.arange(64) + 32) % 64

    def cols(base, idx):
        return base + np.asarray(idx)

    units = []
    for a in range(4):
        x = np.concatenate([cols(a * 64, np.arange(64)), cols((a + 4) * 64, np.arange(64))])
        xs = np.concatenate([cols(a * 64, sw), cols((a + 4) * 64, sw)])
        units.append((x, xs))
    for base in (768, 1024, 512):
        x = cols(base, np.arange(128))
        xs = np.concatenate([cols(base, sw), cols(base + 64, sw)])
        units.append((x, xs))
    units.append((cols(640, np.arange(128)), None))
    for h in range(4):
        units.append((cols(1304 + h * 128, np.arange(128)), cols(1816 + h * 128, np.arange(128))))
    for hp in range(2):
        units.append((cols(2840 + (2 * hp) * 128, np.arange(128)), cols(2840 + (2 * hp + 1) * 128, np.arange(128))))
    for c in range(8):
        units.append((cols(3352 + c * 128, np.arange(128)), cols(3352 + 1024 + c * 128, np.arange(128))))
    wfm = np.zeros((22, 128, 8, 256), f32)
    for u, (a, bb) in enumerate(units):
        wfm[u, :, :, 0:128] = _kchunk(W[:, a])
        if bb is not None:
            wfm[u, :, :, 128:256] = _kchunk(W[:, bb])
    wtk = np.zeros((4, 128, 8, 256), f32)
    wtk[0, :, :, 0:128] = _kchunk(W[:, 896:1024])
    wtk[0, :, :, 128:256] = _kchunk(W[:, 1152:1280])
    wtk[1, :, :, 0:24] = _kchunk(W[:, 1280:1304])
    wtk[2] = _kchunk(W[:, 2328:2584])
    wtk[3] = _kchunk(W[:, 2584:2840])
    wa = np.asarray(w_ada[0], f32)
    wada = np.ascontiguousarray(wa.reshape(8, 128, 12, 512).transpose(2, 1, 0, 3))
    w1 = np.zeros((2, 128, 32, 128), f32)
    pos = np.zeros((2, 128, 32), f32)
    for kv, (w1_, p_) in enumerate(((w_ck1, cmp_pos_k), (w_cv1, cmp_pos_v))):
        a = np.asarray(w1_[0], f32).reshape(32, 64, 128).transpose(1, 0, 2)
        w1[kv, 0:64] = a
        w1[kv, 64:128] = a
        pp = np.asarray(p_[0], f32).T
        pos[kv, 0:64] = pp
        pos[kv, 64:128] = pp
    w2k = np.zeros((128, 256), f32)
    w2k[:, 0:64] = np.asarray(w_ck2[0], f32)
    w2k[:, 128 + 64:256] = np.asarray(w_ck2[0], f32)
    lbl = np.ascontiguousarray(np.asarray(hgrn_lb_logits, f32).reshape(2, 4, 128).transpose(2, 1, 0))
    wbr = np.ascontiguousarray(np.asarray(w_branch[0], f32).reshape(2, 4, 128, D).transpose(0, 2, 1, 3))
    wpq = np.ascontiguousarray(np.asarray(w_peer_q[0], f32).reshape(8, 128, 4, 512).transpose(2, 1, 0, 3))
    skT = np.ascontiguousarray(np.asarray(peer_sub_keys[0], f32).transpose(3, 1, 0, 2).reshape(128, 16, 128))
    uT = np.ascontiguousarray(np.asarray(peer_u[0], f32).reshape(32, 512, 8, 128).transpose(0, 3, 2, 1))
    pv = np.ascontiguousarray(np.asarray(peer_v[0], f32).reshape(32, 4, 128, D).transpose(0, 2, 1, 3))
    return dict(
        wada=wada, bada=np.asarray(b_ada, f32).reshape(1, 6 * D), gmix=np.asarray(g_mix, f32).reshape(1, D),
        gffn=np.asarray(g_ffn, f32).reshape(1, D), gfin=np.asarray(g_final, f32).reshape(1, D), wfm=wfm, wtk=wtk,
        w1=w1, pos=pos, w2k=w2k, w2v=np.ascontiguousarray(np.asarray(w_cv2[0], f32)), lbl=lbl,
        ng=np.asarray(hgrn_out_norm[0], f32).reshape(128, 1), wbr=wbr, wout=_kchunk(np.asarray(w_out[0], f32)),
        wpq=wpq, skT=skT, uT=uT, pv=pv)


def core_inputs(x, c, shared, b0, nb):
    m = dict(shared)
    m["x"] = np.ascontiguousarray(np.asarray(x[b0:b0 + nb], np.float32))
    cc = np.asarray(c[b0:b0 + nb], np.float32)
    m["cT"] = np.ascontiguousarray(cc.reshape(nb, 8, 128).transpose(2, 0, 1))
    return m


def kernel(x, c, w_ada, b_ada, g_mix, g_ffn, w_in, cmp_pos_k, cmp_pos_v, w_ck1, w_ck2, w_cv1, w_cv2, hgrn_lb_logits, hgrn_out_norm,
           w_branch, w_out, w_peer_q, peer_sub_keys, peer_u, peer_v, g_final):
    x = np.asarray(x)
    B = x.shape[0]
    nb = B // NCORES
    shared = _const_inputs()
    shared.update(_weight_inputs(w_ada, b_ada, g_mix, g_ffn, w_in, cmp_pos_k, cmp_pos_v, w_ck1, w_ck2, w_cv1, w_cv2, hgrn_lb_logits,
                                 hgrn_out_norm, w_branch, w_out, w_peer_q, peer_sub_keys, peer_u, peer_v, g_final))
    nc, _ = build_nc(nb=nb)
    in_maps = [core_inputs(x, c, shared, i * nb, nb) for i in range(NCORES)]
    res = run_bass_kernel_spmd(nc, in_maps, core_ids=list(range(NCORES)))
    out = np.concatenate([np.asarray(r["out"]).reshape(nb, T, D) for r in res.results], axis=0)
    return out.astype(np.float32)
```

```python
from contextlib import ExitStack

import numpy as np
import concourse.bass as bass
import concourse.mybir as mybir
from concourse.bass_utils import run_bass_kernel_spmd

F32 = mybir.dt.float32
BF16 = mybir.dt.bfloat16
AF = mybir.ActivationFunctionType
ALU = mybir.AluOpType
AX = mybir.AxisListType

T = 2048
D = 1024
NT = T // 128
EPS = 1e-6
NCORES = 8
DBG = {}


class Buf:
    __slots__ = ("name", "w", "r")

    def __init__(self, name):
        self.name = name
        self.w = {}
        self.r = {}


class Sched:
    def __init__(self, nc, es):
        self.nc = nc
        self.es = es
        self.eng = {"pe": nc.tensor, "act": nc.scalar, "dve": nc.vector, "pool": nc.gpsimd, "sp": nc.sync}
        self.sem = {}
        self.cnt = {}
        for e in ("pe", "act", "dve", "pool"):
            self.sem["E" + e] = es.enter_context(nc.semaphore("s_" + e))
            self.cnt["E" + e] = 0
        self.wm = {e: {} for e in self.eng}
        self.nwait = 0
        self.nins = 0

    def _waits(self, eng, reads, writes):
        deps = {}
        for b in reads:
            for k, v in b.w.items():
                if deps.get(k, 0) < v:
                    deps[k] = v
        for b in writes:
            for k, v in b.w.items():
                if deps.get(k, 0) < v:
                    deps[k] = v
            for k, v in b.r.items():
                if deps.get(k, 0) < v:
                    deps[k] = v
        wm = self.wm[eng]
        e = self.eng[eng]
        for k, v in deps.items():
            if eng == "pe" and k == "Epe":
                continue
            if wm.get(k, 0) < v:
                e.wait_ge(self.sem[k], v)
                wm[k] = v
                self.nwait += 1

    def op(self, eng, fn, r=(), w=()):
        self._waits(eng, r, w)
        k = "E" + eng
        self.cnt[k] += 1
        v = self.cnt[k]
        fn(self.eng[eng]).then_inc(self.sem[k], 1)
        self.nins += 1
        for b in r:
            b.r[k] = v
        for b in w:
            b.w[k] = v

    def dma(self, q, out, in_, r=(), w=(), key="d"):
        k = "D" + key
        if k not in self.sem:
            self.sem[k] = self.es.enter_context(self.nc.semaphore("d_" + key))
            self.cnt[k] = 0
        self._waits(q, r, w)
        if self.cnt[k] > 0 and self.wm[q].get(k, 0) < self.cnt[k]:
            self.eng[q].wait_ge(self.sem[k], self.cnt[k])
            self.wm[q][k] = self.cnt[k]
        self.cnt[k] += 16
        v = self.cnt[k]
        self.eng[q].dma_start(out=out, in_=in_).then_inc(self.sem[k], 16)
        self.nins += 1
        for b in r:
            b.r[k] = v
        for b in w:
            b.w[k] = v

    def barrier(self, engines=("pe", "act", "dve", "pool", "sp")):
        for eng in engines:
            wm = self.wm[eng]
            e = self.eng[eng]
            for k, v in self.cnt.items():
                if v > 0 and wm.get(k, 0) < v and not (eng == "pe" and k == "Epe"):
                    e.wait_ge(self.sem[k], v)
                    wm[k] = v


class _Stop(Exception):
    pass


def build_nc(nb=2, upto="all", dbg=()):
    nc = bass.Bass("TRN2", target_bir_lowering=False)
    NTOK = nb * T

    def din(name, shape, dt=F32):
        return nc.dram_tensor(name, list(shape), dt, kind="ExternalInput").ap()

    x_d = din("x", [nb, T, D])
    cT_d = din("cT", [128, nb, 8])
    wada_d = din("wada", [12, 128, 8, 512])
    bada_d = din("bada", [1, 6 * D])
    gmix_d = din("gmix", [1, D])
    gffn_d = din("gffn", [1, D])
    gfin_d = din("gfin", [1, D])
    wfm_d = din("wfm", [22, 128, 8, 256])
    wtk_d = din("wtk", [4, 128, 8, 256])
    cst_d = din("cst", [128, 640])
    rope_d = din("rope", [128, 2, T])
    scanm_d = din("scanm", [128, 512])
    mcmp_d = din("mcmp", [128, T])
    f1c_d = din("f1c", [128, 2, NT, 32])
    eall_d = din("eall", [128, T])
    selmap_d = din("selmap", [128, 32])
    w1_d = din("w1", [2, 128, 32, 128])
    pos_d = din("pos", [2, 128, 32])
    w2k_d = din("w2k", [128, 256])
    w2v_d = din("w2v", [128, 64])
    lbl_d = din("lbl", [128, 4, 2])
    ng_d = din("ng", [128, 1])
    wbr_d = din("wbr", [2, 128, 4, D])
    wout_d = din("wout", [128, 8, D])
    wpq_d = din("wpq", [4, 128, 8, 512])
    skT_d = din("skT", [128, 16, 128])
    uT_d = din("uT", [32, 128, 8, 512])
    pv_d = din("pv", [32, 128, 4, D])
    out_d = nc.dram_tensor("out", [nb, T, D], F32, kind="ExternalOutput").ap()
    x1s_d = nc.dram_tensor("x1s", [nb, T, D], F32, kind="Internal").ap()
    uTb_d = nc.dram_tensor("uTb", [32, 128, 8, 512], BF16, kind="Internal").ap()
    pvb_d = nc.dram_tensor("pvb", [32, 128, 4, D], BF16, kind="Internal").ap()
    wpqb_d = nc.dram_tensor("wpqb", [4, 128, 8, 512], BF16, kind="Internal").ap()
    dbg_d = {}

    PH = ["conv", "ada1", "hT", "c1", "cmp", "attn", "c2", "hgrn", "merge", "peer", "all"]
    lim = PH.index(upto)

    def active(p):
        return PH.index(p) <= lim

    with ExitStack() as es:
        S = Sched(nc, es)

        uniq = [0]

        def sb(name, shape, dt=F32, st=es):
            uniq[0] += 1
            return st.enter_context(nc.sbuf_tensor("sb%d_%s" % (uniq[0], name), list(shape), dt))

        banks = [es.enter_context(nc.psum_tensor("bank%d" % i, [128, 512], F32)) for i in range(8)]
        bbuf = [Buf("bank%d" % i) for i in range(8)]

        class Ring:
            def __init__(self, idx):
                self.idx = list(idx)
                self.p = 0

            def next(self):
                i = self.idx[self.p % len(self.idx)]
                self.p += 1
                return banks[i], bbuf[i]

        def dump(name, ap, bufs, shape, dt=F32):
            if name not in dbg:
                return
            d = nc.dram_tensor("dbg_" + name, list(shape), dt, kind="ExternalOutput").ap()
            dbg_d[name] = d
            S.dma("sp", d, ap, r=bufs, key="dbg")

        es.enter_context(nc.Block())

        cst = sb("cst", [128, 640])
        cstb = sb("cstb", [128, 640], BF16)
        B_cst = Buf("cst")
        S.dma("sp", cst[:], cst_d[:, :], w=[B_cst], key="cst")
        S.op("act", lambda e: e.activation(out=cstb[:], in_=cst[:], func=AF.Copy), r=[B_cst], w=[B_cst])
        identb = cstb[:, 0:128]
        Mcb = cstb[:, 128:256]
        Mwb = cstb[:, 256:384]
        blkf = cst[:, 384:512]
        onesf = cst[:, 512:640]

        if active("conv") and lim >= PH.index("peer"):
            with ExitStack() as st:
                NCV = 4
                stg = [sb("cv_s%d" % i, [128, 4096], F32, st) for i in range(NCV)]
                stb = [sb("cv_b%d" % i, [128, 4096], BF16, st) for i in range(NCV)]
                Bs = [Buf("cvs%d" % i) for i in range(NCV)]
                Bb = [Buf("cvb%d" % i) for i in range(NCV)]
                jobs = []
                for g in range(32):
                    jobs.append((uT_d[g].rearrange("p k e -> p (k e)"), uTb_d[g].rearrange("p k e -> p (k e)")))
                    jobs.append((pv_d[g].rearrange("p j d -> p (j d)"), pvb_d[g].rearrange("p j d -> p (j d)")))
                for g in range(4):
                    jobs.append((wpq_d[g].rearrange("p k e -> p (k e)"), wpqb_d[g].rearrange("p k e -> p (k e)")))
                for n, (src, dst) in enumerate(jobs):
                    i = n % NCV
                    S.dma("sp", stg[i][:], src, w=[Bs[i]], key="cvl%d" % i)
                    ce = ("dve", "act", "pool")[n % 3]
                    if ce == "act":
                        S.op("act", lambda e, i=i: e.activation(out=stb[i][:], in_=stg[i][:], func=AF.Copy), r=[Bs[i]], w=[Bb[i]])
                    else:
                        S.op(ce, lambda e, i=i: e.tensor_copy(out=stb[i][:], in_=stg[i][:]), r=[Bs[i]], w=[Bb[i]])
                    S.dma("pool", dst, stb[i][:], r=[Bb[i]], key="cvs%d" % i)
                S.barrier()
        B_scr = Buf("scratch_tables")

        for b in range(nb):
            try:
                with ExitStack() as sq:
                    _sequence(nc, S, sq, b, locals())
            except _Stop:
                pass
        S.barrier()
    return nc, dbg_d


def _sequence(nc, S, sq, b, G):
    sb = G["sb"]
    banks, bbuf, Ring, dump, active = G["banks"], G["bbuf"], G["Ring"], G["dump"], G["active"]
    cst, cstb, identb, Mcb, Mwb, blkf, onesf = (G[k] for k in ("cst", "cstb", "identb", "Mcb", "Mwb", "blkf", "onesf"))
    B_cst = G["B_cst"]
    x_d, out_d, x1s_d = G["x_d"], G["out_d"], G["x1s_d"]
    ring = Ring(range(8))

    def T_(name, shape, dt=F32, st=sq):
        return sb("b%d_%s" % (b, name), shape, dt, st)

    def bcast_row(dst, src_row, bufs, key):
        S.dma("sp", dst, src_row.partition_broadcast(128), w=bufs, key=key)

    def ada_round(pieces, outs, tag):
        with ExitStack() as st:
            res = _ada_round(pieces, st, tag, outs)
            S.barrier()
        return res

    def _ada_round(pieces, st, tag, outs):
        csr = T_("csr" + tag, [128, 8, 128], BF16, st)
        cs = T_("cs" + tag, [128, 8], F32, st)
        B_cs = Buf("cs")
        S.dma("sp", cs[:], G["cT_d"][:, b, :], w=[B_cs], key="small")
        S.op("act", lambda e: e.activation(out=cs[:], in_=cs[:], func=AF.Silu), r=[B_cs], w=[B_cs])
        S.op("dve", lambda e: e.tensor_copy(out=csr[:], in_=cs[:].unsqueeze(2).to_broadcast([128, 8, 128])), r=[B_cs], w=[B_cs])
        res = {}
        wst = [T_("adaw%s%d" % (tag, i), [128, 8, 512], F32, st) for i in range(2)]
        wbf = [T_("adab%s%d" % (tag, i), [128, 8, 512], BF16, st) for i in range(2)]
        Bw = [Buf("adaw%d" % i) for i in range(2)]
        Bwb = [Buf("adawb%d" % i) for i in range(2)]
        for n, j in enumerate(pieces):
            i = n % 2
            S.dma("sp", wst[i][:], G["wada_d"][j], w=[Bw[i]], key="adaw%d" % i)
            S.op("pool", lambda e, i=i: e.tensor_copy(out=wbf[i][:], in_=wst[i][:]), r=[Bw[i]], w=[Bwb[i]])
            bk, Bk = ring.next()
            for k in range(8):
                S.op("pe", lambda e, k=k, i=i, bk=bk: e.matmul(bk[:], lhsT=csr[:, k, :], rhs=wbf[i][:, k, :], start=(k == 0), stop=(k == 7)),
                     r=[B_cs, Bwb[i]], w=[Bk])
            o = outs[j]
            Bo = Buf("mod%d" % j)
            bcast_row(o[:], G["bada_d"][0:1, j * 512:(j + 1) * 512], [Bo], "small")
            S.op("dve", lambda e, bk=bk, o=o: e.tensor_tensor(out=o[:], in0=bk[:], in1=o[:], op=ALU.add), r=[Bk, Bo], w=[Bo])
            res[j] = (o, Bo)
        return res

    def make_scale(res, j0, g_d, st, tag):
        with ExitStack() as tmp:
            gb = T_("g" + tag, [128, D], F32, tmp)
            Bg = Buf("g" + tag)
            bcast_row(gb[:], g_d[0:1, :], [Bg], "small")
            for h in range(2):
                o, Bo = res[j0 + h]
                S.op("dve", lambda e, o=o, h=h: e.scalar_tensor_tensor(out=o[:], in0=o[:], scalar=1.0, op0=ALU.add,
                                                                       in1=gb[:, h * 512:(h + 1) * 512], op1=ALU.mult),
                     r=[Bo, Bg], w=[Bo])
            S.barrier()

    def norm_to_T(xt, Bx, Asc, Ash, hT, B_hT, i, st_bufs):
        ss, rstd, h1, hb, Bs_, Bh1, Bhb = st_bufs
        S.op("act", lambda e: e.activation(out=h1[:], in_=xt[:], func=AF.Square, accum_out=ss[:]), r=[Bx], w=[Bh1, Bs_])
        S.op("dve", lambda e: e.tensor_scalar(out=rstd[:], in0=ss[:], scalar1=1.0 / D, scalar2=EPS, op0=ALU.mult, op1=ALU.add), r=[Bs_], w=[Bs_])
        S.op("act", lambda e: e.activation(out=rstd[:], in_=rstd[:], func=AF.Sqrt), r=[Bs_], w=[Bs_])
        S.op("dve", lambda e: e.reciprocal(out=rstd[:], in_=rstd[:]), r=[Bs_], w=[Bs_])
        for h in range(2):
            sl = slice(h * 512, (h + 1) * 512)
            S.op("dve", lambda e, sl=sl, h=h: e.scalar_tensor_tensor(out=h1[:, sl], in0=xt[:, sl], scalar=rstd[:, 0:1], op0=ALU.mult,
                                                                      in1=Asc[h][0][:], op1=ALU.mult),
                 r=[Bx, Bs_, Asc[h][1]], w=[Bh1])
            S.op("pool", lambda e, sl=sl, h=h: e.tensor_tensor(out=hb[:, sl], in0=h1[:, sl], in1=Ash[h][0][:], op=ALU.add),
                 r=[Bh1, Ash[h][1]], w=[Bhb])
        bk, Bk = ring.next()
        bkb = bk[:].bitcast(BF16).rearrange("p (k t) -> p k t", k=8)
        for k in range(8):
            S.op("pe", lambda e, k=k: e.transpose(bkb[:, k, :], hb[:, k * 128:(k + 1) * 128], identb), r=[Bhb, B_cst], w=[Bk])
        S.op("act", lambda e: e.activation(out=hT[:, :, i * 128:(i + 1) * 128], in_=bkb, func=AF.Copy), r=[Bk], w=[B_hT])

    mouts = {j: T_("mod%d" % j, [128, 512], F32) for j in (10, 11)}
    hT = T_("hT", [128, 8, T], BF16)
    B_hT = Buf("hT")
    smx = ExitStack()
    sq.enter_context(smx)
    o_nsaT = T_("o_nsaT", [128, 4, T], BF16, smx)
    B_onT = Buf("o_nsaT")
    o_hgT = T_("o_hgT", [128, 4, T], BF16, smx)
    B_ohT = Buf("o_hgT")
    for j in range(4, 10):
        mouts[j] = T_("mod%d" % j, [128, 512], F32, smx)

    with ExitStack() as st:
        for j in range(4):
            mouts[j] = T_("mod%d" % j, [128, 512], F32, st)
        res = ada_round(list(range(12)), mouts, "m")
        make_scale(res, 2, G["gmix_d"], st, "m")
        make_scale(res, 8, G["gffn_d"], st, "f")
        Ash = [res[0], res[1]]
        Asc = [res[2], res[3]]
        if not active("hT"):
            return None
        xr = [T_("xr%d" % i, [128, D], F32, st) for i in range(2)]
        Bxr = [Buf("xr%d" % i) for i in range(2)]
        nb_ = (T_("n_ss", [128, 1], F32, st), T_("n_rstd", [128, 1], F32, st), T_("n_h1", [128, D], F32, st), T_("n_hb", [128, D], BF16, st),
               Buf("n_ss"), Buf("n_h1"), Buf("n_hb"))
        for i in range(NT):
            j = i % 2
            S.dma("sp", xr[j][:], x_d[b, i * 128:(i + 1) * 128, :], w=[Bxr[j]], key="xr%d" % j)
            norm_to_T(xr[j], Bxr[j], Asc, Ash, hT, B_hT, i, nb_)
        S.barrier()
    dump("hT", hT[:], [B_hT], [128, 8, T], BF16)
    if not active("c1"):
        return None

    def fm_units(units, st, epilogue, nbuf=2):
        wst = [T_("wst%d" % i, [128, 8, 256], F32, st) for i in range(nbuf)]
        wbf = [T_("wbf%d" % i, [128, 8, 256], BF16, st) for i in range(nbuf)]
        Bw = [Buf("wst%d" % i) for i in range(nbuf)]
        Bwb = [Buf("wbf%d" % i) for i in range(nbuf)]
        for n, (u, ng_) in enumerate(units):
            i = n % nbuf
            S.dma("sp", wst[i][:], G["wfm_d"][u], w=[Bw[i]], key="wst%d" % i)
            S.op("pool", lambda e, i=i: e.tensor_copy(out=wbf[i][:], in_=wst[i][:]), r=[Bw[i]], w=[Bwb[i]])
            for tc in range(4):
                bl = []
                for g in range(ng_):
                    bk, Bk = ring.next()
                    for k in range(8):
                        S.op("pe", lambda e, k=k, g=g, bk=bk, i=i, tc=tc: e.matmul(bk[:], lhsT=wbf[i][:, k, g * 128:(g + 1) * 128],
                                                                               rhs=hT[:, k, tc * 512:(tc + 1) * 512], start=(k == 0), stop=(k == 7)),
                             r=[Bwb[i], B_hT], w=[Bk])
                    bl.append((bk, Bk))
                epilogue(u, tc, bl)

    def tk_units(units, st, epilogue):
        wst = [T_("twst%d" % u, [128, 8, 256], F32, st) for u in units]
        wbf = [T_("twbf%d" % u, [128, 8, 256], BF16, st) for u in units]
        Bwb = [Buf("twbf%d" % u) for u in units]
        for n, u in enumerate(units):
            S.dma("sp", wst[n][:], G["wtk_d"][u], w=[Bwb[n]], key="twst")
            S.op("pool", lambda e, n=n: e.tensor_copy(out=wbf[n][:], in_=wst[n][:]), r=[Bwb[n]], w=[Bwb[n]])
        for i in range(NT):
            for n, u in enumerate(units):
                bk, Bk = ring.next()
                ncol = 24 if u == 1 else 256
                for k in range(8):
                    S.op("pe", lambda e, k=k, bk=bk, n=n, i=i, ncol=ncol: e.matmul(bk[:, 0:ncol], lhsT=hT[:, k, i * 128:(i + 1) * 128],
                                                                                 rhs=wbf[n][:, k, 0:ncol], start=(k == 0), stop=(k == 7)),
                         r=[Bwb[n], B_hT], w=[Bk])
                epilogue(u, i, bk, Bk)

    with ExitStack() as sn:
        qT = T_("qT", [128, 4, T], BF16, sn)
        ksT = T_("ksT", [128, T], BF16, sn)
        kwT = T_("kwT", [128, T], BF16, sn)
        kcin = T_("kcin", [128, T], BF16, sn)
        vcin = T_("vcin", [128, T], BF16, sn)
        vsw = T_("vsw", [128, NT, 2, 2, 65], BF16, sn)
        gates = T_("gates", [128, NT, 24], F32, sn)
        B_q, B_ks, B_kw, B_kc, B_vc, B_vsw, B_gt = (Buf(n) for n in ("qT", "ksT", "kwT", "kcin", "vcin", "vsw", "gates"))
        with ExitStack() as st:
            rope = T_("rope", [128, 2, T], F32, st)
            B_rope = Buf("rope")
            S.dma("sp", rope[:], G["rope_d"][:, :, :], w=[B_rope], key="rope")
            t1 = [T_("rt1_%d" % i, [128, 512], F32, st) for i in range(2)]
            t2 = [T_("rt2_%d" % i, [128, 512], F32, st) for i in range(2)]
            Bt1 = [Buf("rt1") for i in range(2)]
            Bt2 = [Buf("rt2") for i in range(2)]
            cnt = [0]

            def ep_c1(u, tc, bl):
                sl = slice(tc * 512, (tc + 1) * 512)
                if u == 7:
                    S.op("act", lambda e: e.activation(out=vcin[:, sl], in_=bl[0][0][:], func=AF.Copy), r=[bl[0][1]], w=[B_vc])
                    return
                dst, Bd = {0: (qT[:, 0, sl], B_q), 1: (qT[:, 1, sl], B_q), 2: (qT[:, 2, sl], B_q), 3: (qT[:, 3, sl], B_q),
                           4: (ksT[:, sl], B_ks), 5: (kwT[:, sl], B_kw), 6: (kcin[:, sl], B_kc)}[u]
                i = cnt[0] % 2
                cnt[0] += 1
                S.op("dve", lambda e: e.tensor_tensor(out=t1[i][:], in0=bl[0][0][:], in1=rope[:, 0, sl], op=ALU.mult), r=[bl[0][1], B_rope], w=[Bt1[i]])
                S.op("dve", lambda e: e.tensor_tensor(out=t2[i][:], in0=bl[1][0][:], in1=rope[:, 1, sl], op=ALU.mult), r=[bl[1][1], B_rope], w=[Bt2[i]])
                S.op("pool", lambda e: e.tensor_tensor(out=dst, in0=t1[i][:], in1=t2[i][:], op=ALU.add), r=[Bt1[i], Bt2[i]], w=[Bd])

            fm_units([(u, 2) for u in range(7)] + [(7, 1)], st, ep_c1)

            S.op("pool", lambda e: e.memset(vsw[:, :, :, :, 64:65], 1.0), w=[B_vsw])

            def ep_t1(u, i, bk, Bk):
                if u == 0:
                    S.op("act", lambda e: e.activation(out=vsw[:, i, :, :, 0:64], in_=bk[:, 0:256].rearrange("p (a g d) -> p a g d", a=2, g=2),
                                                       func=AF.Copy), r=[Bk], w=[B_vsw])
                else:
                    S.op("act", lambda e: e.activation(out=gates[:, i, :], in_=bk[:, 0:24], func=AF.Sigmoid), r=[Bk], w=[B_gt])

            S.barrier()
        with ExitStack() as st:
            tk_units([0, 1], st, ep_t1)
            S.barrier()
        dump("qT", qT[:], [B_q], [128, 4, T], BF16)
        dump("ksT", ksT[:], [B_ks], [128, T], BF16)
        dump("vsw", vsw[:], [B_vsw], [128, NT, 2, 2, 65], BF16)
        dump("gates", gates[:], [B_gt], [128, NT, 24])
        if not active("cmp"):
            return None

        kcc = T_("kcc", [128, 128], BF16, sn)
        rhsC = T_("rhsC", [128, 2, 97], BF16, sn)
        B_kcc, B_rc = Buf("kcc"), Buf("rhsC")
        with ExitStack() as st:
            w1s = T_("w1s", [128, 32, 128], F32, st)
            w1b = [T_("w1b%d" % i, [128, 32, 128], BF16, st) for i in range(2)]
            posf = T_("posf", [128, 2, 32], F32, st)
            posb = T_("posb", [128, 2, 32], BF16, st)
            w2f = T_("w2f", [128, 320], F32, st)
            w2b = T_("w2b", [128, 320], BF16, st)
            smf = T_("smf", [128, 32], F32, st)
            hid = [T_("hid%d" % i, [128, 128], BF16, st) for i in range(4)]
            bias = T_("cbias", [128, 2], F32, st)
            B_w1s, B_w1b, B_pos, B_w2, B_sm, B_hid, B_bias = (Buf(n) for n in ("w1s", "w1b", "pos", "w2", "sm", "hid", "bias"))
            S.dma("sp", w2f[:, 0:256], G["w2k_d"][:, :], w=[B_w2], key="cmpw")
            S.dma("sp", w2f[:, 256:320], G["w2v_d"][:, :], w=[B_w2], key="cmpw")
            S.dma("sp", smf[:], G["selmap_d"][:, :], w=[B_sm], key="cmpw")
            for kv in range(2):
                S.dma("sp", posf[:, kv, :], G["pos_d"][kv], w=[B_pos], key="cmpw")
            S.op("act", lambda e: e.activation(out=w2b[:], in_=w2f[:], func=AF.Copy), r=[B_w2], w=[B_w2])
            S.op("act", lambda e: e.activation(out=posb[:], in_=posf[:], func=AF.Copy), r=[B_pos], w=[B_pos])
            S.op("pool", lambda e: e.memset(kcc[:], 0.0), w=[B_kcc])
            S.op("pool", lambda e: e.memset(rhsC[:], 0.0), w=[B_rc])
            S.op("pool", lambda e: e.memset(rhsC[:, :, 64:65], 1.0), w=[B_rc])
            for g in range(2):
                S.op("act", lambda e, g=g: e.activation(out=rhsC[:, g, 65:97], in_=smf[:], func=AF.Copy), r=[B_sm], w=[B_rc])
            for kv in range(2):
                S.dma("sp", w1s[:], G["w1_d"][kv], w=[B_w1s], key="w1s")
                S.op("pool", lambda e, kv=kv: e.tensor_copy(out=w1b[kv][:], in_=w1s[:]), r=[B_w1s], w=[B_w1b])
            for kv in range(2):
                src, Bsrc = (kcin, B_kc) if kv == 0 else (vcin, B_vc)
                bkb_, Bkb_ = ring.next()
                for j in range(32):
                    S.op("pe", lambda e, j=j, kv=kv, bkb_=bkb_: e.matmul(bkb_[:, 0:1], lhsT=w1b[kv][0:64, j, :], rhs=posb[0:64, kv, j:j + 1],
                                                                       start=(j == 0), stop=(j == 31)), r=[B_w1b, B_pos], w=[Bkb_])
                S.op("act", lambda e, kv=kv, bkb_=bkb_: e.activation(out=bias[:, kv:kv + 1], in_=bkb_[:, 0:1], func=AF.Copy), r=[Bkb_], w=[B_bias])
                for g in range(2):
                    bk, Bk = ring.next()
                    ps = slice(g * 64, (g + 1) * 64)
                    for j in range(32):
                        S.op("pe", lambda e, j=j, kv=kv, bk=bk, ps=ps, src=src: e.matmul(bk[:, 0:127], lhsT=w1b[kv][ps, j, :],
                                                                                       rhs=src[ps, j:j + 16 * 126 + 1:16], start=(j == 0), stop=(j == 31)),
                             r=[B_w1b, Bsrc], w=[Bk])
                    hh = hid[kv * 2 + g]
                    S.op("act", lambda e, hh=hh, bk=bk, kv=kv: e.activation(out=hh[:, 0:127], in_=bk[:, 0:127], func=AF.Gelu_apprx_tanh,
                                                                          bias=bias[:, kv:kv + 1]), r=[Bk, B_bias], w=[B_hid])
            bk, Bk = ring.next()
            S.op("pe", lambda e: e.matmul(bk[:, 0:127], lhsT=w2b[:, 0:128], rhs=hid[0][:, 0:127], start=True, stop=False), r=[B_w2, B_hid], w=[Bk])
            S.op("pe", lambda e: e.matmul(bk[:, 0:127], lhsT=w2b[:, 128:256], rhs=hid[1][:, 0:127], start=False, stop=True), r=[B_w2, B_hid], w=[Bk])
            S.op("act", lambda e: e.activation(out=kcc[:, 0:127], in_=bk[:, 0:127], func=AF.Copy), r=[Bk], w=[B_kcc])
            for g in range(2):
                bk2, Bk2 = ring.next()
                S.op("pe", lambda e, g=g, bk2=bk2: e.matmul(bk2[0:127, 0:64], lhsT=hid[2 + g][:, 0:127], rhs=w2b[:, 256:320], start=True, stop=True),
                     r=[B_w2, B_hid], w=[Bk2])
                S.op("act", lambda e, g=g, bk2=bk2: e.activation(out=rhsC[0:127, g, 0:64], in_=bk2[0:127, 0:64], func=AF.Copy), r=[Bk2], w=[B_rc])
            S.barrier()
        dump("kcc", kcc[:], [B_kcc], [128, 128], BF16)
        dump("rhsC", rhsC[:], [B_rc], [128, 2, 97], BF16)
        if not active("attn"):
            return None

        with ExitStack() as st:
            mcmp_f = T_("mcmp_f", [128, T], F32, st)
            mcmp = T_("mcmp", [128, T], BF16, st)
            eall_f = T_("eall_f", [128, T], F32, st)
            eall = T_("eall", [128, T], BF16, st)
            f1c = T_("f1c", [128, 2, NT, 32], F32, st)
            B_tab = Buf("attn_tables")
            S.dma("sp", mcmp_f[:], G["mcmp_d"][:, :], w=[B_tab], key="atab")
            S.dma("sp", eall_f[:], G["eall_d"][:, :], w=[B_tab], key="atab")
            S.dma("sp", f1c[:], G["f1c_d"][:, :, :, :], w=[B_tab], key="atab")
            S.op("act", lambda e: e.activation(out=mcmp[:], in_=mcmp_f[:], func=AF.Copy), r=[B_tab], w=[B_tab])
            S.op("act", lambda e: e.activation(out=eall[:], in_=eall_f[:], func=AF.Copy), r=[B_tab], w=[B_tab])
            NP = 3
            ringO = Ring([0, 1, 2])
            ringS = Ring([3, 4, 5, 6, 7])
            Pt = [T_("Pt%d" % i, [128, 4, 128], BF16, st) for i in range(NP)]
            BP = [Buf("Pt%d" % i) for i in range(NP)]
            pc = [0]
            oacc = [T_("oacc%d" % i, [128, 512], F32, st) for i in range(2)]
            Boa = [Buf("oacc%d" % i) for i in range(2)]
            oab = T_("oab", [128, 512], BF16, st)
            Boab = Buf("oab")
            sm = T_("att_small", [128, 64], F32, st)
            Bsm = Buf("att_small")
            imp = T_("imp", [128, 32], F32, st)
            scr = T_("imp_scr", [128, 32], F32, st)
            m8 = T_("imp_m8", [128, 16], F32, st)
            selb = T_("selb", [128, 32], BF16, st)
            selT = T_("selT", [128, 128], BF16, st)
            tmpo = T_("tmpo", [128, 4, 64], F32, st)
            Bimp, Bsel, BselT, Btmpo = Buf("imp"), Buf("selb"), Buf("selT"), Buf("tmpo")

            def combine(bo, Bo, i, g, br, first, oa, Boa_):
                o4 = bo[:].rearrange("p (r c) -> p r c", r=4)
                S.op("dve", lambda e: e.tensor_scalar(out=sm[:, 0:4], in0=o4[:, :, 64], scalar1=1e-30, scalar2=None, op0=ALU.max), r=[Bo], w=[Bsm])
                S.op("dve", lambda e: e.reciprocal(out=sm[:, 4:8], in_=sm[:, 0:4]), r=[Bsm], w=[Bsm])
                S.op("dve", lambda e: e.tensor_tensor(out=sm[:, 8:12], in0=sm[:, 4:8], in1=gates[:, i, g * 12 + br:g * 12 + 12:3], op=ALU.mult),
                     r=[Bsm, B_gt], w=[Bsm])
                cb = sm[:, 8:12].unsqueeze(2).to_broadcast([128, 4, 64])
                dst = oa[:, g * 256:(g + 1) * 256].rearrange("p (r c) -> p r c", r=4)
                if first:
                    S.op("dve", lambda e: e.tensor_tensor(out=dst, in0=o4[:, :, 0:64], in1=cb, op=ALU.mult), r=[Bo, Bsm], w=[Boa_])
                else:
                    S.op("dve", lambda e: e.tensor_tensor(out=tmpo[:], in0=o4[:, :, 0:64], in1=cb, op=ALU.mult), r=[Bo, Bsm], w=[Btmpo])
                    S.op("pool", lambda e: e.tensor_tensor(out=dst, in0=dst, in1=tmpo[:], op=ALU.add), r=[Btmpo, Boa_], w=[Boa_])

            def scores(kT, Bk_, ps, c, i, ncols=128):
                bk, Bk = ringS.next()
                S.op("pe", lambda e: e.matmul(bk[0:ncols, :], lhsT=kT[ps, c * 128:c * 128 + ncols], rhs=qT[ps, :, i * 128:(i + 1) * 128],
                                              start=True, stop=True), r=[Bk_, B_q], w=[Bk])
                j = pc[0] % NP
                pc[0] += 1
                S.op("act", lambda e: e.activation(out=Pt[j][0:ncols].rearrange("p r t -> p (r t)"), in_=bk[0:ncols, :], func=AF.Exp, scale=0.125),
                     r=[Bk], w=[BP[j]])
                return Pt[j], BP[j]

            for i in range(NT):
                oa, Boa_ = oacc[i % 2], Boa[i % 2]
                tsl = slice(i * 128, (i + 1) * 128)
                for g in range(2):
                    ps = slice(g * 64, (g + 1) * 64)
                    P, BPj = scores(kcc, B_kcc, ps, 0, i, ncols=127)
                    S.op("dve", lambda e, P=P: e.tensor_tensor(out=P[0:127], in0=P[0:127], in1=mcmp[0:127, tsl].unsqueeze(1).to_broadcast([127, 4, 128]),
                                                          op=ALU.mult), r=[BPj, B_tab], w=[BPj])
                    bo, Bo = ringO.next()
                    o4 = bo[:].rearrange("p (r c) -> p r c", r=4)
                    for r_ in range(4):
                        S.op("pe", lambda e, r_=r_, P=P, o4=o4: e.matmul(o4[:, r_, 0:97], lhsT=P[0:127, r_, :], rhs=rhsC[0:127, g, :], start=True, stop=True),
                             r=[BPj, B_rc], w=[Bo])
                    combine(bo, Bo, i, g, 0, True, oa, Boa_)
                    if i >= 8:
                        S.op("dve", lambda e, o4=o4: e.tensor_scalar(out=imp[:], in0=o4[:, 0, 65:97], scalar1=sm[:, 4:5], scalar2=None, op0=ALU.mult),
                             r=[Bo, Bsm], w=[Bimp])
                        for r_ in range(1, 4):
                            S.op("dve", lambda e, r_=r_, o4=o4: e.scalar_tensor_tensor(out=imp[:], in0=o4[:, r_, 65:97], scalar=sm[:, 4 + r_:5 + r_], op0=ALU.mult,
                                                                                 in1=imp[:], op1=ALU.add), r=[Bo, Bsm, Bimp], w=[Bimp])
                        S.op("dve", lambda e: e.tensor_tensor(out=imp[:], in0=imp[:], in1=f1c[:, 0, i, :], op=ALU.add), r=[Bimp, B_tab], w=[Bimp])
                        S.op("dve", lambda e: e.tensor_tensor(out=imp[:], in0=imp[:], in1=f1c[:, 1, i, :], op=ALU.mult), r=[Bimp, B_tab], w=[Bimp])
                        S.op("dve", lambda e: e.tensor_scalar(out=imp[:], in0=imp[:], scalar1=-1.0, scalar2=None, op0=ALU.add), r=[Bimp], w=[Bimp])
                        S.op("dve", lambda e: e.max(out=m8[:, 0:8], in_=imp[:]), r=[Bimp], w=[Bimp])
                        S.op("dve", lambda e: e.match_replace(out=scr[:], in_to_replace=m8[:, 0:8], in_values=imp[:], imm_value=-2.0), r=[Bimp], w=[Bimp])
                        S.op("dve", lambda e: e.max(out=m8[:, 8:16], in_=scr[:]), r=[Bimp], w=[Bimp])
                        S.op("dve", lambda e: e.tensor_scalar(out=selb[:], in0=imp[:], scalar1=m8[:, 15:16], scalar2=None, op0=ALU.is_ge), r=[Bimp], w=[Bsel])
                        bt, Bt = ringS.next()
                        btb = bt[:].bitcast(BF16)
                        S.op("pe", lambda e, btb=btb: e.transpose(btb[0:32, 0:128], selb[:], identb), r=[Bsel, B_cst], w=[Bt])
                        S.op("act", lambda e, btb=btb: e.activation(out=selT[0:32, :], in_=btb[0:32, 0:128], func=AF.Copy), r=[Bt], w=[BselT])
                    for br, kT, Bk_, sw, clo in ((1, ksT, B_ks, 0, 0), (2, kwT, B_kw, 1, max(0, i - 4))):
                        bo, Bo = ringO.next()
                        o4 = bo[:].rearrange("p (r c) -> p r c", r=4)
                        pend = []

                        def flush_pv():
                            P_, BPj_, c_ = pend.pop(0)
                            for r_ in range(4):
                                S.op("pe", lambda e, r_=r_: e.matmul(o4[:, r_, 0:65], lhsT=P_[:, r_, :], rhs=vsw[:, c_, sw, g, :], start=(c_ == clo and r_ == 0), stop=(c_ == i and r_ == 3)),
                                     r=[BPj_, B_vsw], w=[Bo])

                        for c in range(clo, i + 1):
                            P, BPj = scores(kT, Bk_, ps, c, i)
                            if br == 1 and i >= 8:
                                bm, Bm = ringS.next()
                                S.op("pe", lambda e, bm=bm, c=c: e.matmul(bm[:, 0:128], lhsT=eall[0:32, c * 128:(c + 1) * 128], rhs=selT[0:32, :], start=True, stop=True),
                                     r=[B_tab, BselT], w=[Bm])
                                S.op("dve", lambda e, P=P, bm=bm: e.tensor_tensor(out=P[:], in0=P[:], in1=bm[:, 0:128].unsqueeze(1).to_broadcast([128, 4, 128]), op=ALU.mult),
                                     r=[BPj, Bm], w=[BPj])
                            msk = None
                            if c == i:
                                msk = Mcb
                            elif br == 2 and c == i - 4:
                                msk = Mwb
                            if msk is not None:
                                S.op("pool", lambda e, P=P, msk=msk: e.tensor_tensor(out=P[:], in0=P[:], in1=msk.unsqueeze(1).to_broadcast([128, 4, 128]), op=ALU.mult),
                                     r=[BPj, B_cst], w=[BPj])
                            pend.append((P, BPj, c))
                            if len(pend) > 1:
                                flush_pv()
                        while pend:
                            flush_pv()
                        combine(bo, Bo, i, g, br, False, oa, Boa_)
                S.op("act", lambda e, oa=oa: e.activation(out=oab[:], in_=oa[:], func=AF.Copy), r=[Boa_], w=[Boab])
                bt, Bt = ringS.next()
                btb = bt[:].bitcast(BF16).rearrange("p (k t) -> p k t", k=8)
                for k in range(4):
                    S.op("pe", lambda e, k=k, btb=btb: e.transpose(btb[:, k, :], oab[:, k * 128:(k + 1) * 128], identb), r=[Boab, B_cst], w=[Bt])
                S.op("act", lambda e, btb=btb: e.activation(out=o_nsaT[:, :, tsl], in_=btb[:, 0:4, :], func=AF.Copy), r=[Bt], w=[B_onT])
            S.barrier()
    dump("o_nsaT", o_nsaT[:], [B_onT], [128, 4, T], BF16)
    if not active("c2"):
        return None
    gtf = _sequence2(nc, S, sq, b, G, locals())
    if gtf is None:
        return None
    smx.close()
    _peer(nc, S, sq, b, G, locals(), gtf)


def _sequence2(nc, S, sq, b, G, L):
    sb = G["sb"]
    Ring, dump, active = G["Ring"], G["dump"], G["active"]
    cst, cstb, identb, Mcb, Mwb, blkf, onesf = (G[k] for k in ("cst", "cstb", "identb", "Mcb", "Mwb", "blkf", "onesf"))
    B_cst = G["B_cst"]
    x_d, out_d, x1s_d = G["x_d"], G["out_d"], G["x1s_d"]
    T_, ring, fm_units, tk_units = L["T_"], L["ring"], L["fm_units"], L["tk_units"]
    hT, B_hT, o_nsaT, B_onT = L["hT"], L["B_hT"], L["o_nsaT"], L["B_onT"]
    norm_to_T, ada_round, make_scale, bcast_row = L["norm_to_T"], L["ada_round"], L["make_scale"], L["bcast_row"]
    banks, bbuf = G["banks"], G["bbuf"]

    smx = L["smx"]
    o_hgT, B_ohT, res2 = L["o_hgT"], L["B_ohT"], L["res"]
    with ExitStack() as sh:
        qdec = T_("qdec", [128, 4, T], BF16, sh)
        kinv = T_("kinv", [128, 4, T], BF16, sh)
        sgt = T_("sgt", [128, 4, T], BF16, sh)
        vtok = T_("vtok", [128, NT, 512], BF16, sh)
        ebend = T_("ebend", [128, 4, 64], F32, sh)
        lb = T_("lb", [128, 12], F32, sh)
        lbl = T_("lbl", [128, 4, 2], F32, sh)
        ngt = T_("ngt", [128, 1], F32, sh)
        B_qd, B_ki, B_sg, B_vt, B_eb, B_lb = (Buf(n) for n in ("qdec", "kinv", "sgt", "vtok", "ebend", "lb"))
        S.dma("sp", lbl[:], G["lbl_d"][:, :, :], w=[B_lb], key="small")
        S.dma("sp", ngt[:], G["ng_d"][:, :], w=[B_lb], key="small")
        S.op("dve", lambda e: e.tensor_tensor(out=lb[:, 8:12], in0=lbl[:, :, 0], in1=lbl[:, :, 1], op=ALU.subtract), r=[B_lb], w=[B_lb])
        S.op("act", lambda e: e.activation(out=lb[:, 0:4], in_=lb[:, 8:12], func=AF.Sigmoid), r=[B_lb], w=[B_lb])
        S.op("dve", lambda e: e.tensor_scalar(out=lb[:, 4:8], in0=lb[:, 0:4], scalar1=-1.0, scalar2=1.0, op0=ALU.mult, op1=ALU.add), r=[B_lb], w=[B_lb])
        with ExitStack() as st:
            scanm = T_("scanm", [128, 512], F32, st)
            B_sc = Buf("scanm")
            S.dma("sp", scanm[:], G["scanm_d"][:, :], w=[B_sc], key="small")
            tf = [T_("hg_t%d" % i, [128, 512], F32, st) for i in range(6)]
            Btf = [Buf("hg_t%d" % i) for i in range(6)]

            def ep_c2(u, tc, bl):
                sl = slice(tc * 512, (tc + 1) * 512)
                if u >= 12:
                    for g in range(2):
                        h = (u - 12) * 2 + g
                        S.op("act", lambda e, h=h, g=g: e.activation(out=sgt[:, h, sl], in_=bl[g][0][:], func=AF.Silu), r=[bl[g][1]], w=[B_sg])
                    return
                h = u - 8
                (bq, Bq), (bf, Bf) = bl
                f, lf, bb, eb, en, om = tf
                S.op("act", lambda e: e.activation(out=f[:], in_=bf[:], func=AF.Sigmoid), r=[Bf], w=[Btf[0]])
                S.op("dve", lambda e: e.tensor_scalar(out=f[:], in0=f[:], scalar1=lb[:, 4 + h:5 + h], scalar2=lb[:, h:h + 1], op0=ALU.mult, op1=ALU.add),
                     r=[Btf[0], B_lb], w=[Btf[0]])
                S.op("act", lambda e: e.activation(out=lf[:], in_=f[:], func=AF.Ln), r=[Btf[0]], w=[Btf[1]])
                S.op("dve", lambda e: e.tensor_tensor_scan(out=bb[:], data0=scanm[:], data1=lf[:], initial=0.0, op0=ALU.mult, op1=ALU.add),
                     r=[Btf[1], B_sc], w=[Btf[2]])
                S.op("act", lambda e: e.activation(out=eb[:], in_=bb[:], func=AF.Exp), r=[Btf[2]], w=[Btf[3]])
                S.op("act", lambda e: e.activation(out=en[:], in_=bb[:], func=AF.Exp, scale=-1.0), r=[Btf[2]], w=[Btf[4]])
                S.op("pool", lambda e: e.tensor_copy(out=ebend[:, h, tc * 16:(tc + 1) * 16], in_=eb[:, 31:512:32]), r=[Btf[3]], w=[B_eb])
                S.op("pool", lambda e: e.tensor_scalar(out=om[:], in0=f[:], scalar1=-1.0, scalar2=1.0, op0=ALU.mult, op1=ALU.add), r=[Btf[0]], w=[Btf[5]])
                S.op("dve", lambda e: e.tensor_tensor(out=kinv[:, h, sl], in0=om[:], in1=en[:], op=ALU.mult), r=[Btf[5], Btf[4]], w=[B_ki])
                S.op("act", lambda e: e.activation(out=lf[:], in_=bq[:], func=AF.Silu), r=[Bq, Btf[1]], w=[Btf[1]])
                S.op("dve", lambda e: e.tensor_tensor(out=qdec[:, h, sl], in0=lf[:], in1=eb[:], op=ALU.mult), r=[Btf[1], Btf[3]], w=[B_qd])

            fm_units([(u, 2) for u in range(8, 14)], st, ep_c2, nbuf=1)
            S.barrier()
        with ExitStack() as st:

            def ep_t2(u, i, bk, Bk):
                S.op("act", lambda e: e.activation(out=vtok[:, i, (u - 2) * 256:(u - 1) * 256], in_=bk[:, 0:256], func=AF.Copy), r=[Bk], w=[B_vt])

            tk_units([2, 3], st, ep_t2)
            S.barrier()
        dump("qdec", qdec[:], [B_qd], [128, 4, T], BF16)
        dump("kinv", kinv[:], [B_ki], [128, 4, T], BF16)
        dump("ebend", ebend[:], [B_eb], [128, 4, 64])
        dump("vtok", vtok[:], [B_vt], [128, NT, 512], BF16)
        if not active("hgrn"):
            return None

        with ExitStack() as st:
            Sf = T_("Sf", [128, 4, 128], F32, st)
            Sb = T_("Sb", [128, 4, 128], BF16, st)
            Tt = T_("Tt", [128, 4, 128], F32, st)
            kit = [T_("kit%d" % i, [128, 4, 128], BF16, st) for i in range(2)]
            kitz = [T_("kitz%d" % i, [128, 4, 128], BF16, st) for i in range(2)]
            Bkitz = [Buf("kitz%d" % i) for i in range(2)]
            At = [T_("At%d" % i, [128, 4, 128], BF16, st) for i in range(2)]
            sqf = T_("hsq", [128, 512], F32, st)
            rsd = T_("hrsd", [128, 512], F32, st)
            o1 = T_("ho1", [128, 512], F32, st)
            B_S, B_Sb, B_Tt, B_sq, B_rs, B_o1 = (Buf(n) for n in ("Sf", "Sb", "Tt", "hsq", "hrsd", "ho1"))
            Bkit = [Buf("kit%d" % i) for i in range(2)]
            BAt = [Buf("At%d" % i) for i in range(2)]
            S.op("pool", lambda e: e.memset(Sf[:], 0.0), w=[B_S])
            S.op("pool", lambda e: e.memset(Sb[:], 0.0), w=[B_Sb])
            obank = [0, 1, 2, 3]
            r2 = Ring([4, 5, 6, 7])
            for i in range(DBG.get("hg_tiles", NT)):
                tsl = slice(i * 128, (i + 1) * 128)
                j = i % 2
                bt, Bt = r2.next()
                btb = bt[:].bitcast(BF16).rearrange("p (k t) -> p k t", k=8)
                for h in range(4):
                    S.op("pe", lambda e, h=h, btb=btb: e.transpose(btb[:, h, :], kinv[:, h, tsl], identb), r=[B_ki, B_cst], w=[Bt])
                S.op("act", lambda e, btb=btb, j=j: e.activation(out=kit[j][:], in_=btb[:, 0:4, :], func=AF.Copy), r=[Bt], w=[Bkit[j]])
                S.op("dve", lambda e, j=j: e.tensor_scalar(out=kitz[j][:], in0=kit[j][:], scalar1=cst[:, 351:352], scalar2=None, op0=ALU.mult),
                     r=[Bkit[j], B_cst], w=[Bkitz[j]])
                ba, Ba = r2.next()
                ba4 = ba[:].rearrange("p (h t) -> p h t", h=4)
                for h in range(4):
                    S.op("pe", lambda e, h=h, ba4=ba4: e.matmul(ba4[:, h, :], lhsT=kinv[:, h, tsl], rhs=qdec[:, h, tsl], start=True, stop=True),
                         r=[B_ki, B_qd], w=[Ba])
                S.op("dve", lambda e, ba4=ba4, j=j: e.tensor_tensor(out=At[j][:], in0=ba4, in1=blkf.unsqueeze(1).to_broadcast([128, 4, 128]), op=ALU.mult),
                     r=[Ba, B_cst], w=[BAt[j]])
                for sub in range(DBG.get("hg_subs", 4)):
                    c = 4 * i + sub
                    psl = slice(sub * 32, (sub + 1) * 32) if sub < 3 else slice(64, 128)
                    asl = slice(sub * 32, (sub + 1) * 32)
                    kt_, Bkt_ = (kit[j], Bkit[j]) if sub < 3 else (kitz[j], Bkitz[j])
                    csl = slice(c * 32, (c + 1) * 32)
                    osl = slice((c % 16) * 32, (c % 16) * 32 + 32)
                    for h in range(4):
                        ob, Bob = banks[obank[h]], bbuf[obank[h]]
                        S.op("pe", lambda e, h=h, ob=ob: e.matmul(ob[:, osl], lhsT=vtok[psl, i, h * 128:(h + 1) * 128], rhs=At[j][psl, h, asl], start=True, stop=False),
                             r=[B_vt, BAt[j]], w=[Bob])
                        S.op("pe", lambda e, h=h, ob=ob: e.matmul(ob[:, osl], lhsT=Sb[:, h, :], rhs=qdec[:, h, csl], start=False, stop=True),
                             r=[B_Sb, B_qd], w=[Bob])
                    bu, Bu = r2.next()
                    bu4 = bu[:].rearrange("p (h t) -> p h t", h=4)
                    for h in range(4):
                        S.op("pe", lambda e, h=h, bu4=bu4: e.matmul(bu4[:, h, :], lhsT=kt_[psl, h, :], rhs=vtok[psl, i, h * 128:(h + 1) * 128], start=True, stop=True),
                             r=[Bkt_, B_vt], w=[Bu])
                    S.op("dve", lambda e, bu4=bu4: e.tensor_tensor(out=Tt[:], in0=bu4, in1=Sf[:], op=ALU.add), r=[Bu, B_S], w=[B_Tt])
                    S.op("dve", lambda e, c=c: e.tensor_tensor(out=Sf[:], in0=Tt[:], in1=ebend[:, :, c:c + 1].to_broadcast([128, 4, 128]), op=ALU.mult),
                         r=[B_Tt, B_eb], w=[B_S])
                    S.op("act", lambda e: e.activation(out=Sb[:], in_=Sf[:], func=AF.Copy), r=[B_S], w=[B_Sb])
                if i % 4 == 3 and DBG.get("hg_final", True):
                    span = slice((i // 4) * 512, (i // 4 + 1) * 512)
                    for h in range(4):
                        ob, Bob = banks[obank[h]], bbuf[obank[h]]
                        S.op("act", lambda e, ob=ob: e.activation(out=sqf[:], in_=ob[:], func=AF.Square), r=[Bob], w=[B_sq])
                        bm, Bm = r2.next()
                        S.op("pe", lambda e, bm=bm: e.matmul(bm[:], lhsT=onesf, rhs=sqf[:], start=True, stop=True), r=[B_sq, B_cst], w=[Bm])
                        S.op("dve", lambda e, bm=bm: e.tensor_scalar(out=rsd[:], in0=bm[:], scalar1=EPS, scalar2=None, op0=ALU.add), r=[Bm], w=[B_rs])
                        S.op("act", lambda e: e.activation(out=rsd[:], in_=rsd[:], func=AF.Sqrt), r=[B_rs], w=[B_rs])
                        S.op("dve", lambda e: e.reciprocal(out=rsd[:], in_=rsd[:]), r=[B_rs], w=[B_rs])
                        S.op("dve", lambda e, ob=ob: e.tensor_tensor(out=o1[:], in0=ob[:], in1=rsd[:], op=ALU.mult), r=[Bob, B_rs], w=[B_o1])
                        S.op("dve", lambda e, h=h: e.scalar_tensor_tensor(out=o_hgT[:, h, span], in0=o1[:], scalar=ngt[:, 0:1], op0=ALU.mult, in1=sgt[:, h, span], op1=ALU.mult),
                             r=[B_o1, B_lb, B_sg], w=[B_ohT])
            S.barrier()
    dump("o_hgT", o_hgT[:], [B_ohT], [128, 4, T], BF16)
    if not active("merge"):
        return None

    gtm = [res2[4], res2[5]]
    Ashf = [res2[6], res2[7]]
    Ascf = [res2[8], res2[9]]
    gtf = [res2[10], res2[11]]
    yT = T_("yT", [128, 8, T], BF16, smx)
    B_yT = Buf("yT")
    with ExitStack() as st:
        wbs = T_("wbs", [128, 4, D], F32, st)
        wbb = [T_("wbb%d" % i, [128, 4, D], BF16, st) for i in range(2)]
        B_wbs, B_wbb = Buf("wbs"), Buf("wbb")
        for j in range(2):
            S.dma("sp", wbs[:], G["wbr_d"][j], w=[B_wbs], key="wbs")
            S.op("pool", lambda e, j=j: e.tensor_copy(out=wbb[j][:], in_=wbs[:]), r=[B_wbs], w=[B_wbb])
        gaf = [T_("gaf%d" % i, [128, 512], F32, st) for i in range(2)]
        Bga = [Buf("gaf%d" % i) for i in range(2)]

        def ep_m(u, tc, bl):
            c = u - 14
            sl = slice(tc * 512, (tc + 1) * 512)
            srcs = ((o_nsaT, B_onT), (o_hgT, B_ohT))
            for j in range(2):
                S.op("act", lambda e, j=j: e.activation(out=gaf[j][:], in_=bl[j][0][:], func=AF.Sigmoid), r=[bl[j][1]], w=[Bga[j]])
                bk, Bk = ring.next()
                for k in range(4):
                    S.op("pe", lambda e, k=k, j=j, bk=bk: e.matmul(bk[:], lhsT=wbb[j][:, k, c * 128:(c + 1) * 128], rhs=srcs[j][0][:, k, sl], start=(k == 0), stop=(k == 3)),
                         r=[B_wbb, srcs[j][1]], w=[Bk])
                S.op("dve", lambda e, j=j, bk=bk: e.tensor_tensor(out=gaf[j][:], in0=gaf[j][:], in1=bk[:], op=ALU.mult), r=[Bga[j], Bk], w=[Bga[j]])
            S.op("pool", lambda e: e.tensor_tensor(out=yT[:, c, sl], in0=gaf[0][:], in1=gaf[1][:], op=ALU.add), r=[Bga[0], Bga[1]], w=[B_yT])

        fm_units([(u, 2) for u in range(14, 22)], st, ep_m)
        S.barrier()
    dump("yT", yT[:], [B_yT], [128, 8, T], BF16)

    with ExitStack() as st:
        wos = T_("wos", [128, 8, 512], F32, st)
        wob = T_("wob", [128, 8, D], BF16, st)
        B_wos, B_wob = Buf("wos"), Buf("wob")
        for hf in range(2):
            S.dma("sp", wos[:], G["wout_d"][:, :, hf * 512:(hf + 1) * 512], w=[B_wos], key="wos")
            S.op("pool", lambda e, hf=hf: e.tensor_copy(out=wob[:, :, hf * 512:(hf + 1) * 512], in_=wos[:]), r=[B_wos], w=[B_wob])
        xr = [T_("xr2_%d" % i, [128, D], F32, st) for i in range(2)]
        Bxr = [Buf("xr2_%d" % i) for i in range(2)]
        x1t = [T_("x1t%d" % i, [128, D], F32, st) for i in range(2)]
        Bx1 = [Buf("x1t%d" % i) for i in range(2)]
        tmy = T_("tmy", [128, D], F32, st)
        Btmy = Buf("tmy")
        nb_ = (T_("n2_ss", [128, 1], F32, st), T_("n2_rstd", [128, 1], F32, st), T_("n2_h1", [128, D], F32, st), T_("n2_hb", [128, D], BF16, st),
               Buf("n2_ss"), Buf("n2_h1"), Buf("n2_hb"))
        for i in range(NT):
            j = i % 2
            tsl = slice(i * 128, (i + 1) * 128)
            S.dma("sp", xr[j][:], x_d[b, tsl, :], w=[Bxr[j]], key="xr2_%d" % j)
            for hf in range(2):
                bk, Bk = ring.next()
                sl = slice(hf * 512, (hf + 1) * 512)
                for k in range(8):
                    S.op("pe", lambda e, k=k, bk=bk, sl=sl: e.matmul(bk[:], lhsT=yT[:, k, tsl], rhs=wob[:, k, sl], start=(k == 0), stop=(k == 7)),
                         r=[B_yT, B_wob], w=[Bk])
                S.op("dve", lambda e, bk=bk, sl=sl, hf=hf: e.tensor_tensor(out=tmy[:, sl], in0=bk[:], in1=gtm[hf][0][:], op=ALU.mult), r=[Bk, gtm[hf][1]], w=[Btmy])
            S.op("pool", lambda e, j=j: e.tensor_tensor(out=x1t[j][:], in0=xr[j][:], in1=tmy[:], op=ALU.add), r=[Bxr[j], Btmy], w=[Bx1[j]])
            S.dma("sp", x1s_d[b, tsl, :], x1t[j][:], r=[Bx1[j]], key="x1w%d" % j)
            norm_to_T(x1t[j], Bx1[j], Ascf, Ashf, hT, B_hT, i, nb_)
        S.barrier()
    dump("h2T", hT[:], [B_hT], [128, 8, T], BF16)
    if "x1" in G["dbg"]:
        with ExitStack() as st:
            xx = T_("dbgx1", [128, NT, D], F32, st)
            Bxx = Buf("dbgx1")
            S.dma("sp", xx[:], x1s_d[b].rearrange("(i p) d -> p i d", p=128), w=[Bxx], key="dbgx")
            dump("x1", xx[:], [Bxx], [128, NT, D])
            S.barrier()
    if not active("peer"):
        return None
    return gtf


def _peer(nc, S, sq, b, G, L, gtf):
    sb = G["sb"]
    Ring, dump = G["Ring"], G["dump"]
    identb = G["identb"]
    B_cst = G["B_cst"]
    x1s_d, out_d = G["x1s_d"], G["out_d"]
    T_, hT, B_hT, bcast_row = L["T_"], L["hT"], L["B_hT"], L["bcast_row"]
    banks, bbuf = G["banks"], G["bbuf"]
    uTb_d, pvb_d, wpqb_d = G["uTb_d"], G["pvb_d"], G["wpqb_d"]
    B_scr = G["B_scr"]
    with ExitStack() as st:
        skb = T_("skb", [128, 16, 128], BF16, st)
        gfin = T_("gfin", [128, D], F32, st)
        B_sk, B_gf = Buf("skb"), Buf("gfin")
        with ExitStack() as tmp:
            skf = T_("skf", [128, 16, 128], F32, tmp)
            S.dma("sp", skf[:], G["skT_d"][:, :, :], w=[B_sk], key="small")
            S.op("act", lambda e: e.activation(out=skb[:], in_=skf[:], func=AF.Copy), r=[B_sk], w=[B_sk])
            S.barrier()
        bcast_row(gfin[:], G["gfin_d"][0:1, :], [B_gf], "small")
        qpT = T_("qpT", [128, 16, 256], BF16, st)
        B_qp = Buf("qpT")
        ssb = [T_("ssb%d" % i, [128, 16, 128], F32, st) for i in range(2)]
        Bss = [Buf("ssb%d" % i) for i in range(2)]
        top = T_("ptop", [128, 16, 16], F32, st)
        scr = T_("pscr", [128, 128], F32, st)
        comb = T_("pcomb", [128, 8, 256], F32, st)
        scrc = T_("pscrc", [128, 256], F32, st)
        ctop = [T_("pctop%d" % i, [128, 8, 16], F32, st) for i in range(2)]
        pez = T_("pez", [128, 8, 16], F32, st)
        negc = [T_("pnegc%d" % i, [128, 8], F32, st) for i in range(2)]
        Btk = Buf("topk_scratch")
        Bct = [Buf("ctop%d" % i) for i in range(2)]
        tb = [T_("ptb%d" % i, [128, 16, 128], F32, st) for i in range(2)]
        eb = [T_("peb%d" % i, [128, 16, 128], BF16, st) for i in range(2)]
        wh = [T_("pwh0", [128, 16, 128], BF16, st)] * 2
        Btb = [Buf("ptb%d" % i) for i in range(2)]
        Beb = [Buf("peb%d" % i) for i in range(2)]
        Bwh = [Buf("pwh0")] * 2
        acc = [T_("pacc%d" % i, [128, 2, 4096], BF16, st) for i in range(2)]
        Bacc = [[Buf("pacc%d_%d" % (i, t)) for t in range(2)] for i in range(2)]
        ub = [T_("pub%d" % i, [128, 8, 512], BF16, st) for i in range(2)]
        vb = [T_("pvb%d" % i, [128, 4, D], BF16, st) for i in range(2)]
        Bub = [Buf("pub%d" % i) for i in range(2)]
        Bvb = [Buf("pvb%d" % i) for i in range(2)]
        ga = [T_("pga%d" % i, [128, 512], BF16, st) for i in range(3)]
        wa = [T_("pwa%d" % i, [128, 512], BF16, st) for i in range(3)]
        waT = [T_("pwaT%d" % i, [128, 4, 128], BF16, st) for i in range(3)]
        Bga = [Buf("pga%d" % i) for i in range(3)]
        Bwa = [Buf("pwa%d" % i) for i in range(3)]
        BwaT = [Buf("pwaT%d" % i) for i in range(3)]
        x1t = T_("px1", [128, D], F32, st)
        xo = x1t
        ptmp = T_("ptmp", [128, D], F32, st)
        pss = T_("pss", [128, 2], F32, st)
        Bx1 = Buf("px1")
        Bxo, Bpt, Bpss = Bx1, Buf("ptmp"), Buf("pss")
        r2 = Ring([4, 5, 6, 7])
        rA = Ring([4, 5])
        ucnt = [0]
        mcnt = [0]
        wcnt = [0]

        for st_ in range(T // 256):
            n0 = st_ * 256
            for pi in range(4):
                j = ucnt[0] % 2
                ucnt[0] += 1
                S.dma("sp", ub[j][:], wpqb_d[pi], r=[B_scr], w=[Bub[j]], key="pub%d" % j)
                for gg in range(4):
                    bk, Bk = r2.next()
                    for k in range(8):
                        S.op("pe", lambda e, k=k, gg=gg, bk=bk, j=j: e.matmul(bk[:, 0:256], lhsT=ub[j][:, k, gg * 128:(gg + 1) * 128], rhs=hT[:, k, n0:n0 + 256],
                                                                             start=(k == 0), stop=(k == 7)), r=[Bub[j], B_hT], w=[Bk])
                    S.op("act", lambda e, gg=gg, bk=bk, pi=pi: e.activation(out=qpT[:, pi * 4 + gg, :], in_=bk[:, 0:256], func=AF.Copy), r=[Bk], w=[B_qp])
            for tt in range(2):
                for q4 in range(4):
                    bk, Bk = r2.next()
                    for hh in range(4):
                        hp = q4 * 4 + hh
                        S.op("pe", lambda e, hp=hp, hh=hh, bk=bk: e.matmul(bk[:, hh * 128:(hh + 1) * 128], lhsT=qpT[:, hp, tt * 128:(tt + 1) * 128], rhs=skb[:, hp, :],
                                                                          start=True, stop=True), r=[B_qp, B_sk], w=[Bk])
                    S.op("act", lambda e, q4=q4, bk=bk: e.activation(out=ssb[tt][:, q4 * 4:(q4 + 1) * 4, :], in_=bk[:].rearrange("p (a k) -> p a k", a=4), func=AF.Copy),
                         r=[Bk], w=[Bss[tt]])
                for hp in range(16):
                    S.op("dve", lambda e, hp=hp: e.max(out=top[:, hp, 0:8], in_=ssb[tt][:, hp, :]), r=[Bss[tt]], w=[Btk])
                    S.op("dve", lambda e, hp=hp: e.match_replace(out=scr[:], in_to_replace=top[:, hp, 0:8], in_values=ssb[tt][:, hp, :], imm_value=-1e30), r=[Bss[tt], Btk], w=[Btk])
                    S.op("dve", lambda e, hp=hp: e.max(out=top[:, hp, 8:16], in_=scr[:]), r=[Btk], w=[Btk])
                t4 = top[:].rearrange("p (h q) r -> p h q r", q=2)
                S.op("dve", lambda e, t4=t4: e.tensor_tensor(out=comb[:].rearrange("p h (a c) -> p h a c", a=16), in0=t4[:, :, 0, :].unsqueeze(3).to_broadcast([128, 8, 16, 16]),
                                                          in1=t4[:, :, 1, :].unsqueeze(2).to_broadcast([128, 8, 16, 16]), op=ALU.add), r=[Btk], w=[Btk])
                for h in range(8):
                    S.op("dve", lambda e, h=h: e.max(out=ctop[tt][:, h, 0:8], in_=comb[:, h, :]), r=[Btk], w=[Bct[tt]])
                    S.op("dve", lambda e, h=h: e.match_replace(out=scrc[:], in_to_replace=ctop[tt][:, h, 0:8], in_values=comb[:, h, :], imm_value=-1e30), r=[Btk, Bct[tt]], w=[Btk])
                    S.op("dve", lambda e, h=h: e.max(out=ctop[tt][:, h, 8:16], in_=scrc[:]), r=[Btk], w=[Bct[tt]])
                S.op("dve", lambda e: e.tensor_tensor(out=pez[:], in0=ctop[tt][:], in1=ctop[tt][:, :, 0:1].to_broadcast([128, 8, 16]), op=ALU.subtract), r=[Bct[tt]], w=[Btk])
                S.op("act", lambda e: e.activation(out=pez[:], in_=pez[:], func=AF.Exp), r=[Btk], w=[Btk])
                S.op("dve", lambda e: e.reduce_sum(out=negc[tt][:], in_=pez[:], axis=AX.X), r=[Btk], w=[Bct[tt]])
                S.op("act", lambda e: e.activation(out=negc[tt][:], in_=negc[tt][:], func=AF.Ln), r=[Bct[tt]], w=[Bct[tt]])
                S.op("dve", lambda e: e.tensor_tensor(out=negc[tt][:], in0=negc[tt][:], in1=ctop[tt][:, :, 0], op=ALU.add), r=[Bct[tt]], w=[Bct[tt]])
                S.op("dve", lambda e: e.tensor_scalar(out=negc[tt][:], in0=negc[tt][:], scalar1=-1.0, scalar2=None, op0=ALU.mult), r=[Bct[tt]], w=[Bct[tt]])

            def w_tadd(q):
                k, n = divmod(q, 32)
                tt, rem = divmod(n, 16)
                hh, h = divmod(rem, 8)
                r0 = k * 32 + hh * 16
                i = q % 2
                S.op("dve", lambda e: e.tensor_tensor(out=tb[i][:], in0=ssb[tt][:, 2 * h, r0:r0 + 16].unsqueeze(2).to_broadcast([128, 16, 128]),
                                                      in1=ssb[tt][:, 2 * h + 1, :].unsqueeze(1).to_broadcast([128, 16, 128]), op=ALU.add), r=[Bss[tt]], w=[Btb[i]])

            def w_rest(q):
                k, n = divmod(q, 32)
                tt, rem = divmod(n, 16)
                hh, h = divmod(rem, 8)
                kb = k % 2
                i = q % 2
                S.op("act", lambda e: e.activation(out=eb[i][:], in_=tb[i][:], func=AF.Exp, bias=negc[tt][:, h:h + 1]), r=[Btb[i], Bct[tt]], w=[Beb[i]])
                dst = acc[kb][:, tt, hh * 2048:(hh + 1) * 2048].rearrange("p (a c) -> p a c", a=16)
                if h == 0:
                    S.op("dve", lambda e: e.scalar_tensor_tensor(out=dst, in0=tb[i][:], scalar=ctop[tt][:, h, 15:16], op0=ALU.is_ge, in1=eb[i][:], op1=ALU.mult),
                         r=[Btb[i], Beb[i], Bct[tt]], w=[Bacc[kb][tt]])
                else:
                    S.op("dve", lambda e: e.scalar_tensor_tensor(out=wh[i][:], in0=tb[i][:], scalar=ctop[tt][:, h, 15:16], op0=ALU.is_ge, in1=eb[i][:], op1=ALU.mult),
                         r=[Btb[i], Beb[i], Bct[tt]], w=[Bwh[i]])
                    S.op("dve", lambda e: e.tensor_tensor(out=dst, in0=dst, in1=wh[i][:], op=ALU.add), r=[Bwh[i], Bacc[kb][tt]], w=[Bacc[kb][tt]])

            wq = [0]

            def w_emit(upto_q):
                while wq[0] < upto_q:
                    q = wq[0]
                    if q == 0:
                        w_tadd(0)
                    if q + 1 < 128:
                        w_tadd(q + 1)
                    w_rest(q)
                    wq[0] += 1

            mst = {}

            def mA(s_):
                k, mi = divmod(s_, 16)
                jg, tt = divmod(mi, 2)
                eg = k * 8 + jg
                if tt == 0:
                    j = ucnt[0] % 2
                    ucnt[0] += 1
                    S.dma("sp", ub[j][:], uTb_d[eg], r=[B_scr], w=[Bub[j]], key="pub%d" % j)
                    S.dma("sp", vb[j][:], pvb_d[eg], r=[B_scr], w=[Bvb[j]], key="pvb%d" % j)
                j = (ucnt[0] - 1) % 2
                bk, Bk = rA.next()
                tok = slice(n0 + tt * 128, n0 + (tt + 1) * 128)
                for kk in range(8):
                    S.op("pe", lambda e, kk=kk: e.matmul(bk[:], lhsT=hT[:, kk, tok], rhs=ub[j][:, kk, :], start=(kk == 0), stop=(kk == 7)), r=[B_hT, Bub[j]], w=[Bk])
                mst[s_] = dict(j=j, bk=bk, Bk=Bk, k=k, jg=jg, tt=tt, eg=eg, m=s_ % 3)

            def mB(s_):
                d_ = mst[s_]
                m, bk, Bk, kb, tt, jg = d_["m"], d_["bk"], d_["Bk"], d_["k"] % 2, d_["tt"], d_["jg"]
                S.op("act", lambda e: e.activation(out=ga[m][:], in_=bk[:], func=AF.Gelu_apprx_tanh), r=[Bk], w=[Bga[m]])
                S.op("dve", lambda e: e.tensor_tensor(out=wa[m][:], in0=ga[m][:], in1=acc[kb][:, tt, jg * 512:(jg + 1) * 512], op=ALU.mult), r=[Bga[m], Bacc[kb][tt]], w=[Bwa[m]])

            def mC(s_):
                d_ = mst[s_]
                m = d_["m"]
                hb_ = 6 + s_ % 2
                btb = banks[hb_][:].bitcast(BF16).rearrange("p (k t) -> p k t", k=8)[:, 0:4, :]
                for jj in range(4):
                    S.op("pe", lambda e, jj=jj: e.transpose(btb[:, jj, :], wa[m][:, jj * 128:(jj + 1) * 128], identb), r=[Bwa[m], B_cst], w=[bbuf[hb_]])
                S.op("act", lambda e: e.activation(out=waT[m][:], in_=btb, func=AF.Copy), r=[bbuf[hb_]], w=[BwaT[m]])

            def mD(s_):
                d_ = mst.pop(s_)
                m, j, tt, eg = d_["m"], d_["j"], d_["tt"], d_["eg"]
                for jj in range(4):
                    for hf in range(2):
                        yb, By = banks[tt * 2 + hf], bbuf[tt * 2 + hf]
                        S.op("pe", lambda e, jj=jj, hf=hf, yb=yb: e.matmul(yb[:], lhsT=waT[m][:, jj, :], rhs=vb[j][:, jj, hf * 512:(hf + 1) * 512],
                                                                          start=(eg == 0 and jj == 0), stop=(eg == 31 and jj == 3)), r=[BwaT[m], Bvb[j]], w=[By])

            ms = [0]

            def m_emit(nsteps):
                for _ in range(nsteps):
                    s_ = ms[0]
                    if s_ < 64:
                        mA(s_)
                        mB(s_)
                    if 0 <= s_ - 1 < 64:
                        mC(s_ - 1)
                    if 0 <= s_ - 2 < 64:
                        mD(s_ - 2)
                    ms[0] += 1

            w_emit(32)
            for k in range(4):
                for ch in range(4):
                    m_emit(4)
                    if k < 3:
                        w_emit(32 * (k + 1) + 8 * (ch + 1))
            m_emit(2)
            for tt in range(2):
                tok = slice(n0 + tt * 128, n0 + (tt + 1) * 128)
                S.dma("sp", x1t[:], x1s_d[b, tok, :], w=[Bx1], key="px1")
                for hf in range(2):
                    sl = slice(hf * 512, (hf + 1) * 512)
                    yb, By = banks[tt * 2 + hf], bbuf[tt * 2 + hf]
                    S.op("dve", lambda e, yb=yb, sl=sl, hf=hf: e.tensor_tensor(out=ptmp[:, sl], in0=yb[:], in1=gtf[hf][0][:], op=ALU.mult), r=[By, gtf[hf][1]], w=[Bpt])
                S.op("pool", lambda e: e.tensor_tensor(out=xo[:], in0=x1t[:], in1=ptmp[:], op=ALU.add), r=[Bx1, Bpt], w=[Bxo])
                S.op("act", lambda e: e.activation(out=ptmp[:], in_=xo[:], func=AF.Square, accum_out=pss[:, 0:1]), r=[Bxo, Bpt], w=[Bpt, Bpss])
                S.op("dve", lambda e: e.tensor_scalar(out=pss[:, 1:2], in0=pss[:, 0:1], scalar1=1.0 / D, scalar2=EPS, op0=ALU.mult, op1=ALU.add), r=[Bpss], w=[Bpss])
                S.op("act", lambda e: e.activation(out=pss[:, 1:2], in_=pss[:, 1:2], func=AF.Sqrt), r=[Bpss], w=[Bpss])
                S.op("dve", lambda e: e.reciprocal(out=pss[:, 1:2], in_=pss[:, 1:2]), r=[Bpss], w=[Bpss])
                S.op("dve", lambda e: e.scalar_tensor_tensor(out=xo[:], in0=xo[:], scalar=pss[:, 1:2], op0=ALU.mult, in1=gfin[:], op1=ALU.mult), r=[Bxo, Bpss, B_gf], w=[Bxo])
                S.dma("sp", out_d[b, tok, :], xo[:], r=[Bxo], key="outw")
        S.barrier()


def _sel_map():
    i = np.arange(127)[:, None]
    j = np.arange(32)[None, :]
    d = i - 4 * j
    cnt = np.minimum(d, 3) - np.maximum(d - 1, 0) + 1
    return np.clip(cnt, 0, None).astype(np.float32)


def _const_inputs():
    f32 = np.float32
    s = np.arange(128)[:, None]
    t = np.arange(128)[None, :]
    cst = np.concatenate([np.eye(128), (s <= t), (s > t), ((s // 32 == t // 32) & (s <= t)), np.full((128, 128), 1.0 / 128)], axis=1).astype(f32)
    half = 32
    inv = (np.float32(10000.0) ** (-np.arange(half, dtype=f32) / np.float32(half))).astype(f32)
    ang = (np.arange(T, dtype=f32)[:, None] * inv[None, :]).astype(f32)
    cos = np.cos(ang).astype(f32).T
    sin = np.sin(ang).astype(f32).T
    cos64 = np.concatenate([cos, cos], axis=0)
    sin64 = np.concatenate([-sin, sin], axis=0)
    rope = np.stack([np.concatenate([cos64, cos64], 0), np.concatenate([sin64, sin64], 0)], axis=1).astype(f32)
    scanm = np.ones((128, 512), f32)
    scanm[:, 0::32] = 0.0
    n = np.arange(128)[:, None]
    tt = np.arange(T)[None, :]
    mcmp = ((16 * n + 31 <= tt) & (n < 127)).astype(f32)
    tok = (np.arange(NT)[None, :] * 128 + np.arange(128)[:, None])
    jb = np.arange(32)[None, None, :]
    cur = (tok // 64)[:, :, None]
    forced = (jb == 0) | (jb == cur) | (jb == cur - 1)
    causal = (jb * 64 <= tok[:, :, None])
    f1c = np.stack([1000.0 * forced + 1.0, causal.astype(np.float64)], axis=1).astype(f32)
    eall = np.zeros((128, T), f32)
    col = np.arange(T)
    eall[(2 * (col // 128) + (col % 128) // 64), col] = 1.0
    selmap = np.zeros((128, 32), f32)
    selmap[:127] = _sel_map()
    return dict(cst=cst, rope=rope, scanm=scanm, mcmp=mcmp, f1c=f1c, eall=eall, selmap=selmap)


def _kchunk(w):
    return np.ascontiguousarray(w.reshape(8, 128, w.shape[1]).transpose(1, 0, 2))


def _weight_inputs(w_ada, b_ada, g_mix, g_ffn, w_in, cmp_pos_k, cmp_pos_v, w_ck1, w_ck2, w_cv1, w_cv2, hgrn_lb_logits, hgrn_out_norm,
                   w_branch, w_out, w_peer_q, peer_sub_keys, peer_u, peer_v, g_final):
    f32 = np.float32
    W = np.asarray(w_in[0], f32)
    sw = (np.arange(64) + 32) % 64

    def cols(base, idx):
        return base + np.asarray(idx)

    units = []
    for a in range(4):
        x = np.concatenate([cols(a * 64, np.arange(64)), cols((a + 4) * 64, np.arange(64))])
        xs = np.concatenate([cols(a * 64, sw), cols((a + 4) * 64, sw)])
        units.append((x, xs))
    for base in (768, 1024, 512):
        x = cols(base, np.arange(128))
        xs = np.concatenate([cols(base, sw), cols(base + 64, sw)])
        units.append((x, xs))
    units.append((cols(640, np.arange(128)), None))
    for h in range(4):
        units.append((cols(1304 + h * 128, np.arange(128)), cols(1816 + h * 128, np.arange(128))))
    for hp in range(2):
        units.append((cols(2840 + (2 * hp) * 128, np.arange(128)), cols(2840 + (2 * hp + 1) * 128, np.arange(128))))
    for c in range(8):
        units.append((cols(3352 + c * 128, np.arange(128)), cols(3352 + 1024 + c * 128, np.arange(128))))
    wfm = np.zeros((22, 128, 8, 256), f32)
    for u, (a, bb) in enumerate(units):
        wfm[u, :, :, 0:128] = _kchunk(W[:, a])
        if bb is not None:
            wfm[u, :, :, 128:256] = _kchunk(W[:, bb])
    wtk = np.zeros((4, 128, 8, 256), f32)
    wtk[0, :, :, 0:128] = _kchunk(W[:, 896:1024])
    wtk[0, :, :, 128:256] = _kchunk(W[:, 1152:1280])
    wtk[1, :, :, 0:24] = _kchunk(W[:, 1280:1304])
    wtk[2] = _kchunk(W[:, 2328:2584])
    wtk[3] = _kchunk(W[:, 2584:2840])
    wa = np.asarray(w_ada[0], f32)
    wada = np.ascontiguousarray(wa.reshape(8, 128, 12, 512).transpose(2, 1, 0, 3))
    w1 = np.zeros((2, 128, 32, 128), f32)
    pos = np.zeros((2, 128, 32), f32)
    for kv, (w1_, p_) in enumerate(((w_ck1, cmp_pos_k), (w_cv1, cmp_pos_v))):
        a = np.asarray(w1_[0], f32).reshape(32, 64, 128).transpose(1, 0, 2)
        w1[kv, 0:64] = a
        w1[kv, 64:128] = a
        pp = np.asarray(p_[0], f32).T
        pos[kv, 0:64] = pp
        pos[kv, 64:128] = pp
    w2k = np.zeros((128, 256), f32)
    w2k[:, 0:64] = np.asarray(w_ck2[0], f32)
    w2k[:, 128 + 64:256] = np.asarray(w_ck2[0], f32)
    lbl = np.ascontiguousarray(np.asarray(hgrn_lb_logits, f32).reshape(2, 4, 128).transpose(2, 1, 0))
    wbr = np.ascontiguousarray(np.asarray(w_branch[0], f32).reshape(2, 4, 128, D).transpose(0, 2, 1, 3))
    wpq = np.ascontiguousarray(np.asarray(w_peer_q[0], f32).reshape(8, 128, 4, 512).transpose(2, 1, 0, 3))
    skT = np.ascontiguousarray(np.asarray(peer_sub_keys[0], f32).transpose(3, 1, 0, 2).reshape(128, 16, 128))
    uT = np.ascontiguousarray(np.asarray(peer_u[0], f32).reshape(32, 512, 8, 128).transpose(0, 3, 2, 1))
    pv = np.ascontiguousarray(np.asarray(peer_v[0], f32).reshape(32, 4, 128, D).transpose(0, 2, 1, 3))
    return dict(
        wada=wada, bada=np.asarray(b_ada, f32).reshape(1, 6 * D), gmix=np.asarray(g_mix, f32).reshape(1, D),
        gffn=np.asarray(g_ffn, f32).reshape(1, D), gfin=np.asarray(g_final, f32).reshape(1, D), wfm=wfm, wtk=wtk,
        w1=w1, pos=pos, w2k=w2k, w2v=np.ascontiguousarray(np.asarray(w_cv2[0], f32)), lbl=lbl,
        ng=np.asarray(hgrn_out_norm[0], f32).reshape(128, 1), wbr=wbr, wout=_kchunk(np.asarray(w_out[0], f32)),
        wpq=wpq, skT=skT, uT=uT, pv=pv)


def core_inputs(x, c, shared, b0, nb):
    m = dict(shared)
    m["x"] = np.ascontiguousarray(np.asarray(x[b0:b0 + nb], np.float32))
    cc = np.asarray(c[b0:b0 + nb], np.float32)
    m["cT"] = np.ascontiguousarray(cc.reshape(nb, 8, 128).transpose(2, 0, 1))
    return m


def kernel(x, c, w_ada, b_ada, g_mix, g_ffn, w_in, cmp_pos_k, cmp_pos_v, w_ck1, w_ck2, w_cv1, w_cv2, hgrn_lb_logits, hgrn_out_norm,
           w_branch, w_out, w_peer_q, peer_sub_keys, peer_u, peer_v, g_final):
    x = np.asarray(x)
    B = x.shape[0]
    nb = B // NCORES
    shared = _const_inputs()
    shared.update(_weight_inputs(w_ada, b_ada, g_mix, g_ffn, w_in, cmp_pos_k, cmp_pos_v, w_ck1, w_ck2, w_cv1, w_cv2, hgrn_lb_logits,
                                 hgrn_out_norm, w_branch, w_out, w_peer_q, peer_sub_keys, peer_u, peer_v, g_final))
    nc, _ = build_nc(nb=nb)
    in_maps = [core_inputs(x, c, shared, i * nb, nb) for i in range(NCORES)]
    res = run_bass_kernel_spmd(nc, in_maps, core_ids=list(range(NCORES)))
    out = np.concatenate([np.asarray(r["out"]).reshape(nb, T, D) for r in res.results], axis=0)
    return out.astype(np.float32)
```

```python
from contextlib import ExitStack

import numpy as np
import concourse.bass as bass
import concourse.mybir as mybir
from concourse.bass_utils import run_bass_kernel_spmd

F32 = mybir.dt.float32
BF16 = mybir.dt.bfloat16
AF = mybir.ActivationFunctionType
ALU = mybir.AluOpType
AX = mybir.AxisListType

T = 2048
D = 1024
NT = T // 128
EPS = 1e-6
NCORES = 8
DBG = {}


class Buf:
    __slots__ = ("name", "w", "r")

    def __init__(self, name):
        self.name = name
        self.w = {}
        self.r = {}


class Sched:
    def __init__(self, nc, es):
        self.nc = nc
        self.es = es
        self.eng = {"pe": nc.tensor, "act": nc.scalar, "dve": nc.vector, "pool": nc.gpsimd, "sp": nc.sync}
        self.sem = {}
        self.cnt = {}
        for e in ("pe", "act", "dve", "pool"):
            self.sem["E" + e] = es.enter_context(nc.semaphore("s_" + e))
            self.cnt["E" + e] = 0
        self.wm = {e: {} for e in self.eng}
        self.nwait = 0
        self.nins = 0

    def _waits(self, eng, reads, writes):
        deps = {}
        for b in reads:
            for k, v in b.w.items():
                if deps.get(k, 0) < v:
                    deps[k] = v
        for b in writes:
            for k, v in b.w.items():
                if deps.get(k, 0) < v:
                    deps[k] = v
            for k, v in b.r.items():
                if deps.get(k, 0) < v:
                    deps[k] = v
        wm = self.wm[eng]
        e = self.eng[eng]
        for k, v in deps.items():
            if eng == "pe" and k == "Epe":
                continue
            if wm.get(k, 0) < v:
                e.wait_ge(self.sem[k], v)
                wm[k] = v
                self.nwait += 1

    def op(self, eng, fn, r=(), w=()):
        self._waits(eng, r, w)
        k = "E" + eng
        self.cnt[k] += 1
        v = self.cnt[k]
        fn(self.eng[eng]).then_inc(self.sem[k], 1)
        self.nins += 1
        for b in r:
            b.r[k] = v
        for b in w:
            b.w[k] = v

    def dma(self, q, out, in_, r=(), w=(), key="d"):
        k = "D" + key
        if k not in self.sem:
            self.sem[k] = self.es.enter_context(self.nc.semaphore("d_" + key))
            self.cnt[k] = 0
        self._waits(q, r, w)
        if self.cnt[k] > 0 and self.wm[q].get(k, 0) < self.cnt[k]:
            self.eng[q].wait_ge(self.sem[k], self.cnt[k])
            self.wm[q][k] = self.cnt[k]
        self.cnt[k] += 16
        v = self.cnt[k]
        self.eng[q].dma_start(out=out, in_=in_).then_inc(self.sem[k], 16)
        self.nins += 1
        for b in r:
            b.r[k] = v
        for b in w:
            b.w[k] = v

    def barrier(self, engines=("pe", "act", "dve", "pool", "sp")):
        for eng in engines:
            wm = self.wm[eng]
            e = self.eng[eng]
            for k, v in self.cnt.items():
                if v > 0 and wm.get(k, 0) < v and not (eng == "pe" and k == "Epe"):
                    e.wait_ge(self.sem[k], v)
                    wm[k] = v


class _Stop(Exception):
    pass


def build_nc(nb=2, upto="all", dbg=()):
    nc = bass.Bass("TRN2", target_bir_lowering=False)
    NTOK = nb * T

    def din(name, shape, dt=F32):
        return nc.dram_tensor(name, list(shape), dt, kind="ExternalInput").ap()

    x_d = din("x", [nb, T, D])
    cT_d = din("cT", [128, nb, 8])
    wada_d = din("wada", [12, 128, 8, 512])
    bada_d = din("bada", [1, 6 * D])
    gmix_d = din("gmix", [1, D])
    gffn_d = din("gffn", [1, D])
    gfin_d = din("gfin", [1, D])
    wfm_d = din("wfm", [22, 128, 8, 256])
    wtk_d = din("wtk", [4, 128, 8, 256])
    cst_d = din("cst", [128, 640])
    rope_d = din("rope", [128, 2, T])
    scanm_d = din("scanm", [128, 512])
    mcmp_d = din("mcmp", [128, T])
    f1c_d = din("f1c", [128, 2, NT, 32])
    eall_d = din("eall", [128, T])
    selmap_d = din("selmap", [128, 32])
    w1_d = din("w1", [2, 128, 32, 128])
    pos_d = din("pos", [2, 128, 32])
    w2k_d = din("w2k", [128, 256])
    w2v_d = din("w2v", [128, 64])
    lbl_d = din("lbl", [128, 4, 2])
    ng_d = din("ng", [128, 1])
    wbr_d = din("wbr", [2, 128, 4, D])
    wout_d = din("wout", [128, 8, D])
    wpq_d = din("wpq", [4, 128, 8, 512])
    skT_d = din("skT", [128, 16, 128])
    uT_d = din("uT", [32, 128, 8, 512])
    pv_d = din("pv", [32, 128, 4, D])
    out_d = nc.dram_tensor("out", [nb, T, D], F32, kind="ExternalOutput").ap()
    x1s_d = nc.dram_tensor("x1s", [nb, T, D], F32, kind="Internal").ap()
    uTb_d = nc.dram_tensor("uTb", [32, 128, 8, 512], BF16, kind="Internal").ap()
    pvb_d = nc.dram_tensor("pvb", [32, 128, 4, D], BF16, kind="Internal").ap()
    wpqb_d = nc.dram_tensor("wpqb", [4, 128, 8, 512], BF16, kind="Internal").ap()
    dbg_d = {}

    PH = ["conv", "ada1", "hT", "c1", "cmp", "attn", "c2", "hgrn", "merge", "peer", "all"]
    lim = PH.index(upto)

    def active(p):
        return PH.index(p) <= lim

    with ExitStack() as es:
        S = Sched(nc, es)

        uniq = [0]

        def sb(name, shape, dt=F32, st=es):
            uniq[0] += 1
            return st.enter_context(nc.sbuf_tensor("sb%d_%s" % (uniq[0], name), list(shape), dt))

        banks = [es.enter_context(nc.psum_tensor("bank%d" % i, [128, 512], F32)) for i in range(8)]
        bbuf = [Buf("bank%d" % i) for i in range(8)]

        class Ring:
            def __init__(self, idx):
                self.idx = list(idx)
                self.p = 0

            def next(self):
                i = self.idx[self.p % len(self.idx)]
                self.p += 1
                return banks[i], bbuf[i]

        def dump(name, ap, bufs, shape, dt=F32):
            if name not in dbg:
                return
            d = nc.dram_tensor("dbg_" + name, list(shape), dt, kind="ExternalOutput").ap()
            dbg_d[name] = d
            S.dma("sp", d, ap, r=bufs, key="dbg")

        es.enter_context(nc.Block())

        cst = sb("cst", [128, 640])
        cstb = sb("cstb", [128, 640], BF16)
        B_cst = Buf("cst")
        S.dma("sp", cst[:], cst_d[:, :], w=[B_cst], key="cst")
        S.op("act", lambda e: e.activation(out=cstb[:], in_=cst[:], func=AF.Copy), r=[B_cst], w=[B_cst])
        identb = cstb[:, 0:128]
        Mcb = cstb[:, 128:256]
        Mwb = cstb[:, 256:384]
        blkf = cst[:, 384:512]
        onesf = cst[:, 512:640]

        if active("conv") and lim >= PH.index("peer"):
            with ExitStack() as st:
                NCV = 4
                stg = [sb("cv_s%d" % i, [128, 4096], F32, st) for i in range(NCV)]
                stb = [sb("cv_b%d" % i, [128, 4096], BF16, st) for i in range(NCV)]
                Bs = [Buf("cvs%d" % i) for i in range(NCV)]
                Bb = [Buf("cvb%d" % i) for i in range(NCV)]
                jobs = []
                for g in range(32):
                    jobs.append((uT_d[g].rearrange("p k e -> p (k e)"), uTb_d[g].rearrange("p k e -> p (k e)")))
                    jobs.append((pv_d[g].rearrange("p j d -> p (j d)"), pvb_d[g].rearrange("p j d -> p (j d)")))
                for g in range(4):
                    jobs.append((wpq_d[g].rearrange("p k e -> p (k e)"), wpqb_d[g].rearrange("p k e -> p (k e)")))
                for n, (src, dst) in enumerate(jobs):
                    i = n % NCV
                    S.dma("sp", stg[i][:], src, w=[Bs[i]], key="cvl%d" % i)
                    ce = ("dve", "act", "pool")[n % 3]
                    if ce == "act":
                        S.op("act", lambda e, i=i: e.activation(out=stb[i][:], in_=stg[i][:], func=AF.Copy), r=[Bs[i]], w=[Bb[i]])
                    else:
                        S.op(ce, lambda e, i=i: e.tensor_copy(out=stb[i][:], in_=stg[i][:]), r=[Bs[i]], w=[Bb[i]])
                    S.dma("pool", dst, stb[i][:], r=[Bb[i]], key="cvs%d" % i)
                S.barrier()
        B_scr = Buf("scratch_tables")

        for b in range(nb):
            try:
                with ExitStack() as sq:
                    _sequence(nc, S, sq, b, locals())
            except _Stop:
                pass
        S.barrier()
    return nc, dbg_d


def _sequence(nc, S, sq, b, G):
    sb = G["sb"]
    banks, bbuf, Ring, dump, active = G["banks"], G["bbuf"], G["Ring"], G["dump"], G["active"]
    cst, cstb, identb, Mcb, Mwb, blkf, onesf = (G[k] for k in ("cst", "cstb", "identb", "Mcb", "Mwb", "blkf", "onesf"))
    B_cst = G["B_cst"]
    x_d, out_d, x1s_d = G["x_d"], G["out_d"], G["x1s_d"]
    ring = Ring(range(8))

    def T_(name, shape, dt=F32, st=sq):
        return sb("b%d_%s" % (b, name), shape, dt, st)

    def bcast_row(dst, src_row, bufs, key):
        S.dma("sp", dst, src_row.partition_broadcast(128), w=bufs, key=key)

    def ada_round(pieces, outs, tag):
        with ExitStack() as st:
            res = _ada_round(pieces, st, tag, outs)
            S.barrier()
        return res

    def _ada_round(pieces, st, tag, outs):
        csr = T_("csr" + tag, [128, 8, 128], BF16, st)
        cs = T_("cs" + tag, [128, 8], F32, st)
        B_cs = Buf("cs")
        S.dma("sp", cs[:], G["cT_d"][:, b, :], w=[B_cs], key="small")
        S.op("act", lambda e: e.activation(out=cs[:], in_=cs[:], func=AF.Silu), r=[B_cs], w=[B_cs])
        S.op("dve", lambda e: e.tensor_copy(out=csr[:], in_=cs[:].unsqueeze(2).to_broadcast([128, 8, 128])), r=[B_cs], w=[B_cs])
        res = {}
        wst = [T_("adaw%s%d" % (tag, i), [128, 8, 512], F32, st) for i in range(2)]
        wbf = [T_("adab%s%d" % (tag, i), [128, 8, 512], BF16, st) for i in range(2)]
        Bw = [Buf("adaw%d" % i) for i in range(2)]
        Bwb = [Buf("adawb%d" % i) for i in range(2)]
        for n, j in enumerate(pieces):
            i = n % 2
            S.dma("sp", wst[i][:], G["wada_d"][j], w=[Bw[i]], key="adaw%d" % i)
            S.op("pool", lambda e, i=i: e.tensor_copy(out=wbf[i][:], in_=wst[i][:]), r=[Bw[i]], w=[Bwb[i]])
            bk, Bk = ring.next()
            for k in range(8):
                S.op("pe", lambda e, k=k, i=i, bk=bk: e.matmul(bk[:], lhsT=csr[:, k, :], rhs=wbf[i][:, k, :], start=(k == 0), stop=(k == 7)),
                     r=[B_cs, Bwb[i]], w=[Bk])
            o = outs[j]
            Bo = Buf("mod%d" % j)
            bcast_row(o[:], G["bada_d"][0:1, j * 512:(j + 1) * 512], [Bo], "small")
            S.op("dve", lambda e, bk=bk, o=o: e.tensor_tensor(out=o[:], in0=bk[:], in1=o[:], op=ALU.add), r=[Bk, Bo], w=[Bo])
            res[j] = (o, Bo)
        return res

    def make_scale(res, j0, g_d, st, tag):
        with ExitStack() as tmp:
            gb = T_("g" + tag, [128, D], F32, tmp)
            Bg = Buf("g" + tag)
            bcast_row(gb[:], g_d[0:1, :], [Bg], "small")
            for h in range(2):
                o, Bo = res[j0 + h]
                S.op("dve", lambda e, o=o, h=h: e.scalar_tensor_tensor(out=o[:], in0=o[:], scalar=1.0, op0=ALU.add,
                                                                       in1=gb[:, h * 512:(h + 1) * 512], op1=ALU.mult),
                     r=[Bo, Bg], w=[Bo])
            S.barrier()

    def norm_to_T(xt, Bx, Asc, Ash, hT, B_hT, i, st_bufs):
        ss, rstd, h1, hb, Bs_, Bh1, Bhb = st_bufs
        S.op("act", lambda e: e.activation(out=h1[:], in_=xt[:], func=AF.Square, accum_out=ss[:]), r=[Bx], w=[Bh1, Bs_])
        S.op("dve", lambda e: e.tensor_scalar(out=rstd[:], in0=ss[:], scalar1=1.0 / D, scalar2=EPS, op0=ALU.mult, op1=ALU.add), r=[Bs_], w=[Bs_])
        S.op("act", lambda e: e.activation(out=rstd[:], in_=rstd[:], func=AF.Sqrt), r=[Bs_], w=[Bs_])
        S.op("dve", lambda e: e.reciprocal(out=rstd[:], in_=rstd[:]), r=[Bs_], w=[Bs_])
        for h in range(2):
            sl = slice(h * 512, (h + 1) * 512)
            S.op("dve", lambda e, sl=sl, h=h: e.scalar_tensor_tensor(out=h1[:, sl], in0=xt[:, sl], scalar=rstd[:, 0:1], op0=ALU.mult,
                                                                      in1=Asc[h][0][:], op1=ALU.mult),
                 r=[Bx, Bs_, Asc[h][1]], w=[Bh1])
            S.op("pool", lambda e, sl=sl, h=h: e.tensor_tensor(out=hb[:, sl], in0=h1[:, sl], in1=Ash[h][0][:], op=ALU.add),
                 r=[Bh1, Ash[h][1]], w=[Bhb])
        bk, Bk = ring.next()
        bkb = bk[:].bitcast(BF16).rearrange("p (k t) -> p k t", k=8)
        for k in range(8):
            S.op("pe", lambda e, k=k: e.transpose(bkb[:, k, :], hb[:, k * 128:(k + 1) * 128], identb), r=[Bhb, B_cst], w=[Bk])
        S.op("act", lambda e: e.activation(out=hT[:, :, i * 128:(i + 1) * 128], in_=bkb, func=AF.Copy), r=[Bk], w=[B_hT])

    mouts = {j: T_("mod%d" % j, [128, 512], F32) for j in (10, 11)}
    hT = T_("hT", [128, 8, T], BF16)
    B_hT = Buf("hT")
    smx = ExitStack()
    sq.enter_context(smx)
    o_nsaT = T_("o_nsaT", [128, 4, T], BF16, smx)
    B_onT = Buf("o_nsaT")
    o_hgT = T_("o_hgT", [128, 4, T], BF16, smx)
    B_ohT = Buf("o_hgT")
    for j in range(4, 10):
        mouts[j] = T_("mod%d" % j, [128, 512], F32, smx)

    with ExitStack() as st:
        for j in range(4):
            mouts[j] = T_("mod%d" % j, [128, 512], F32, st)
        res = ada_round(list(range(12)), mouts, "m")
        make_scale(res, 2, G["gmix_d"], st, "m")
        make_scale(res, 8, G["gffn_d"], st, "f")
        Ash = [res[0], res[1]]
        Asc = [res[2], res[3]]
        if not active("hT"):
            return None
        xr = [T_("xr%d" % i, [128, D], F32, st) for i in range(2)]
        Bxr = [Buf("xr%d" % i) for i in range(2)]
        nb_ = (T_("n_ss", [128, 1], F32, st), T_("n_rstd", [128, 1], F32, st), T_("n_h1", [128, D], F32, st), T_("n_hb", [128, D], BF16, st),
               Buf("n_ss"), Buf("n_h1"), Buf("n_hb"))
        for i in range(NT):
            j = i % 2
            S.dma("sp", xr[j][:], x_d[b, i * 128:(i + 1) * 128, :], w=[Bxr[j]], key="xr%d" % j)
            norm_to_T(xr[j], Bxr[j], Asc, Ash, hT, B_hT, i, nb_)
        S.barrier()
    dump("hT", hT[:], [B_hT], [128, 8, T], BF16)
    if not active("c1"):
        return None

    def fm_units(units, st, epilogue, nbuf=2):
        wst = [T_("wst%d" % i, [128, 8, 256], F32, st) for i in range(nbuf)]
        wbf = [T_("wbf%d" % i, [128, 8, 256], BF16, st) for i in range(nbuf)]
        Bw = [Buf("wst%d" % i) for i in range(nbuf)]
        Bwb = [Buf("wbf%d" % i) for i in range(nbuf)]
        for n, (u, ng_) in enumerate(units):
            i = n % nbuf
            S.dma("sp", wst[i][:], G["wfm_d"][u], w=[Bw[i]], key="wst%d" % i)
            S.op("pool", lambda e, i=i: e.tensor_copy(out=wbf[i][:], in_=wst[i][:]), r=[Bw[i]], w=[Bwb[i]])
            for tc in range(4):
                bl = []
                for g in range(ng_):
                    bk, Bk = ring.next()
                    for k in range(8):
                        S.op("pe", lambda e, k=k, g=g, bk=bk, i=i, tc=tc: e.matmul(bk[:], lhsT=wbf[i][:, k, g * 128:(g + 1) * 128],
                                                                               rhs=hT[:, k, tc * 512:(tc + 1) * 512], start=(k == 0), stop=(k == 7)),
                             r=[Bwb[i], B_hT], w=[Bk])
                    bl.append((bk, Bk))
                epilogue(u, tc, bl)

    def tk_units(units, st, epilogue):
        wst = [T_("twst%d" % u, [128, 8, 256], F32, st) for u in units]
        wbf = [T_("twbf%d" % u, [128, 8, 256], BF16, st) for u in units]
        Bwb = [Buf("twbf%d" % u) for u in units]
        for n, u in enumerate(units):
            S.dma("sp", wst[n][:], G["wtk_d"][u], w=[Bwb[n]], key="twst")
            S.op("pool", lambda e, n=n: e.tensor_copy(out=wbf[n][:], in_=wst[n][:]), r=[Bwb[n]], w=[Bwb[n]])
        for i in range(NT):
            for n, u in enumerate(units):
                bk, Bk = ring.next()
                ncol = 24 if u == 1 else 256
                for k in range(8):
                    S.op("pe", lambda e, k=k, bk=bk, n=n, i=i, ncol=ncol: e.matmul(bk[:, 0:ncol], lhsT=hT[:, k, i * 128:(i + 1) * 128],
                                                                                 rhs=wbf[n][:, k, 0:ncol], start=(k == 0), stop=(k == 7)),
                         r=[Bwb[n], B_hT], w=[Bk])
                epilogue(u, i, bk, Bk)

    with ExitStack() as sn:
        qT = T_("qT", [128, 4, T], BF16, sn)
        ksT = T_("ksT", [128, T], BF16, sn)
        kwT = T_("kwT", [128, T], BF16, sn)
        kcin = T_("kcin", [128, T], BF16, sn)
        vcin = T_("vcin", [128, T], BF16, sn)
        vsw = T_("vsw", [128, NT, 2, 2, 65], BF16, sn)
        gates = T_("gates", [128, NT, 24], F32, sn)
        B_q, B_ks, B_kw, B_kc, B_vc, B_vsw, B_gt = (Buf(n) for n in ("qT", "ksT", "kwT", "kcin", "vcin", "vsw", "gates"))
        with ExitStack() as st:
            rope = T_("rope", [128, 2, T], F32, st)
            B_rope = Buf("rope")
            S.dma("sp", rope[:], G["rope_d"][:, :, :], w=[B_rope], key="rope")
            t1 = [T_("rt1_%d" % i, [128, 512], F32, st) for i in range(2)]
            t2 = [T_("rt2_%d" % i, [128, 512], F32, st) for i in range(2)]
            Bt1 = [Buf("rt1") for i in range(2)]
            Bt2 = [Buf("rt2") for i in range(2)]
            cnt = [0]

            def ep_c1(u, tc, bl):
                sl = slice(tc * 512, (tc + 1) * 512)
                if u == 7:
                    S.op("act", lambda e: e.activation(out=vcin[:, sl], in_=bl[0][0][:], func=AF.Copy), r=[bl[0][1]], w=[B_vc])
                    return
                dst, Bd = {0: (qT[:, 0, sl], B_q), 1: (qT[:, 1, sl], B_q), 2: (qT[:, 2, sl], B_q), 3: (qT[:, 3, sl], B_q),
                           4: (ksT[:, sl], B_ks), 5: (kwT[:, sl], B_kw), 6: (kcin[:, sl], B_kc)}[u]
                i = cnt[0] % 2
                cnt[0] += 1
                S.op("dve", lambda e: e.tensor_tensor(out=t1[i][:], in0=bl[0][0][:], in1=rope[:, 0, sl], op=ALU.mult), r=[bl[0][1], B_rope], w=[Bt1[i]])
                S.op("dve", lambda e: e.tensor_tensor(out=t2[i][:], in0=bl[1][0][:], in1=rope[:, 1, sl], op=ALU.mult), r=[bl[1][1], B_rope], w=[Bt2[i]])
                S.op("pool", lambda e: e.tensor_tensor(out=dst, in0=t1[i][:], in1=t2[i][:], op=ALU.add), r=[Bt1[i], Bt2[i]], w=[Bd])

            fm_units([(u, 2) for u in range(7)] + [(7, 1)], st, ep_c1)

            S.op("pool", lambda e: e.memset(vsw[:, :, :, :, 64:65], 1.0), w=[B_vsw])

            def ep_t1(u, i, bk, Bk):
                if u == 0:
                    S.op("act", lambda e: e.activation(out=vsw[:, i, :, :, 0:64], in_=bk[:, 0:256].rearrange("p (a g d) -> p a g d", a=2, g=2),
                                                       func=AF.Copy), r=[Bk], w=[B_vsw])
                else:
                    S.op("act", lambda e: e.activation(out=gates[:, i, :], in_=bk[:, 0:24], func=AF.Sigmoid), r=[Bk], w=[B_gt])

            S.barrier()
        with ExitStack() as st:
            tk_units([0, 1], st, ep_t1)
            S.barrier()
        dump("qT", qT[:], [B_q], [128, 4, T], BF16)
        dump("ksT", ksT[:], [B_ks], [128, T], BF16)
        dump("vsw", vsw[:], [B_vsw], [128, NT, 2, 2, 65], BF16)
        dump("gates", gates[:], [B_gt], [128, NT, 24])
        if not active("cmp"):
            return None

        kcc = T_("kcc", [128, 128], BF16, sn)
        rhsC = T_("rhsC", [128, 2, 97], BF16, sn)
        B_kcc, B_rc = Buf("kcc"), Buf("rhsC")
        with ExitStack() as st:
            w1s = T_("w1s", [128, 32, 128], F32, st)
            w1b = [T_("w1b%d" % i, [128, 32, 128], BF16, st) for i in range(2)]
            posf = T_("posf", [128, 2, 32], F32, st)
            posb = T_("posb", [128, 2, 32], BF16, st)
            w2f = T_("w2f", [128, 320], F32, st)
            w2b = T_("w2b", [128, 320], BF16, st)
            smf = T_("smf", [128, 32], F32, st)
            hid = [T_("hid%d" % i, [128, 128], BF16, st) for i in range(4)]
            bias = T_("cbias", [128, 2], F32, st)
            B_w1s, B_w1b, B_pos, B_w2, B_sm, B_hid, B_bias = (Buf(n) for n in ("w1s", "w1b", "pos", "w2", "sm", "hid", "bias"))
            S.dma("sp", w2f[:, 0:256], G["w2k_d"][:, :], w=[B_w2], key="cmpw")
            S.dma("sp", w2f[:, 256:320], G["w2v_d"][:, :], w=[B_w2], key="cmpw")
            S.dma("sp", smf[:], G["selmap_d"][:, :], w=[B_sm], key="cmpw")
            for kv in range(2):
                S.dma("sp", posf[:, kv, :], G["pos_d"][kv], w=[B_pos], key="cmpw")
            S.op("act", lambda e: e.activation(out=w2b[:], in_=w2f[:], func=AF.Copy), r=[B_w2], w=[B_w2])
            S.op("act", lambda e: e.activation(out=posb[:], in_=posf[:], func=AF.Copy), r=[B_pos], w=[B_pos])
            S.op("pool", lambda e: e.memset(kcc[:], 0.0), w=[B_kcc])
            S.op("pool", lambda e: e.memset(rhsC[:], 0.0), w=[B_rc])
            S.op("pool", lambda e: e.memset(rhsC[:, :, 64:65], 1.0), w=[B_rc])
            for g in range(2):
                S.op("act", lambda e, g=g: e.activation(out=rhsC[:, g, 65:97], in_=smf[:], func=AF.Copy), r=[B_sm], w=[B_rc])
            for kv in range(2):
                S.dma("sp", w1s[:], G["w1_d"][kv], w=[B_w1s], key="w1s")
                S.op("pool", lambda e, kv=kv: e.tensor_copy(out=w1b[kv][:], in_=w1s[:]), r=[B_w1s], w=[B_w1b])
            for kv in range(2):
                src, Bsrc = (kcin, B_kc) if kv == 0 else (vcin, B_vc)
                bkb_, Bkb_ = ring.next()
                for j in range(32):
                    S.op("pe", lambda e, j=j, kv=kv, bkb_=bkb_: e.matmul(bkb_[:, 0:1], lhsT=w1b[kv][0:64, j, :], rhs=posb[0:64, kv, j:j + 1],
                                                                       start=(j == 0), stop=(j == 31)), r=[B_w1b, B_pos], w=[Bkb_])
                S.op("act", lambda e, kv=kv, bkb_=bkb_: e.activation(out=bias[:, kv:kv + 1], in_=bkb_[:, 0:1], func=AF.Copy), r=[Bkb_], w=[B_bias])
                for g in range(2):
                    bk, Bk = ring.next()
                    ps = slice(g * 64, (g + 1) * 64)
                    for j in range(32):
                        S.op("pe", lambda e, j=j, kv=kv, bk=bk, ps=ps, src=src: e.matmul(bk[:, 0:127], lhsT=w1b[kv][ps, j, :],
                                                                                       rhs=src[ps, j:j + 16 * 126 + 1:16], start=(j == 0), stop=(j == 31)),
                             r=[B_w1b, Bsrc], w=[Bk])
                    hh = hid[kv * 2 + g]
                    S.op("act", lambda e, hh=hh, bk=bk, kv=kv: e.activation(out=hh[:, 0:127], in_=bk[:, 0:127], func=AF.Gelu_apprx_tanh,
                                                                          bias=bias[:, kv:kv + 1]), r=[Bk, B_bias], w=[B_hid])
            bk, Bk = ring.next()
            S.op("pe", lambda e: e.matmul(bk[:, 0:127], lhsT=w2b[:, 0:128], rhs=hid[0][:, 0:127], start=True, stop=False), r=[B_w2, B_hid], w=[Bk])
            S.op("pe", lambda e: e.matmul(bk[:, 0:127], lhsT=w2b[:, 128:256], rhs=hid[1][:, 0:127], start=False, stop=True), r=[B_w2, B_hid], w=[Bk])
            S.op("act", lambda e: e.activation(out=kcc[:, 0:127], in_=bk[:, 0:127], func=AF.Copy), r=[Bk], w=[B_kcc])
            for g in range(2):
                bk2, Bk2 = ring.next()
                S.op("pe", lambda e, g=g, bk2=bk2: e.matmul(bk2[0:127, 0:64], lhsT=hid[2 + g][:, 0:127], rhs=w2b[:, 256:320], start=True, stop=True),
                     r=[B_w2, B_hid], w=[Bk2])
                S.op("act", lambda e, g=g, bk2=bk2: e.activation(out=rhsC[0:127, g, 0:64], in_=bk2[0:127, 0:64], func=AF.Copy), r=[Bk2], w=[B_rc])
            S.barrier()
        dump("kcc", kcc[:], [B_kcc], [128, 128], BF16)
        dump("rhsC", rhsC[:], [B_rc], [128, 2, 97], BF16)
        if not active("attn"):
            return None

        with ExitStack() as st:
            mcmp_f = T_("mcmp_f", [128, T], F32, st)
            mcmp = T_("mcmp", [128, T], BF16, st)
            eall_f = T_("eall_f", [128, T], F32, st)
            eall = T_("eall", [128, T], BF16, st)
            f1c = T_("f1c", [128, 2, NT, 32], F32, st)
            B_tab = Buf("attn_tables")
            S.dma("sp", mcmp_f[:], G["mcmp_d"][:, :], w=[B_tab], key="atab")
            S.dma("sp", eall_f[:], G["eall_d"][:, :], w=[B_tab], key="atab")
            S.dma("sp", f1c[:], G["f1c_d"][:, :, :, :], w=[B_tab], key="atab")
            S.op("act", lambda e: e.activation(out=mcmp[:], in_=mcmp_f[:], func=AF.Copy), r=[B_tab], w=[B_tab])
            S.op("act", lambda e: e.activation(out=eall[:], in_=eall_f[:], func=AF.Copy), r=[B_tab], w=[B_tab])
            NP = 3
            ringO = Ring([0, 1, 2])
            ringS = Ring([3, 4, 5, 6, 7])
            Pt = [T_("Pt%d" % i, [128, 4, 128], BF16, st) for i in range(NP)]
            BP = [Buf("Pt%d" % i) for i in range(NP)]
            pc = [0]
            oacc = [T_("oacc%d" % i, [128, 512], F32, st) for i in range(2)]
            Boa = [Buf("oacc%d" % i) for i in range(2)]
            oab = T_("oab", [128, 512], BF16, st)
            Boab = Buf("oab")
            sm = T_("att_small", [128, 64], F32, st)
            Bsm = Buf("att_small")
            imp = T_("imp", [128, 32], F32, st)
            scr = T_("imp_scr", [128, 32], F32, st)
            m8 = T_("imp_m8", [128, 16], F32, st)
            selb = T_("selb", [128, 32], BF16, st)
            selT = T_("selT", [128, 128], BF16, st)
            tmpo = T_("tmpo", [128, 4, 64], F32, st)
            Bimp, Bsel, BselT, Btmpo = Buf("imp"), Buf("selb"), Buf("selT"), Buf("tmpo")

            def combine(bo, Bo, i, g, br, first, oa, Boa_):
                o4 = bo[:].rearrange("p (r c) -> p r c", r=4)
                S.op("dve", lambda e: e.tensor_scalar(out=sm[:, 0:4], in0=o4[:, :, 64], scalar1=1e-30, scalar2=None, op0=ALU.max), r=[Bo], w=[Bsm])
                S.op("dve", lambda e: e.reciprocal(out=sm[:, 4:8], in_=sm[:, 0:4]), r=[Bsm], w=[Bsm])
                S.op("dve", lambda e: e.tensor_tensor(out=sm[:, 8:12], in0=sm[:, 4:8], in1=gates[:, i, g * 12 + br:g * 12 + 12:3], op=ALU.mult),
                     r=[Bsm, B_gt], w=[Bsm])
                cb = sm[:, 8:12].unsqueeze(2).to_broadcast([128, 4, 64])
                dst = oa[:, g * 256:(g + 1) * 256].rearrange("p (r c) -> p r c", r=4)
                if first:
                    S.op("dve", lambda e: e.tensor_tensor(out=dst, in0=o4[:, :, 0:64], in1=cb, op=ALU.mult), r=[Bo, Bsm], w=[Boa_])
                else:
                    S.op("dve", lambda e: e.tensor_tensor(out=tmpo[:], in0=o4[:, :, 0:64], in1=cb, op=ALU.mult), r=[Bo, Bsm], w=[Btmpo])
                    S.op("pool", lambda e: e.tensor_tensor(out=dst, in0=dst, in1=tmpo[:], op=ALU.add), r=[Btmpo, Boa_], w=[Boa_])

            def scores(kT, Bk_, ps, c, i, ncols=128):
                bk, Bk = ringS.next()
                S.op("pe", lambda e: e.matmul(bk[0:ncols, :], lhsT=kT[ps, c * 128:c * 128 + ncols], rhs=qT[ps, :, i * 128:(i + 1) * 128],
                                              start=True, stop=True), r=[Bk_, B_q], w=[Bk])
                j = pc[0] % NP
                pc[0] += 1
                S.op("act", lambda e: e.activation(out=Pt[j][0:ncols].rearrange("p r t -> p (r t)"), in_=bk[0:ncols, :], func=AF.Exp, scale=0.125),
                     r=[Bk], w=[BP[j]])
                return Pt[j], BP[j]

            for i in range(NT):
                oa, Boa_ = oacc[i % 2], Boa[i % 2]
                tsl = slice(i * 128, (i + 1) * 128)
                for g in range(2):
                    ps = slice(g * 64, (g + 1) * 64)
                    P, BPj = scores(kcc, B_kcc, ps, 0, i, ncols=127)
                    S.op("dve", lambda e, P=P: e.tensor_tensor(out=P[0:127], in0=P[0:127], in1=mcmp[0:127, tsl].unsqueeze(1).to_broadcast([127, 4, 128]),
                                                          op=ALU.mult), r=[BPj, B_tab], w=[BPj])
                    bo, Bo = ringO.next()
                    o4 = bo[:].rearrange("p (r c) -> p r c", r=4)
                    for r_ in range(4):
                        S.op("pe", lambda e, r_=r_, P=P, o4=o4: e.matmul(o4[:, r_, 0:97], lhsT=P[0:127, r_, :], rhs=rhsC[0:127, g, :], start=True, stop=True),
                             r=[BPj, B_rc], w=[Bo])
                    combine(bo, Bo, i, g, 0, True, oa, Boa_)
                    if i >= 8:
                        S.op("dve", lambda e, o4=o4: e.tensor_scalar(out=imp[:], in0=o4[:, 0, 65:97], scalar1=sm[:, 4:5], scalar2=None, op0=ALU.mult),
                             r=[Bo, Bsm], w=[Bimp])
                        for r_ in range(1, 4):
                            S.op("dve", lambda e, r_=r_, o4=o4: e.scalar_tensor_tensor(out=imp[:], in0=o4[:, r_, 65:97], scalar=sm[:, 4 + r_:5 + r_], op0=ALU.mult,
                                                                                 in1=imp[:], op1=ALU.add), r=[Bo, Bsm, Bimp], w=[Bimp])
                        S.op("dve", lambda e: e.tensor_tensor(out=imp[:], in0=imp[:], in1=f1c[:, 0, i, :], op=ALU.add), r=[Bimp, B_tab], w=[Bimp])
                        S.op("dve", lambda e: e.tensor_tensor(out=imp[:], in0=imp[:], in1=f1c[:, 1, i, :], op=ALU.mult), r=[Bimp, B_tab], w=[Bimp])
                        S.op("dve", lambda e: e.tensor_scalar(out=imp[:], in0=imp[:], scalar1=-1.0, scalar2=None, op0=ALU.add), r=[Bimp], w=[Bimp])
                        S.op("dve", lambda e: e.max(out=m8[:, 0:8], in_=imp[:]), r=[Bimp], w=[Bimp])
                        S.op("dve", lambda e: e.match_replace(out=scr[:], in_to_replace=m8[:, 0:8], in_values=imp[:], imm_value=-2.0), r=[Bimp], w=[Bimp])
                        S.op("dve", lambda e: e.max(out=m8[:, 8:16], in_=scr[:]), r=[Bimp], w=[Bimp])
                        S.op("dve", lambda e: e.tensor_scalar(out=selb[:], in0=imp[:], scalar1=m8[:, 15:16], scalar2=None, op0=ALU.is_ge), r=[Bimp], w=[Bsel])
                        bt, Bt = ringS.next()
                        btb = bt[:].bitcast(BF16)
                        S.op("pe", lambda e, btb=btb: e.transpose(btb[0:32, 0:128], selb[:], identb), r=[Bsel, B_cst], w=[Bt])
                        S.op("act", lambda e, btb=btb: e.activation(out=selT[0:32, :], in_=btb[0:32, 0:128], func=AF.Copy), r=[Bt], w=[BselT])
                    for br, kT, Bk_, sw, clo in ((1, ksT, B_ks, 0, 0), (2, kwT, B_kw, 1, max(0, i - 4))):
                        bo, Bo = ringO.next()
                        o4 = bo[:].rearrange("p (r c) -> p r c", r=4)
                        pend = []

                        def flush_pv():
                            P_, BPj_, c_ = pend.pop(0)
                            for r_ in range(4):
                                S.op("pe", lambda e, r_=r_: e.matmul(o4[:, r_, 0:65], lhsT=P_[:, r_, :], rhs=vsw[:, c_, sw, g, :], start=(c_ == clo and r_ == 0), stop=(c_ == i and r_ == 3)),
                                     r=[BPj_, B_vsw], w=[Bo])

                        for c in range(clo, i + 1):
                            P, BPj = scores(kT, Bk_, ps, c, i)
                            if br == 1 and i >= 8:
                                bm, Bm = ringS.next()
                                S.op("pe", lambda e, bm=bm, c=c: e.matmul(bm[:, 0:128], lhsT=eall[0:32, c * 128:(c + 1) * 128], rhs=selT[0:32, :], start=True, stop=True),
                                     r=[B_tab, BselT], w=[Bm])
                                S.op("dve", lambda e, P=P, bm=bm: e.tensor_tensor(out=P[:], in0=P[:], in1=bm[:, 0:128].unsqueeze(1).to_broadcast([128, 4, 128]), op=ALU.mult),
                                     r=[BPj, Bm], w=[BPj])
                            msk = None
                            if c == i:
                                msk = Mcb
                            elif br == 2 and c == i - 4:
                                msk = Mwb
                            if msk is not None:
                                S.op("pool", lambda e, P=P, msk=msk: e.tensor_tensor(out=P[:], in0=P[:], in1=msk.unsqueeze(1).to_broadcast([128, 4, 128]), op=ALU.mult),
                                     r=[BPj, B_cst], w=[BPj])
                            pend.append((P, BPj, c))
                            if len(pend) > 1:
                                flush_pv()
                        while pend:
                            flush_pv()
                        combine(bo, Bo, i, g, br, False, oa, Boa_)
                S.op("act", lambda e, oa=oa: e.activation(out=oab[:], in_=oa[:], func=AF.Copy), r=[Boa_], w=[Boab])
                bt, Bt = ringS.next()
                btb = bt[:].bitcast(BF16).rearrange("p (k t) -> p k t", k=8)
                for k in range(4):
                    S.op("pe", lambda e, k=k, btb=btb: e.transpose(btb[:, k, :], oab[:, k * 128:(k + 1) * 128], identb), r=[Boab, B_cst], w=[Bt])
                S.op("act", lambda e, btb=btb: e.activation(out=o_nsaT[:, :, tsl], in_=btb[:, 0:4, :], func=AF.Copy), r=[Bt], w=[B_onT])
            S.barrier()
    dump("o_nsaT", o_nsaT[:], [B_onT], [128, 4, T], BF16)
    if not active("c2"):
        return None
    gtf = _sequence2(nc, S, sq, b, G, locals())
    if gtf is None:
        return None
    smx.close()
    _peer(nc, S, sq, b, G, locals(), gtf)


def _sequence2(nc, S, sq, b, G, L):
    sb = G["sb"]
    Ring, dump, active = G["Ring"], G["dump"], G["active"]
    cst, cstb, identb, Mcb, Mwb, blkf, onesf = (G[k] for k in ("cst", "cstb", "identb", "Mcb", "Mwb", "blkf", "onesf"))
    B_cst = G["B_cst"]
    x_d, out_d, x1s_d = G["x_d"], G["out_d"], G["x1s_d"]
    T_, ring, fm_units, tk_units = L["T_"], L["ring"], L["fm_units"], L["tk_units"]
    hT, B_hT, o_nsaT, B_onT = L["hT"], L["B_hT"], L["o_nsaT"], L["B_onT"]
    norm_to_T, ada_round, make_scale, bcast_row = L["norm_to_T"], L["ada_round"], L["make_scale"], L["bcast_row"]
    banks, bbuf = G["banks"], G["bbuf"]

    smx = L["smx"]
    o_hgT, B_ohT, res2 = L["o_hgT"], L["B_ohT"], L["res"]
    with ExitStack() as sh:
        qdec = T_("qdec", [128, 4, T], BF16, sh)
        kinv = T_("kinv", [128, 4, T], BF16, sh)
        sgt = T_("sgt", [128, 4, T], BF16, sh)
        vtok = T_("vtok", [128, NT, 512], BF16, sh)
        ebend = T_("ebend", [128, 4, 64], F32, sh)
        lb = T_("lb", [128, 12], F32, sh)
        lbl = T_("lbl", [128, 4, 2], F32, sh)
        ngt = T_("ngt", [128, 1], F32, sh)
        B_qd, B_ki, B_sg, B_vt, B_eb, B_lb = (Buf(n) for n in ("qdec", "kinv", "sgt", "vtok", "ebend", "lb"))
        S.dma("sp", lbl[:], G["lbl_d"][:, :, :], w=[B_lb], key="small")
        S.dma("sp", ngt[:], G["ng_d"][:, :], w=[B_lb], key="small")
        S.op("dve", lambda e: e.tensor_tensor(out=lb[:, 8:12], in0=lbl[:, :, 0], in1=lbl[:, :, 1], op=ALU.subtract), r=[B_lb], w=[B_lb])
        S.op("act", lambda e: e.activation(out=lb[:, 0:4], in_=lb[:, 8:12], func=AF.Sigmoid), r=[B_lb], w=[B_lb])
        S.op("dve", lambda e: e.tensor_scalar(out=lb[:, 4:8], in0=lb[:, 0:4], scalar1=-1.0, scalar2=1.0, op0=ALU.mult, op1=ALU.add), r=[B_lb], w=[B_lb])
        with ExitStack() as st:
            scanm = T_("scanm", [128, 512], F32, st)
            B_sc = Buf("scanm")
            S.dma("sp", scanm[:], G["scanm_d"][:, :], w=[B_sc], key="small")
            tf = [T_("hg_t%d" % i, [128, 512], F32, st) for i in range(6)]
            Btf = [Buf("hg_t%d" % i) for i in range(6)]

            def ep_c2(u, tc, bl):
                sl = slice(tc * 512, (tc + 1) * 512)
                if u >= 12:
                    for g in range(2):
                        h = (u - 12) * 2 + g
                        S.op("act", lambda e, h=h, g=g: e.activation(out=sgt[:, h, sl], in_=bl[g][0][:], func=AF.Silu), r=[bl[g][1]], w=[B_sg])
                    return
                h = u - 8
                (bq, Bq), (bf, Bf) = bl
                f, lf, bb, eb, en, om = tf
                S.op("act", lambda e: e.activation(out=f[:], in_=bf[:], func=AF.Sigmoid), r=[Bf], w=[Btf[0]])
                S.op("dve", lambda e: e.tensor_scalar(out=f[:], in0=f[:], scalar1=lb[:, 4 + h:5 + h], scalar2=lb[:, h:h + 1], op0=ALU.mult, op1=ALU.add),
                     r=[Btf[0], B_lb], w=[Btf[0]])
                S.op("act", lambda e: e.activation(out=lf[:], in_=f[:], func=AF.Ln), r=[Btf[0]], w=[Btf[1]])
                S.op("dve", lambda e: e.tensor_tensor_scan(out=bb[:], data0=scanm[:], data1=lf[:], initial=0.0, op0=ALU.mult, op1=ALU.add),
                     r=[Btf[1], B_sc], w=[Btf[2]])
                S.op("act", lambda e: e.activation(out=eb[:], in_=bb[:], func=AF.Exp), r=[Btf[2]], w=[Btf[3]])
                S.op("act", lambda e: e.activation(out=en[:], in_=bb[:], func=AF.Exp, scale=-1.0), r=[Btf[2]], w=[Btf[4]])
                S.op("pool", lambda e: e.tensor_copy(out=ebend[:, h, tc * 16:(tc + 1) * 16], in_=eb[:, 31:512:32]), r=[Btf[3]], w=[B_eb])
                S.op("pool", lambda e: e.tensor_scalar(out=om[:], in0=f[:], scalar1=-1.0, scalar2=1.0, op0=ALU.mult, op1=ALU.add), r=[Btf[0]], w=[Btf[5]])
                S.op("dve", lambda e: e.tensor_tensor(out=kinv[:, h, sl], in0=om[:], in1=en[:], op=ALU.mult), r=[Btf[5], Btf[4]], w=[B_ki])
                S.op("act", lambda e: e.activation(out=lf[:], in_=bq[:], func=AF.Silu), r=[Bq, Btf[1]], w=[Btf[1]])
                S.op("dve", lambda e: e.tensor_tensor(out=qdec[:, h, sl], in0=lf[:], in1=eb[:], op=ALU.mult), r=[Btf[1], Btf[3]], w=[B_qd])

            fm_units([(u, 2) for u in range(8, 14)], st, ep_c2, nbuf=1)
            S.barrier()
        with ExitStack() as st:

            def ep_t2(u, i, bk, Bk):
                S.op("act", lambda e: e.activation(out=vtok[:, i, (u - 2) * 256:(u - 1) * 256], in_=bk[:, 0:256], func=AF.Copy), r=[Bk], w=[B_vt])

            tk_units([2, 3], st, ep_t2)
            S.barrier()
        dump("qdec", qdec[:], [B_qd], [128, 4, T], BF16)
        dump("kinv", kinv[:], [B_ki], [128, 4, T], BF16)
        dump("ebend", ebend[:], [B_eb], [128, 4, 64])
        dump("vtok", vtok[:], [B_vt], [128, NT, 512], BF16)
        if not active("hgrn"):
            return None

        with ExitStack() as st:
            Sf = T_("Sf", [128, 4, 128], F32, st)
            Sb = T_("Sb", [128, 4, 128], BF16, st)
            Tt = T_("Tt", [128, 4, 128], F32, st)
            kit = [T_("kit%d" % i, [128, 4, 128], BF16, st) for i in range(2)]
            kitz = [T_("kitz%d" % i, [128, 4, 128], BF16, st) for i in range(2)]
            Bkitz = [Buf("kitz%d" % i) for i in range(2)]
            At = [T_("At%d" % i, [128, 4, 128], BF16, st) for i in range(2)]
            sqf = T_("hsq", [128, 512], F32, st)
            rsd = T_("hrsd", [128, 512], F32, st)
            o1 = T_("ho1", [128, 512], F32, st)
            B_S, B_Sb, B_Tt, B_sq, B_rs, B_o1 = (Buf(n) for n in ("Sf", "Sb", "Tt", "hsq", "hrsd", "ho1"))
            Bkit = [Buf("kit%d" % i) for i in range(2)]
            BAt = [Buf("At%d" % i) for i in range(2)]
            S.op("pool", lambda e: e.memset(Sf[:], 0.0), w=[B_S])
            S.op("pool", lambda e: e.memset(Sb[:], 0.0), w=[B_Sb])
            obank = [0, 1, 2, 3]
            r2 = Ring([4, 5, 6, 7])
            for i in range(DBG.get("hg_tiles", NT)):
                tsl = slice(i * 128, (i + 1) * 128)
                j = i % 2
                bt, Bt = r2.next()
                btb = bt[:].bitcast(BF16).rearrange("p (k t) -> p k t", k=8)
                for h in range(4):
                    S.op("pe", lambda e, h=h, btb=btb: e.transpose(btb[:, h, :], kinv[:, h, tsl], identb), r=[B_ki, B_cst], w=[Bt])
                S.op("act", lambda e, btb=btb, j=j: e.activation(out=kit[j][:], in_=btb[:, 0:4, :], func=AF.Copy), r=[Bt], w=[Bkit[j]])
                S.op("dve", lambda e, j=j: e.tensor_scalar(out=kitz[j][:], in0=kit[j][:], scalar1=cst[:, 351:352], scalar2=None, op0=ALU.mult),
                     r=[Bkit[j], B_cst], w=[Bkitz[j]])
                ba, Ba = r2.next()
                ba4 = ba[:].rearrange("p (h t) -> p h t", h=4)
                for h in range(4):
                    S.op("pe", lambda e, h=h, ba4=ba4: e.matmul(ba4[:, h, :], lhsT=kinv[:, h, tsl], rhs=qdec[:, h, tsl], start=True, stop=True),
                         r=[B_ki, B_qd], w=[Ba])
                S.op("dve", lambda e, ba4=ba4, j=j: e.tensor_tensor(out=At[j][:], in0=ba4, in1=blkf.unsqueeze(1).to_broadcast([128, 4, 128]), op=ALU.mult),
                     r=[Ba, B_cst], w=[BAt[j]])
                for sub in range(DBG.get("hg_subs", 4)):
                    c = 4 * i + sub
                    psl = slice(sub * 32, (sub + 1) * 32) if sub < 3 else slice(64, 128)
                    asl = slice(sub * 32, (sub + 1) * 32)
                    kt_, Bkt_ = (kit[j], Bkit[j]) if sub < 3 else (kitz[j], Bkitz[j])
                    csl = slice(c * 32, (c + 1) * 32)
                    osl = slice((c % 16) * 32, (c % 16) * 32 + 32)
                    for h in range(4):
                        ob, Bob = banks[obank[h]], bbuf[obank[h]]
                        S.op("pe", lambda e, h=h, ob=ob: e.matmul(ob[:, osl], lhsT=vtok[psl, i, h * 128:(h + 1) * 128], rhs=At[j][psl, h, asl], start=True, stop=False),
                             r=[B_vt, BAt[j]], w=[Bob])
                        S.op("pe", lambda e, h=h, ob=ob: e.matmul(ob[:, osl], lhsT=Sb[:, h, :], rhs=qdec[:, h, csl], start=False, stop=True),
                             r=[B_Sb, B_qd], w=[Bob])
                    bu, Bu = r2.next()
                    bu4 = bu[:].rearrange("p (h t) -> p h t", h=4)
                    for h in range(4):
                        S.op("pe", lambda e, h=h, bu4=bu4: e.matmul(bu4[:, h, :], lhsT=kt_[psl, h, :], rhs=vtok[psl, i, h * 128:(h + 1) * 128], start=True, stop=True),
                             r=[Bkt_, B_vt], w=[Bu])
                    S.op("dve", lambda e, bu4=bu4: e.tensor_tensor(out=Tt[:], in0=bu4, in1=Sf[:], op=ALU.add), r=[Bu, B_S], w=[B_Tt])
                    S.op("dve", lambda e, c=c: e.tensor_tensor(out=Sf[:], in0=Tt[:], in1=ebend[:, :, c:c + 1].to_broadcast([128, 4, 128]), op=ALU.mult),
                         r=[B_Tt, B_eb], w=[B_S])
                    S.op("act", lambda e: e.activation(out=Sb[:], in_=Sf[:], func=AF.Copy), r=[B_S], w=[B_Sb])
                if i % 4 == 3 and DBG.get("hg_final", True):
                    span = slice((i // 4) * 512, (i // 4 + 1) * 512)
                    for h in range(4):
                        ob, Bob = banks[obank[h]], bbuf[obank[h]]
                        S.op("act", lambda e, ob=ob: e.activation(out=sqf[:], in_=ob[:], func=AF.Square), r=[Bob], w=[B_sq])
                        bm, Bm = r2.next()
                        S.op("pe", lambda e, bm=bm: e.matmul(bm[:], lhsT=onesf, rhs=sqf[:], start=True, stop=True), r=[B_sq, B_cst], w=[Bm])
                        S.op("dve", lambda e, bm=bm: e.tensor_scalar(out=rsd[:], in0=bm[:], scalar1=EPS, scalar2=None, op0=ALU.add), r=[Bm], w=[B_rs])
                        S.op("act", lambda e: e.activation(out=rsd[:], in_=rsd[:], func=AF.Sqrt), r=[B_rs], w=[B_rs])
                        S.op("dve", lambda e: e.reciprocal(out=rsd[:], in_=rsd[:]), r=[B_rs], w=[B_rs])
                        S.op("dve", lambda e, ob=ob: e.tensor_tensor(out=o1[:], in0=ob[:], in1=rsd[:], op=ALU.mult), r=[Bob, B_rs], w=[B_o1])
                        S.op("dve", lambda e, h=h: e.scalar_tensor_tensor(out=o_hgT[:, h, span], in0=o1[:], scalar=ngt[:, 0:1], op0=ALU.mult, in1=sgt[:, h, span], op1=ALU.mult),
                             r=[B_o1, B_lb, B_sg], w=[B_ohT])
            S.barrier()
    dump("o_hgT", o_hgT[:], [B_ohT], [128, 4, T], BF16)
    if not active("merge"):
        return None

    gtm = [res2[4], res2[5]]
    Ashf = [res2[6], res2[7]]
    Ascf = [res2[8], res2[9]]
    gtf = [res2[10], res2[11]]
    yT = T_("yT", [128, 8, T], BF16, smx)
    B_yT = Buf("yT")
    with ExitStack() as st:
        wbs = T_("wbs", [128, 4, D], F32, st)
        wbb = [T_("wbb%d" % i, [128, 4, D], BF16, st) for i in range(2)]
        B_wbs, B_wbb = Buf("wbs"), Buf("wbb")
        for j in range(2):
            S.dma("sp", wbs[:], G["wbr_d"][j], w=[B_wbs], key="wbs")
            S.op("pool", lambda e, j=j: e.tensor_copy(out=wbb[j][:], in_=wbs[:]), r=[B_wbs], w=[B_wbb])
        gaf = [T_("gaf%d" % i, [128, 512], F32, st) for i in range(2)]
        Bga = [Buf("gaf%d" % i) for i in range(2)]

        def ep_m(u, tc, bl):
            c = u - 14
            sl = slice(tc * 512, (tc + 1) * 512)
            srcs = ((o_nsaT, B_onT), (o_hgT, B_ohT))
            for j in range(2):
                S.op("act", lambda e, j=j: e.activation(out=gaf[j][:], in_=bl[j][0][:], func=AF.Sigmoid), r=[bl[j][1]], w=[Bga[j]])
                bk, Bk = ring.next()
                for k in range(4):
                    S.op("pe", lambda e, k=k, j=j, bk=bk: e.matmul(bk[:], lhsT=wbb[j][:, k, c * 128:(c + 1) * 128], rhs=srcs[j][0][:, k, sl], start=(k == 0), stop=(k == 3)),
                         r=[B_wbb, srcs[j][1]], w=[Bk])
                S.op("dve", lambda e, j=j, bk=bk: e.tensor_tensor(out=gaf[j][:], in0=gaf[j][:], in1=bk[:], op=ALU.mult), r=[Bga[j], Bk], w=[Bga[j]])
            S.op("pool", lambda e: e.tensor_tensor(out=yT[:, c, sl], in0=gaf[0][:], in1=gaf[1][:], op=ALU.add), r=[Bga[0], Bga[1]], w=[B_yT])

        fm_units([(u, 2) for u in range(14, 22)], st, ep_m)
        S.barrier()
    dump("yT", yT[:], [B_yT], [128, 8, T], BF16)

    with ExitStack() as st:
        wos = T_("wos", [128, 8, 512], F32, st)
        wob = T_("wob", [128, 8, D], BF16, st)
        B_wos, B_wob = Buf("wos"), Buf("wob")
        for hf in range(2):
            S.dma("sp", wos[:], G["wout_d"][:, :, hf * 512:(hf + 1) * 512], w=[B_wos], key="wos")
            S.op("pool", lambda e, hf=hf: e.tensor_copy(out=wob[:, :, hf * 512:(hf + 1) * 512], in_=wos[:]), r=[B_wos], w=[B_wob])
        xr = [T_("xr2_%d" % i, [128, D], F32, st) for i in range(2)]
        Bxr = [Buf("xr2_%d" % i) for i in range(2)]
        x1t = [T_("x1t%d" % i, [128, D], F32, st) for i in range(2)]
        Bx1 = [Buf("x1t%d" % i) for i in range(2)]
        tmy = T_("tmy", [128, D], F32, st)
        Btmy = Buf("tmy")
        nb_ = (T_("n2_ss", [128, 1], F32, st), T_("n2_rstd", [128, 1], F32, st), T_("n2_h1", [128, D], F32, st), T_("n2_hb", [128, D], BF16, st),
               Buf("n2_ss"), Buf("n2_h1"), Buf("n2_hb"))
        for i in range(NT):
            j = i % 2
            tsl = slice(i * 128, (i + 1) * 128)
            S.dma("sp", xr[j][:], x_d[b, tsl, :], w=[Bxr[j]], key="xr2_%d" % j)
            for hf in range(2):
                bk, Bk = ring.next()
                sl = slice(hf * 512, (hf + 1) * 512)
                for k in range(8):
                    S.op("pe", lambda e, k=k, bk=bk, sl=sl: e.matmul(bk[:], lhsT=yT[:, k, tsl], rhs=wob[:, k, sl], start=(k == 0), stop=(k == 7)),
                         r=[B_yT, B_wob], w=[Bk])
                S.op("dve", lambda e, bk=bk, sl=sl, hf=hf: e.tensor_tensor(out=tmy[:, sl], in0=bk[:], in1=gtm[hf][0][:], op=ALU.mult), r=[Bk, gtm[hf][1]], w=[Btmy])
            S.op("pool", lambda e, j=j: e.tensor_tensor(out=x1t[j][:], in0=xr[j][:], in1=tmy[:], op=ALU.add), r=[Bxr[j], Btmy], w=[Bx1[j]])
            S.dma("sp", x1s_d[b, tsl, :], x1t[j][:], r=[Bx1[j]], key="x1w%d" % j)
            norm_to_T(x1t[j], Bx1[j], Ascf, Ashf, hT, B_hT, i, nb_)
        S.barrier()
    dump("h2T", hT[:], [B_hT], [128, 8, T], BF16)
    if "x1" in G["dbg"]:
        with ExitStack() as st:
            xx = T_("dbgx1", [128, NT, D], F32, st)
            Bxx = Buf("dbgx1")
            S.dma("sp", xx[:], x1s_d[b].rearrange("(i p) d -> p i d", p=128), w=[Bxx], key="dbgx")
            dump("x1", xx[:], [Bxx], [128, NT, D])
            S.barrier()
    if not active("peer"):
        return None
    return gtf


def _peer(nc, S, sq, b, G, L, gtf):
    sb = G["sb"]
    Ring, dump = G["Ring"], G["dump"]
    identb = G["identb"]
    B_cst = G["B_cst"]
    x1s_d, out_d = G["x1s_d"], G["out_d"]
    T_, hT, B_hT, bcast_row = L["T_"], L["hT"], L["B_hT"], L["bcast_row"]
    banks, bbuf = G["banks"], G["bbuf"]
    uTb_d, pvb_d, wpqb_d = G["uTb_d"], G["pvb_d"], G["wpqb_d"]
    B_scr = G["B_scr"]
    with ExitStack() as st:
        skb = T_("skb", [128, 16, 128], BF16, st)
        gfin = T_("gfin", [128, D], F32, st)
        B_sk, B_gf = Buf("skb"), Buf("gfin")
        with ExitStack() as tmp:
            skf = T_("skf", [128, 16, 128], F32, tmp)
            S.dma("sp", skf[:], G["skT_d"][:, :, :], w=[B_sk], key="small")
            S.op("act", lambda e: e.activation(out=skb[:], in_=skf[:], func=AF.Copy), r=[B_sk], w=[B_sk])
            S.barrier()
        bcast_row(gfin[:], G["gfin_d"][0:1, :], [B_gf], "small")
        qpT = T_("qpT", [128, 16, 256], BF16, st)
        B_qp = Buf("qpT")
        ssb = [T_("ssb%d" % i, [128, 16, 128], F32, st) for i in range(2)]
        Bss = [Buf("ssb%d" % i) for i in range(2)]
        top = T_("ptop", [128, 16, 16], F32, st)
        scr = T_("pscr", [128, 128], F32, st)
        comb = T_("pcomb", [128, 8, 256], F32, st)
        scrc = T_("pscrc", [128, 256], F32, st)
        ctop = [T_("pctop%d" % i, [128, 8, 16], F32, st) for i in range(2)]
        pez = T_("pez", [128, 8, 16], F32, st)
        negc = [T_("pnegc%d" % i, [128, 8], F32, st) for i in range(2)]
        Btk = Buf("topk_scratch")
        Bct = [Buf("ctop%d" % i) for i in range(2)]
        tb = [T_("ptb%d" % i, [128, 16, 128], F32, st) for i in range(2)]
        eb = [T_("peb%d" % i, [128, 16, 128], BF16, st) for i in range(2)]
        wh = [T_("pwh%d" % i, [128, 16, 128], BF16, st) for i in range(2)]
        Btb = [Buf("ptb%d" % i) for i in range(2)]
        Beb = [Buf("peb%d" % i) for i in range(2)]
        Bwh = [Buf("pwh%d" % i) for i in range(2)]
        acc = [T_("pacc%d" % i, [128, 2, 4096], BF16, st) for i in range(2)]
        Bacc = [[Buf("pacc%d_%d" % (i, t)) for t in range(2)] for i in range(2)]
        ub = [T_("pub%d" % i, [128, 8, 512], BF16, st) for i in range(2)]
        vb = [T_("pvb%d" % i, [128, 4, D], BF16, st) for i in range(2)]
        Bub = [Buf("pub%d" % i) for i in range(2)]
        Bvb = [Buf("pvb%d" % i) for i in range(2)]
        ga = [T_("pga%d" % i, [128, 512], BF16, st) for i in range(3)]
        wa = [T_("pwa%d" % i, [128, 512], BF16, st) for i in range(3)]
        waT = [T_("pwaT%d" % i, [128, 4, 128], BF16, st) for i in range(3)]
        Bga = [Buf("pga%d" % i) for i in range(3)]
        Bwa = [Buf("pwa%d" % i) for i in range(3)]
        BwaT = [Buf("pwaT%d" % i) for i in range(3)]
        x1t = T_("px1", [128, D], F32, st)
        xo = x1t
        ptmp = T_("ptmp", [128, D], F32, st)
        pss = T_("pss", [128, 2], F32, st)
        Bx1 = Buf("px1")
        Bxo, Bpt, Bpss = Bx1, Buf("ptmp"), Buf("pss")
        r2 = Ring([4, 5, 6, 7])
        rA = Ring([4, 5])
        ucnt = [0]
        mcnt = [0]
        wcnt = [0]

        for st_ in range(T // 256):
            n0 = st_ * 256
            for pi in range(4):
                j = ucnt[0] % 2
                ucnt[0] += 1
                S.dma("sp", ub[j][:], wpqb_d[pi], r=[B_scr], w=[Bub[j]], key="pub%d" % j)
                for gg in range(4):
                    bk, Bk = r2.next()
                    for k in range(8):
                        S.op("pe", lambda e, k=k, gg=gg, bk=bk, j=j: e.matmul(bk[:, 0:256], lhsT=ub[j][:, k, gg * 128:(gg + 1) * 128], rhs=hT[:, k, n0:n0 + 256],
                                                                             start=(k == 0), stop=(k == 7)), r=[Bub[j], B_hT], w=[Bk])
                    S.op("act", lambda e, gg=gg, bk=bk, pi=pi: e.activation(out=qpT[:, pi * 4 + gg, :], in_=bk[:, 0:256], func=AF.Copy), r=[Bk], w=[B_qp])
            for tt in range(2):
                for q4 in range(4):
                    bk, Bk = r2.next()
                    for hh in range(4):
                        hp = q4 * 4 + hh
                        S.op("pe", lambda e, hp=hp, hh=hh, bk=bk: e.matmul(bk[:, hh * 128:(hh + 1) * 128], lhsT=qpT[:, hp, tt * 128:(tt + 1) * 128], rhs=skb[:, hp, :],
                                                                          start=True, stop=True), r=[B_qp, B_sk], w=[Bk])
                    S.op("act", lambda e, q4=q4, bk=bk: e.activation(out=ssb[tt][:, q4 * 4:(q4 + 1) * 4, :], in_=bk[:].rearrange("p (a k) -> p a k", a=4), func=AF.Copy),
                         r=[Bk], w=[Bss[tt]])
                for hp in range(16):
                    S.op("dve", lambda e, hp=hp: e.max(out=top[:, hp, 0:8], in_=ssb[tt][:, hp, :]), r=[Bss[tt]], w=[Btk])
                    S.op("dve", lambda e, hp=hp: e.match_replace(out=scr[:], in_to_replace=top[:, hp, 0:8], in_values=ssb[tt][:, hp, :], imm_value=-1e30), r=[Bss[tt], Btk], w=[Btk])
                    S.op("dve", lambda e, hp=hp: e.max(out=top[:, hp, 8:16], in_=scr[:]), r=[Btk], w=[Btk])
                t4 = top[:].rearrange("p (h q) r -> p h q r", q=2)
                S.op("dve", lambda e, t4=t4: e.tensor_tensor(out=comb[:].rearrange("p h (a c) -> p h a c", a=16), in0=t4[:, :, 0, :].unsqueeze(3).to_broadcast([128, 8, 16, 16]),
                                                          in1=t4[:, :, 1, :].unsqueeze(2).to_broadcast([128, 8, 16, 16]), op=ALU.add), r=[Btk], w=[Btk])
                for h in range(8):
                    S.op("dve", lambda e, h=h: e.max(out=ctop[tt][:, h, 0:8], in_=comb[:, h, :]), r=[Btk], w=[Bct[tt]])
                    S.op("dve", lambda e, h=h: e.match_replace(out=scrc[:], in_to_replace=ctop[tt][:, h, 0:8], in_values=comb[:, h, :], imm_value=-1e30), r=[Btk, Bct[tt]], w=[Btk])
                    S.op("dve", lambda e, h=h: e.max(out=ctop[tt][:, h, 8:16], in_=scrc[:]), r=[Btk], w=[Bct[tt]])
                S.op("dve", lambda e: e.tensor_tensor(out=pez[:], in0=ctop[tt][:], in1=ctop[tt][:, :, 0:1].to_broadcast([128, 8, 16]), op=ALU.subtract), r=[Bct[tt]], w=[Btk])
                S.op("act", lambda e: e.activation(out=pez[:], in_=pez[:], func=AF.Exp), r=[Btk], w=[Btk])
                S.op("dve", lambda e: e.reduce_sum(out=negc[tt][:], in_=pez[:], axis=AX.X), r=[Btk], w=[Bct[tt]])
                S.op("act", lambda e: e.activation(out=negc[tt][:], in_=negc[tt][:], func=AF.Ln), r=[Bct[tt]], w=[Bct[tt]])
                S.op("dve", lambda e: e.tensor_tensor(out=negc[tt][:], in0=negc[tt][:], in1=ctop[tt][:, :, 0], op=ALU.add), r=[Bct[tt]], w=[Bct[tt]])
                S.op("dve", lambda e: e.tensor_scalar(out=negc[tt][:], in0=negc[tt][:], scalar1=-1.0, scalar2=None, op0=ALU.mult), r=[Bct[tt]], w=[Bct[tt]])

            def w_tadd(q):
                k, n = divmod(q, 32)
                tt, rem = divmod(n, 16)
                hh, h = divmod(rem, 8)
                r0 = k * 32 + hh * 16
                i = q % 2
                S.op("dve", lambda e: e.tensor_tensor(out=tb[i][:], in0=ssb[tt][:, 2 * h, r0:r0 + 16].unsqueeze(2).to_broadcast([128, 16, 128]),
                                                      in1=ssb[tt][:, 2 * h + 1, :].unsqueeze(1).to_broadcast([128, 16, 128]), op=ALU.add), r=[Bss[tt]], w=[Btb[i]])

            def w_rest(q):
                k, n = divmod(q, 32)
                tt, rem = divmod(n, 16)
                hh, h = divmod(rem, 8)
                kb = k % 2
                i = q % 2
                S.op("act", lambda e: e.activation(out=eb[i][:], in_=tb[i][:], func=AF.Exp, bias=negc[tt][:, h:h + 1]), r=[Btb[i], Bct[tt]], w=[Beb[i]])
                dst = acc[kb][:, tt, hh * 2048:(hh + 1) * 2048].rearrange("p (a c) -> p a c", a=16)
                if h == 0:
                    S.op("dve", lambda e: e.scalar_tensor_tensor(out=dst, in0=tb[i][:], scalar=ctop[tt][:, h, 15:16], op0=ALU.is_ge, in1=eb[i][:], op1=ALU.mult),
                         r=[Btb[i], Beb[i], Bct[tt]], w=[Bacc[kb][tt]])
                else:
                    S.op("dve", lambda e: e.scalar_tensor_tensor(out=wh[i][:], in0=tb[i][:], scalar=ctop[tt][:, h, 15:16], op0=ALU.is_ge, in1=eb[i][:], op1=ALU.mult),
                         r=[Btb[i], Beb[i], Bct[tt]], w=[Bwh[i]])
                    return lambda: S.op("dve", lambda e: e.tensor_tensor(out=dst, in0=dst, in1=wh[i][:], op=ALU.add), r=[Bwh[i], Bacc[kb][tt]], w=[Bacc[kb][tt]])
                return None

            wq = [0]

            def w_emit(upto_q):
                pend = None
                while wq[0] < upto_q:
                    q = wq[0]
                    if q == 0:
                        w_tadd(0)
                    if q + 1 < 128:
                        w_tadd(q + 1)
                    newp = w_rest(q)
                    if pend is not None:
                        pend()
                    pend = newp
                    wq[0] += 1
                if pend is not None:
                    pend()

            mst = {}

            def mA(s_):
                k, mi = divmod(s_, 16)
                jg, tt = divmod(mi, 2)
                eg = k * 8 + jg
                if tt == 0:
                    j = ucnt[0] % 2
                    ucnt[0] += 1
                    S.dma("sp", ub[j][:], uTb_d[eg], r=[B_scr], w=[Bub[j]], key="pub%d" % j)
                    S.dma("sp", vb[j][:], pvb_d[eg], r=[B_scr], w=[Bvb[j]], key="pvb%d" % j)
                j = (ucnt[0] - 1) % 2
                bk, Bk = rA.next()
                tok = slice(n0 + tt * 128, n0 + (tt + 1) * 128)
                for kk in range(8):
                    S.op("pe", lambda e, kk=kk: e.matmul(bk[:], lhsT=hT[:, kk, tok], rhs=ub[j][:, kk, :], start=(kk == 0), stop=(kk == 7)), r=[B_hT, Bub[j]], w=[Bk])
                mst[s_] = dict(j=j, bk=bk, Bk=Bk, k=k, jg=jg, tt=tt, eg=eg, m=s_ % 3)

            def mB(s_):
                d_ = mst[s_]
                m, bk, Bk, kb, tt, jg = d_["m"], d_["bk"], d_["Bk"], d_["k"] % 2, d_["tt"], d_["jg"]
                S.op("act", lambda e: e.activation(out=ga[m][:], in_=bk[:], func=AF.Gelu_apprx_tanh), r=[Bk], w=[Bga[m]])
                S.op("dve", lambda e: e.tensor_tensor(out=wa[m][:], in0=ga[m][:], in1=acc[kb][:, tt, jg * 512:(jg + 1) * 512], op=ALU.mult), r=[Bga[m], Bacc[kb][tt]], w=[Bwa[m]])

            def mC(s_):
                d_ = mst[s_]
                m = d_["m"]
                hb_ = 6 + s_ % 2
                btb = banks[hb_][:].bitcast(BF16).rearrange("p (k t) -> p k t", k=8)[:, 0:4, :]
                for jj in range(4):
                    S.op("pe", lambda e, jj=jj: e.transpose(btb[:, jj, :], wa[m][:, jj * 128:(jj + 1) * 128], identb), r=[Bwa[m], B_cst], w=[bbuf[hb_]])
                S.op("act", lambda e: e.activation(out=waT[m][:], in_=btb, func=AF.Copy), r=[bbuf[hb_]], w=[BwaT[m]])

            def mD(s_):
                d_ = mst.pop(s_)
                m, j, tt, eg = d_["m"], d_["j"], d_["tt"], d_["eg"]
                for jj in range(4):
                    for hf in range(2):
                        yb, By = banks[tt * 2 + hf], bbuf[tt * 2 + hf]
                        S.op("pe", lambda e, jj=jj, hf=hf, yb=yb: e.matmul(yb[:], lhsT=waT[m][:, jj, :], rhs=vb[j][:, jj, hf * 512:(hf + 1) * 512],
                                                                          start=(eg == 0 and jj == 0), stop=(eg == 31 and jj == 3)), r=[BwaT[m], Bvb[j]], w=[By])

            ms = [0]

            def m_emit(nsteps):
                for _ in range(nsteps):
                    s_ = ms[0]
                    if s_ < 64:
                        mA(s_)
                        mB(s_)
                    if 0 <= s_ - 1 < 64:
                        mC(s_ - 1)
                    if 0 <= s_ - 2 < 64:
                        mD(s_ - 2)
                    ms[0] += 1

            w_emit(32)
            for k in range(4):
                for ch in range(4):
                    m_emit(4)
                    if k < 3:
                        w_emit(32 * (k + 1) + 8 * (ch + 1))
            m_emit(2)
            for tt in range(2):
                tok = slice(n0 + tt * 128, n0 + (tt + 1) * 128)
                S.dma("sp", x1t[:], x1s_d[b, tok, :], w=[Bx1], key="px1")
                for hf in range(2):
                    sl = slice(hf * 512, (hf + 1) * 512)
                    yb, By = banks[tt * 2 + hf], bbuf[tt * 2 + hf]
                    S.op("dve", lambda e, yb=yb, sl=sl, hf=hf: e.tensor_tensor(out=ptmp[:, sl], in0=yb[:], in1=gtf[hf][0][:], op=ALU.mult), r=[By, gtf[hf][1]], w=[Bpt])
                S.op("pool", lambda e: e.tensor_tensor(out=xo[:], in0=x1t[:], in1=ptmp[:], op=ALU.add), r=[Bx1, Bpt], w=[Bxo])
                S.op("act", lambda e: e.activation(out=ptmp[:], in_=xo[:], func=AF.Square, accum_out=pss[:, 0:1]), r=[Bxo, Bpt], w=[Bpt, Bpss])
                S.op("dve", lambda e: e.tensor_scalar(out=pss[:, 1:2], in0=pss[:, 0:1], scalar1=1.0 / D, scalar2=EPS, op0=ALU.mult, op1=ALU.add), r=[Bpss], w=[Bpss])
                S.op("act", lambda e: e.activation(out=pss[:, 1:2], in_=pss[:, 1:2], func=AF.Sqrt), r=[Bpss], w=[Bpss])
                S.op("dve", lambda e: e.reciprocal(out=pss[:, 1:2], in_=pss[:, 1:2]), r=[Bpss], w=[Bpss])
                S.op("dve", lambda e: e.scalar_tensor_tensor(out=xo[:], in0=xo[:], scalar=pss[:, 1:2], op0=ALU.mult, in1=gfin[:], op1=ALU.mult), r=[Bxo, Bpss, B_gf], w=[Bxo])
                S.dma("sp", out_d[b, tok, :], xo[:], r=[Bxo], key="outw")
        S.barrier()


def _sel_map():
    i = np.arange(127)[:, None]
    j = np.arange(32)[None, :]
    d = i - 4 * j
    cnt = np.minimum(d, 3) - np.maximum(d - 1, 0) + 1
    return np.clip(cnt, 0, None).astype(np.float32)


def _const_inputs():
    f32 = np.float32
    s = np.arange(128)[:, None]
    t = np.arange(128)[None, :]
    cst = np.concatenate([np.eye(128), (s <= t), (s > t), ((s // 32 == t // 32) & (s <= t)), np.full((128, 128), 1.0 / 128)], axis=1).astype(f32)
    half = 32
    inv = (np.float32(10000.0) ** (-np.arange(half, dtype=f32) / np.float32(half))).astype(f32)
    ang = (np.arange(T, dtype=f32)[:, None] * inv[None, :]).astype(f32)
    cos = np.cos(ang).astype(f32).T
    sin = np.sin(ang).astype(f32).T
    cos64 = np.concatenate([cos, cos], axis=0)
    sin64 = np.concatenate([-sin, sin], axis=0)
    rope = np.stack([np.concatenate([cos64, cos64], 0), np.concatenate([sin64, sin64], 0)], axis=1).astype(f32)
    scanm = np.ones((128, 512), f32)
    scanm[:, 0::32] = 0.0
    n = np.arange(128)[:, None]
    tt = np.arange(T)[None, :]
    mcmp = ((16 * n + 31 <= tt) & (n < 127)).astype(f32)
    tok = (np.arange(NT)[None, :] * 128 + np.arange(128)[:, None])
    jb = np.arange(32)[None, None, :]
    cur = (tok // 64)[:, :, None]
    forced = (jb == 0) | (jb == cur) | (jb == cur - 1)
    causal = (jb * 64 <= tok[:, :, None])
    f1c = np.stack([1000.0 * forced + 1.0, causal.astype(np.float64)], axis=1).astype(f32)
    eall = np.zeros((128, T), f32)
    col = np.arange(T)
    eall[(2 * (col // 128) + (col % 128) // 64), col] = 1.0
    selmap = np.zeros((128, 32), f32)
    selmap[:127] = _sel_map()
    return dict(cst=cst, rope=rope, scanm=scanm, mcmp=mcmp, f1c=f1c, eall=eall, selmap=selmap)


def _kchunk(w):
    return np.ascontiguousarray(w.reshape(8, 128, w.shape[1]).transpose(1, 0, 2))


def _weight_inputs(w_ada, b_ada, g_mix, g_ffn, w_in, cmp_pos_k, cmp_pos_v, w_ck1, w_ck2, w_cv1, w_cv2, hgrn_lb_logits, hgrn_out_norm,
                   w_branch, w_out, w_peer_q, peer_sub_keys, peer_u, peer_v, g_final):
    f32 = np.float32
    W = np.asarray(w_in[0], f32)
    sw = (np.arange(64) + 32) % 64

    def cols(base, idx):
        return base + np.asarray(idx)

    units = []
    for a in range(4):
        x = np.concatenate([cols(a * 64, np.arange(64)), cols((a + 4) * 64, np.arange(64))])
        xs = np.concatenate([cols(a * 64, sw), cols((a + 4) * 64, sw)])
        units.append((x, xs))
    for base in (768, 1024, 512):
        x = cols(base, np.arange(128))
        xs = np.concatenate([cols(base, sw), cols(base + 64, sw)])
        units.append((x, xs))
    units.append((cols(640, np.arange(128)), None))
    for h in range(4):
        units.append((cols(1304 + h * 128, np.arange(128)), cols(1816 + h * 128, np.arange(128))))
    for hp in range(2):
        units.append((cols(2840 + (2 * hp) * 128, np.arange(128)), cols(2840 + (2 * hp + 1) * 128, np.arange(128))))
    for c in range(8):
        units.append((cols(3352 + c * 128, np.arange(128)), cols(3352 + 1024 + c * 128, np.arange(128))))
    wfm = np.zeros((22, 128, 8, 256), f32)
    for u, (a, bb) in enumerate(units):
        wfm[u, :, :, 0:128] = _kchunk(W[:, a])
        if bb is not None:
            wfm[u, :, :, 128:256] = _kchunk(W[:, bb])
    wtk = np.zeros((4, 128, 8, 256), f32)
    wtk[0, :, :, 0:128] = _kchunk(W[:, 896:1024])
    wtk[0, :, :, 128:256] = _kchunk(W[:, 1152:1280])
    wtk[1, :, :, 0:24] = _kchunk(W[:, 1280:1304])
    wtk[2] = _kchunk(W[:, 2328:2584])
    wtk[3] = _kchunk(W[:, 2584:2840])
    wa = np.asarray(w_ada[0], f32)
    wada = np.ascontiguousarray(wa.reshape(8, 128, 12, 512).transpose(2, 1, 0, 3))
    w1 = np.zeros((2, 128, 32, 128), f32)
    pos = np.zeros((2, 128, 32), f32)
    for kv, (w1_, p_) in enumerate(((w_ck1, cmp_pos_k), (w_cv1, cmp_pos_v))):
        a = np.asarray(w1_[0], f32).reshape(32, 64, 128).transpose(1, 0, 2)
        w1[kv, 0:64] = a
        w1[kv, 64:128] = a
        pp = np.asarray(p_[0], f32).T
        pos[kv, 0:64] = pp
        pos[kv, 64:128] = pp
    w2k = np.zeros((128, 256), f32)
    w2k[:, 0:64] = np.asarray(w_ck2[0], f32)
    w2k[:, 128 + 64:256] = np.asarray(w_ck2[0], f32)
    lbl = np.ascontiguousarray(np.asarray(hgrn_lb_logits, f32).reshape(2, 4, 128).transpose(2, 1, 0))
    wbr = np.ascontiguousarray(np.asarray(w_branch[0], f32).reshape(2, 4, 128, D).transpose(0, 2, 1, 3))
    wpq = np.ascontiguousarray(np.asarray(w_peer_q[0], f32).reshape(8, 128, 4, 512).transpose(2, 1, 0, 3))
    skT = np.ascontiguousarray(np.asarray(peer_sub_keys[0], f32).transpose(3, 1, 0, 2).reshape(128, 16, 128))
    uT = np.ascontiguousarray(np.asarray(peer_u[0], f32).reshape(32, 512, 8, 128).transpose(0, 3, 2, 1))
    pv = np.ascontiguousarray(np.asarray(peer_v[0], f32).reshape(32, 4, 128, D).transpose(0, 2, 1, 3))
    return dict(
        wada=wada, bada=np.asarray(b_ada, f32).reshape(1, 6 * D), gmix=np.asarray(g_mix, f32).reshape(1, D),
        gffn=np.asarray(g_ffn, f32).reshape(1, D), gfin=np.asarray(g_final, f32).reshape(1, D), wfm=wfm, wtk=wtk,
        w1=w1, pos=pos, w2k=w2k, w2v=np.ascontiguousarray(np.asarray(w_cv2[0], f32)), lbl=lbl,
        ng=np.asarray(hgrn_out_norm[0], f32).reshape(128, 1), wbr=wbr, wout=_kchunk(np.asarray(w_out[0], f32)),
        wpq=wpq, skT=skT, uT=uT, pv=pv)


def core_inputs(x, c, shared, b0, nb):
    m = dict(shared)
    m["x"] = np.ascontiguousarray(np.asarray(x[b0:b0 + nb], np.float32))
    cc = np.asarray(c[b0:b0 + nb], np.float32)
    m["cT"] = np.ascontiguousarray(cc.reshape(nb, 8, 128).transpose(2, 0, 1))
    return m


def kernel(x, c, w_ada, b_ada, g_mix, g_ffn, w_in, cmp_pos_k, cmp_pos_v, w_ck1, w_ck2, w_cv1, w_cv2, hgrn_lb_logits, hgrn_out_norm,
           w_branch, w_out, w_peer_q, peer_sub_keys, peer_u, peer_v, g_final):
    x = np.asarray(x)
    B = x.shape[0]
    nb = B // NCORES
    shared = _const_inputs()
    shared.update(_weight_inputs(w_ada, b_ada, g_mix, g_ffn, w_in, cmp_pos_k, cmp_pos_v, w_ck1, w_ck2, w_cv1, w_cv2, hgrn_lb_logits,
                                 hgrn_out_norm, w_branch, w_out, w_peer_q, peer_sub_keys, peer_u, peer_v, g_final))
    nc, _ = build_nc(nb=nb)
    in_maps = [core_inputs(x, c, shared, i * nb, nb) for i in range(NCORES)]
    res = run_bass_kernel_spmd(nc, in_maps, core_ids=list(range(NCORES)))
    out = np.concatenate([np.asarray(r["out"]).reshape(nb, T, D) for r in res.results], axis=0)
    return out.astype(np.float32)
```
